# Optimizing a Trainium2 kernel written in Bass

```python
import jax, jax.numpy as jnp
from jax import lax
import numpy as np

D_MODEL = 2048
BATCH = 4
SEQ = 2048
DEPTH = 2

HEAD_DIM = 128
N_HEADS = D_MODEL // HEAD_DIM
MLA_HEADS = N_HEADS // 4
DIL_HEADS = (N_HEADS - MLA_HEADS) // 2
NSA_HEADS = N_HEADS - MLA_HEADS - DIL_HEADS
MLA_Q_RANK = 384
MLA_KV_RANK = 128
MLA_NOPE_DIM = 128
MLA_ROPE_DIM = 64
MLA_V_DIM = HEAD_DIM
DIL_PAIRS = ((128, 1), (512, 4), (2048, 16))
NSA_CMP_LEN = 32
NSA_CMP_STRIDE = 16
NSA_CMP_HIDDEN = 256
NSA_SEL_BLOCK = 64
NSA_TOP_N = 16
NSA_WINDOW = 512
NSA_BRANCHES = 3
NSA_FORCE_SCORE = 1e4
ROPE_THETA = 500000.0
ROT_DIM = HEAD_DIM // 4
N_GROUPS = 4
EXPERTS_PER_GROUP = 8
N_EXPERTS = N_GROUPS * EXPERTS_PER_GROUP
EXPERT_FF = 512
MOE_TOP_K = 2
MOE_ROWS = 256
Q_BLOCK = 128
DN_ALPHA = (2 * DEPTH) ** 0.25
DN_BETA = (8 * DEPTH) ** -0.25
LN_EPS = 1e-5
RMS_EPS = 1e-6
NEG_INF = -1e30
D_MIX = MLA_HEADS * MLA_V_DIM + (DIL_HEADS + NSA_HEADS) * HEAD_DIM
IN_SPLITS = (MLA_Q_RANK, MLA_KV_RANK, MLA_ROPE_DIM,
             DIL_HEADS * HEAD_DIM, DIL_HEADS * HEAD_DIM, DIL_HEADS * HEAD_DIM,
             NSA_HEADS * HEAD_DIM, HEAD_DIM, HEAD_DIM, HEAD_DIM, HEAD_DIM, HEAD_DIM, HEAD_DIM,
             NSA_HEADS * NSA_BRANCHES)
IN_COLS = sum(IN_SPLITS)

kernel_name = 'hybrid_mla_dilated_nsa_hmoe'


def layer_norm(x, g, b):
    xf = x.astype(jnp.float32)
    mu = jnp.mean(xf, -1, keepdims=True)
    var = jnp.mean(jnp.square(xf - mu), -1, keepdims=True)
    return ((xf - mu) * lax.rsqrt(var + LN_EPS) * g.astype(jnp.float32) + b.astype(jnp.float32)).astype(x.dtype)


def rms_norm(x, g):
    xf = x.astype(jnp.float32)
    return (xf * lax.rsqrt(jnp.mean(jnp.square(xf), -1, keepdims=True) + RMS_EPS) * g.astype(jnp.float32)).astype(x.dtype)


def rope(x, pos, rot_dim):
    half = rot_dim // 2
    inv_freq = ROPE_THETA ** (-jnp.arange(half, dtype=jnp.float32) / half)
    ang = pos.astype(jnp.float32)[:, None] * inv_freq[None, :]
    cos = jnp.cos(ang)[:, None, :]
    sin = jnp.sin(ang)[:, None, :]
    x1 = x[..., :half].astype(jnp.float32)
    x2 = x[..., half:rot_dim].astype(jnp.float32)
    rot = jnp.concatenate([x1 * cos - x2 * sin, x2 * cos + x1 * sin], axis=-1).astype(x.dtype)
    return jnp.concatenate([rot, x[..., rot_dim:]], axis=-1)


def split_cols(h, widths):
    cuts = [int(c) for c in np.cumsum(widths)[:-1]]
    return jnp.split(h, cuts, axis=-1)


def causal_block_attention(q, k, v):
    B, S, H, dq = q.shape
    nq = S // Q_BLOCK
    scale = dq ** -0.5
    qb = q.reshape(B, nq, Q_BLOCK, H, dq).swapaxes(0, 1)
    kpos = jnp.arange(S)

    def one_block(args):
        qi, i = args
        s = jnp.einsum('bqhd,bkhd->bhqk', qi, k).astype(jnp.float32) * scale
        qpos = i * Q_BLOCK + jnp.arange(Q_BLOCK)
        mask = kpos[None, :] <= qpos[:, None]
        p = jax.nn.softmax(jnp.where(mask, s, NEG_INF), axis=-1)
        return jnp.einsum('bhqk,bkhd->bqhd', p.astype(v.dtype), v)

    out = lax.map(one_block, (qb, jnp.arange(nq)))
    return out.swapaxes(0, 1).reshape(B, S, H, v.shape[-1])


def banded_attention(q, k, v, max_dist):
    N, L, H, d = q.shape
    Hk = k.shape[2]
    R = H // Hk
    dv = v.shape[-1]
    nblk = -(-L // Q_BLOCK)
    Lp = nblk * Q_BLOCK
    nprev = -(-max_dist // Q_BLOCK)
    tail = Lp - L
    qp = jnp.pad(q, ((0, 0), (0, tail), (0, 0), (0, 0)))
    kp = jnp.pad(k, ((0, 0), (nprev * Q_BLOCK, tail), (0, 0), (0, 0)))
    vp = jnp.pad(v, ((0, 0), (nprev * Q_BLOCK, tail), (0, 0), (0, 0)))
    kb = kp.reshape(N, nprev + nblk, Q_BLOCK, Hk, d)
    vb = vp.reshape(N, nprev + nblk, Q_BLOCK, Hk, dv)
    kw = jnp.concatenate([kb[:, j:j + nblk] for j in range(nprev + 1)], axis=2)
    vw = jnp.concatenate([vb[:, j:j + nblk] for j in range(nprev + 1)], axis=2)
    qb = qp.reshape(N, nblk, Q_BLOCK, Hk, R, d)
    s = jnp.einsum('nbqgrd,nbkgd->nbgrqk', qb, kw).astype(jnp.float32) * d ** -0.5
    qpos = jnp.arange(nblk)[:, None] * Q_BLOCK + jnp.arange(Q_BLOCK)[None, :]
    kpos = jnp.arange(nblk)[:, None] * Q_BLOCK - nprev * Q_BLOCK + jnp.arange((nprev + 1) * Q_BLOCK)[None, :]
    dist = qpos[:, :, None] - kpos[:, None, :]
    mask = (dist >= 0) & (dist <= max_dist) & (kpos[:, None, :] >= 0)
    s = jnp.where(mask[None, :, None, None], s, NEG_INF)
    lse = jax.nn.logsumexp(s, axis=-1, keepdims=True)
    p = jnp.exp(s - lse)
    o = jnp.einsum('nbgrqk,nbkgd->nbqgrd', p.astype(v.dtype), vw).reshape(N, Lp, H, dv)[:, :L]
    lse = lse[..., 0].transpose(0, 1, 4, 2, 3).reshape(N, Lp, H)[:, :L]
    return o, lse


def mla_attention(q_lat, kv_lat, k_rope, q_lat_norm, w_q_up, kv_lat_norm, w_kv_up, pos):
    B, S, _ = q_lat.shape
    q = (rms_norm(q_lat, q_lat_norm) @ w_q_up).reshape(B, S, MLA_HEADS, MLA_NOPE_DIM + MLA_ROPE_DIM)
    q = jnp.concatenate([q[..., :MLA_NOPE_DIM], rope(q[..., MLA_NOPE_DIM:], pos, MLA_ROPE_DIM)], -1)
    kv = (rms_norm(kv_lat, kv_lat_norm) @ w_kv_up).reshape(B, S, MLA_HEADS, MLA_NOPE_DIM + MLA_V_DIM)
    k_nope, v = kv[..., :MLA_NOPE_DIM], kv[..., MLA_NOPE_DIM:]
    k_pe = rope(k_rope[:, :, None, :], pos, MLA_ROPE_DIM)
    k = jnp.concatenate([k_nope, jnp.broadcast_to(k_pe, (B, S, MLA_HEADS, MLA_ROPE_DIM))], -1)
    return causal_block_attention(q, k, v)


def dilated_attention(q, k, v):
    B, S, H, d = q.shape
    outs, lses = [], []
    for window, dil in DIL_PAIRS:
        L = S // dil

        def to_sub(t):
            return t.reshape(B, L, dil, H, d).transpose(0, 2, 1, 3, 4).reshape(B * dil, L, H, d)

        o, lse = banded_attention(to_sub(q), to_sub(k), to_sub(v), window // dil)
        outs.append(o.reshape(B, dil, L, H, d).transpose(0, 2, 1, 3, 4).reshape(B, S, H, d))
        lses.append(lse.reshape(B, dil, L, H).transpose(0, 2, 1, 3).reshape(B, S, H))
    wts = jax.nn.softmax(jnp.stack(lses, 0), axis=0)
    return jnp.einsum('nbsh,nbshd->bshd', wts.astype(q.dtype), jnp.stack(outs, 0))


def selected_block_attention(q, k, v, sel_idx):
    B, S, H, d = q.shape
    n_blocks = S // NSA_SEL_BLOCK
    n = sel_idx.shape[-1]
    kb = k.reshape(B, n_blocks, NSA_SEL_BLOCK, d)
    vb = v.reshape(B, n_blocks, NSA_SEL_BLOCK, v.shape[-1])
    nq = S // Q_BLOCK
    qc = q.reshape(B, nq, Q_BLOCK, H, d).swapaxes(0, 1)
    ic = sel_idx.reshape(B, nq, Q_BLOCK, n).swapaxes(0, 1)
    offs = jnp.arange(NSA_SEL_BLOCK)
    gather = jax.vmap(lambda blocks, ids: blocks[ids])

    def one_chunk(args):
        qi, ii, c = args
        kg = gather(kb, ii).reshape(B, Q_BLOCK, n * NSA_SEL_BLOCK, d)
        vg = gather(vb, ii).reshape(B, Q_BLOCK, n * NSA_SEL_BLOCK, v.shape[-1])
        kpos = (ii[..., None] * NSA_SEL_BLOCK + offs).reshape(B, Q_BLOCK, n * NSA_SEL_BLOCK)
        qpos = c * Q_BLOCK + jnp.arange(Q_BLOCK)
        mask = kpos <= qpos[None, :, None]
        s = jnp.einsum('bqhd,bqkd->bqhk', qi, kg).astype(jnp.float32) * d ** -0.5
        p = jax.nn.softmax(jnp.where(mask[:, :, None, :], s, NEG_INF), axis=-1)
        return jnp.einsum('bqhk,bqkd->bqhd', p.astype(v.dtype), vg)

    out = lax.map(one_chunk, (qc, ic, jnp.arange(nq)))
    return out.swapaxes(0, 1).reshape(B, S, H, v.shape[-1])


def nsa_attention(q, k_cmp, v_cmp, k_slc, v_slc, k_win, v_win, gates,
                  cmp_pos_k, cmp_w1_k, cmp_w2_k, cmp_pos_v, cmp_w1_v, cmp_w2_v, pos):
    B, S, H, d = q.shape
    q = rope(q, pos, ROT_DIM)
    n_chunk = S // NSA_CMP_STRIDE
    n_cmp = n_chunk - 1

    def compress(t, pe, w1, w2):
        ch = t.reshape(B, n_chunk, NSA_CMP_STRIDE, d)
        blocks = jnp.concatenate([ch[:, :-1], ch[:, 1:]], axis=2)
        h = jax.nn.gelu((blocks + pe).reshape(B, n_cmp, NSA_CMP_LEN * d) @ w1)
        return h @ w2

    cmp_start = jnp.arange(n_cmp) * NSA_CMP_STRIDE
    cmp_end = cmp_start + NSA_CMP_LEN - 1
    kc = rope(compress(k_cmp, cmp_pos_k, cmp_w1_k, cmp_w2_k)[:, :, None, :], cmp_end, ROT_DIM)[:, :, 0]
    vc = compress(v_cmp, cmp_pos_v, cmp_w1_v, cmp_w2_v)
    valid_c = cmp_end[None, :] <= pos[:, None]
    s = jnp.einsum('bshd,bcd->bhsc', q, kc).astype(jnp.float32) * d ** -0.5
    p_cmp = jax.nn.softmax(jnp.where(valid_c, s, NEG_INF), axis=-1) * valid_c
    o_cmp = jnp.einsum('bhsc,bcd->bshd', p_cmp.astype(q.dtype), vc)
    n_sel_blocks = S // NSA_SEL_BLOCK
    sel_start = jnp.arange(n_sel_blocks) * NSA_SEL_BLOCK
    cover = ((cmp_start[:, None] < sel_start[None, :] + NSA_SEL_BLOCK) &
             (cmp_start[:, None] + NSA_CMP_LEN > sel_start[None, :])).astype(jnp.float32)
    imp = jnp.einsum('bhsc,cj->bsj', p_cmp, cover)
    jj = jnp.arange(n_sel_blocks)[None, :]
    qblk = (pos // NSA_SEL_BLOCK)[:, None]
    valid_s = sel_start[None, :] <= pos[:, None]
    forced = (jj == 0) | (jj == qblk) | (jj == qblk - 1)
    score = jnp.where(valid_s, jnp.where(forced, NSA_FORCE_SCORE, imp), -1.0)
    _, sel_idx = lax.top_k(score, min(NSA_TOP_N, n_sel_blocks))
    o_slc = selected_block_attention(q, rope(k_slc[:, :, None, :], pos, ROT_DIM)[:, :, 0], v_slc, sel_idx)
    o_win, _ = banded_attention(q, rope(k_win[:, :, None, :], pos, ROT_DIM), v_win[:, :, None, :], NSA_WINDOW - 1)
    g = jax.nn.sigmoid(gates.astype(jnp.float32)).reshape(B, S, H, NSA_BRANCHES).astype(q.dtype)
    return g[..., 0:1] * o_cmp + g[..., 1:2] * o_slc + g[..., 2:3] * o_win


def hybrid_mixer(x, w_in, q_lat_norm, w_q_up, kv_lat_norm, w_kv_up,
                 cmp_pos_k, cmp_w1_k, cmp_w2_k, cmp_pos_v, cmp_w1_v, cmp_w2_v, w_out):
    B, S, _ = x.shape
    pos = jnp.arange(S, dtype=jnp.int32)
    (q_lat, kv_lat, k_rope, dq, dk, dv, nq, nkc, nvc, nks, nvs, nkw, nvw, ngate) = split_cols(x @ w_in, IN_SPLITS)

    def heads(t, h):
        return t.reshape(B, S, h, -1)

    o_mla = mla_attention(q_lat, kv_lat, k_rope, q_lat_norm, w_q_up, kv_lat_norm, w_kv_up, pos)
    o_dil = dilated_attention(rope(heads(dq, DIL_HEADS), pos, ROT_DIM),
                              rope(heads(dk, DIL_HEADS), pos, ROT_DIM), heads(dv, DIL_HEADS))
    o_nsa = nsa_attention(heads(nq, NSA_HEADS), nkc, nvc, nks, nvs, nkw, nvw, ngate,
                          cmp_pos_k, cmp_w1_k, cmp_w2_k, cmp_pos_v, cmp_w1_v, cmp_w2_v, pos)
    o = jnp.concatenate([o_mla.reshape(B, S, -1), o_dil.reshape(B, S, -1), o_nsa.reshape(B, S, -1)], -1)
    return o @ w_out


def hier_moe(x, w_grp, w_exp, w_gate, w_up, w_down):
    B, S, D = x.shape
    T = B * S
    xf = x.reshape(T, D)
    grp_prob = jax.nn.softmax((xf @ w_grp).astype(jnp.float32), axis=-1)
    g_idx = jnp.argmax(grp_prob, axis=-1)
    p_g = jnp.take_along_axis(grp_prob, g_idx[:, None], 1)[:, 0]
    e_logits = (xf @ w_exp).astype(jnp.float32).reshape(T, N_GROUPS, EXPERTS_PER_GROUP)
    e_logits = jnp.take_along_axis(e_logits, g_idx[:, None, None], 1)[:, 0]
    top_v, top_i = lax.top_k(e_logits, MOE_TOP_K)
    gate = p_g[:, None] * jax.nn.softmax(top_v, axis=-1)
    eid = g_idx[:, None].astype(jnp.int32) * EXPERTS_PER_GROUP + top_i.astype(jnp.int32)
    M = T * MOE_TOP_K
    n_blocks = -(-M // MOE_ROWS) + N_EXPERTS
    tok = jnp.repeat(jnp.arange(T, dtype=jnp.int32), MOE_TOP_K)
    ex = eid.reshape(M)
    order = jnp.argsort(ex)
    ex_s, tok_s, gw_s = ex[order], tok[order], gate.reshape(M)[order]
    counts = jnp.zeros((N_EXPERTS,), jnp.int32).at[ex].add(1)
    seg_start = jnp.cumsum(counts) - counts
    padded = (counts + MOE_ROWS - 1) // MOE_ROWS * MOE_ROWS
    pad_end = jnp.cumsum(padded)
    dest = (pad_end - padded)[ex_s] + jnp.arange(M, dtype=jnp.int32) - seg_start[ex_s]
    buf_tok = jnp.full((n_blocks * MOE_ROWS,), T, jnp.int32).at[dest].set(tok_s)
    buf_w = jnp.zeros((n_blocks * MOE_ROWS,), x.dtype).at[dest].set(gw_s.astype(x.dtype))
    blk_start = jnp.arange(n_blocks, dtype=jnp.int32) * MOE_ROWS
    blk_ex = jnp.minimum(jnp.searchsorted(pad_end, blk_start, side='right'), N_EXPERTS - 1)
    x_pad = jnp.concatenate([xf, jnp.zeros((1, D), xf.dtype)], axis=0)
    xb = x_pad[buf_tok].reshape(n_blocks, MOE_ROWS, D)

    def expert_rows(args):
        rows, e = args
        h = jax.nn.silu(rows @ w_gate[e]) * (rows @ w_up[e])
        return h @ w_down[e]

    yb = lax.map(expert_rows, (xb, blk_ex)).reshape(n_blocks * MOE_ROWS, D)
    out = jnp.zeros((T + 1, D), x.dtype).at[buf_tok].add(yb * buf_w[:, None])[:T]
    return out.reshape(B, S, D)


def setup_inputs(seed: int = 0) -> dict:
    key = jax.random.key(seed)
    ks = jax.random.split(key, 24)
    L = DEPTH

    def nrm(k, shape, scale):
        return jax.random.normal(k, shape, jnp.float32) * scale

    cmp_in = NSA_CMP_LEN * HEAD_DIM
    return {
        'x': nrm(ks[0], (BATCH, SEQ, D_MODEL), 1.0),
        'w_in': nrm(ks[1], (L, D_MODEL, IN_COLS), D_MODEL ** -0.5),
        'q_lat_norm': 1.0 + nrm(ks[2], (L, MLA_Q_RANK), 0.02),
        'w_q_up': nrm(ks[3], (L, MLA_Q_RANK, MLA_HEADS * (MLA_NOPE_DIM + MLA_ROPE_DIM)), MLA_Q_RANK ** -0.5),
        'kv_lat_norm': 1.0 + nrm(ks[4], (L, MLA_KV_RANK), 0.02),
        'w_kv_up': nrm(ks[5], (L, MLA_KV_RANK, MLA_HEADS * (MLA_NOPE_DIM + MLA_V_DIM)), MLA_KV_RANK ** -0.5),
        'cmp_pos_k': nrm(ks[6], (L, NSA_CMP_LEN, HEAD_DIM), 0.1),
        'cmp_w1_k': nrm(ks[7], (L, cmp_in, NSA_CMP_HIDDEN), cmp_in ** -0.5),
        'cmp_w2_k': nrm(ks[8], (L, NSA_CMP_HIDDEN, HEAD_DIM), NSA_CMP_HIDDEN ** -0.5),
        'cmp_pos_v': nrm(ks[9], (L, NSA_CMP_LEN, HEAD_DIM), 0.1),
        'cmp_w1_v': nrm(ks[10], (L, cmp_in, NSA_CMP_HIDDEN), cmp_in ** -0.5),
        'cmp_w2_v': nrm(ks[11], (L, NSA_CMP_HIDDEN, HEAD_DIM), NSA_CMP_HIDDEN ** -0.5),
        'w_out': nrm(ks[12], (L, D_MIX, D_MODEL), D_MIX ** -0.5 * DN_BETA),
        'ln1_g': 1.0 + nrm(ks[13], (L, D_MODEL), 0.02),
        'ln1_b': nrm(ks[14], (L, D_MODEL), 0.02),
        'w_grp': nrm(ks[15], (L, D_MODEL, N_GROUPS), D_MODEL ** -0.5),
        'w_exp': nrm(ks[16], (L, D_MODEL, N_EXPERTS), D_MODEL ** -0.5),
        'w_gate': nrm(ks[17], (L, N_EXPERTS, D_MODEL, EXPERT_FF), D_MODEL ** -0.5),
        'w_up': nrm(ks[18], (L, N_EXPERTS, D_MODEL, EXPERT_FF), D_MODEL ** -0.5),
        'w_down': nrm(ks[19], (L, N_EXPERTS, EXPERT_FF, D_MODEL), EXPERT_FF ** -0.5 * DN_BETA),
        'ln2_g': 1.0 + nrm(ks[20], (L, D_MODEL), 0.02),
        'ln2_b': nrm(ks[21], (L, D_MODEL), 0.02),
    }


def reference(x, w_in, q_lat_norm, w_q_up, kv_lat_norm, w_kv_up, cmp_pos_k, cmp_w1_k, cmp_w2_k,
              cmp_pos_v, cmp_w1_v, cmp_w2_v, w_out, ln1_g, ln1_b, w_grp, w_exp, w_gate, w_up, w_down,
              ln2_g, ln2_b):
    for l in range(DEPTH):
        mix = hybrid_mixer(x, w_in[l], q_lat_norm[l], w_q_up[l], kv_lat_norm[l], w_kv_up[l],
                           cmp_pos_k[l], cmp_w1_k[l], cmp_w2_k[l], cmp_pos_v[l], cmp_w1_v[l], cmp_w2_v[l],
                           w_out[l])
        x = layer_norm(DN_ALPHA * x + mix, ln1_g[l], ln1_b[l])
        ffn = hier_moe(x, w_grp[l], w_exp[l], w_gate[l], w_up[l], w_down[l])
        x = layer_norm(DN_ALPHA * x + ffn, ln2_g[l], ln2_b[l])
    return x
```

```python
import contextlib
import numpy as np
import concourse.bass as bass
import concourse.mybir as mybir
from concourse.bass_utils import run_bass_kernel_spmd

F32 = mybir.dt.float32
BF16 = mybir.dt.bfloat16
I32 = mybir.dt.int32
ALU = mybir.AluOpType
AF = mybir.ActivationFunctionType
AX = mybir.AxisListType

S = 2048
D = 2048
NT = 16
DEPTH = 2
IN_COLS = 4434
CAP = 256
NEXP = 32
THETA = 500000.0
ALPHA = (2 * DEPTH) ** 0.25
LN_EPS = 1e-5
RMS_EPS = 1e-6
C_QLAT, C_KVLAT, C_KROPE = 0, 384, 512
C_DQ, C_DK, C_DV = 576, 1344, 2112
C_NQ = 2880
C_NKC, C_NVC, C_NKS, C_NVS, C_NKW, C_NVW = 3648, 3776, 3904, 4032, 4160, 4288
C_GATE = 4416


class Prog:
    CENG = ("pe", "act", "dve", "pool")
    DMAQ = ("sp", "act", "pool")
    RING = 8

    def __init__(self, nc):
        self.nc = nc
        self.es = contextlib.ExitStack()
        self.streams = {e: [] for e in ("pe", "act", "dve", "pool", "sp")}
        self.cnt = {e: 0 for e in self.CENG}
        self.sems = {}
        for e in self.CENG:
            self.sems[e] = self.es.enter_context(nc.semaphore("s_" + e))
        self.dsems = {}
        self.dcnt = {}
        for q in self.DMAQ:
            self.dcnt[q] = 0
            for i in range(self.RING):
                self.dsems[(q, i)] = self.es.enter_context(nc.semaphore("d_%s%d" % (q, i)))
        self.seen = {s: {} for s in self.streams}
        self.tiles = {}

    def sb(self, name, shape, dt):
        return self.es.enter_context(self.nc.sbuf_tensor(name, list(shape), dt))

    def ps(self, name, shape, dt=F32):
        return self.es.enter_context(self.nc.psum_tensor(name, list(shape), dt))

    def _need(self, stream, ev, waits, is_dma=False):
        if ev is None:
            return
        key, val = ev
        if key == stream and key == "pe" and not is_dma:
            return
        if self.seen[stream].get(key, 0) >= val:
            return
        if waits.get(key, 0) < val:
            waits[key] = val

    def _deps(self, stream, reads, writes, is_dma=False):
        waits = {}
        for t in reads:
            st = self.tiles.setdefault(t, {"w": None, "r": []})
            self._need(stream, st["w"], waits, is_dma)
        for t in writes:
            st = self.tiles.setdefault(t, {"w": None, "r": []})
            self._need(stream, st["w"], waits, is_dma)
            for ev in st["r"]:
                self._need(stream, ev, waits, is_dma)
        return waits

    def _commit(self, ev, reads, writes):
        for t in reads:
            if t in writes:
                continue
            r = self.tiles[t]["r"]
            r.append(ev)
            if len(r) > 48:
                best = {}
                for k, v in r:
                    if best.get(k, 0) < v:
                        best[k] = v
                self.tiles[t]["r"] = list(best.items())
        for t in writes:
            self.tiles[t]["w"] = ev
            self.tiles[t]["r"] = []

    def _sem(self, key):
        return self.sems[key] if key in self.sems else self.dsems[key]

    def _emit_waits(self, stream, waits):
        for key, val in waits.items():
            self.streams[stream].append(("w", self._sem(key), val))
            self.seen[stream][key] = val

    def op(self, eng, fn, reads=(), writes=()):
        reads, writes = tuple(reads), tuple(writes)
        self._emit_waits(eng, self._deps(eng, reads, writes))
        self.cnt[eng] += 1
        ev = (eng, self.cnt[eng])
        self.streams[eng].append(("c", fn, self.sems[eng]))
        self._commit(ev, reads, writes)
        return ev

    def _dma_common(self, q, reads, writes):
        waits = self._deps(q, reads, writes, True)
        k = self.dcnt[q]
        self.dcnt[q] += 1
        key = (q, k % self.RING)
        tgt = 16 * (k // self.RING + 1)
        if tgt > 16:
            prev = tgt - 16
            if self.seen[q].get(key, 0) < prev and waits.get(key, 0) < prev:
                waits[key] = prev
        self._emit_waits(q, waits)
        return key, tgt

    def dma(self, q, out, in_, reads=(), writes=(), **kw):
        reads, writes = tuple(reads), tuple(writes)
        key, tgt = self._dma_common(q, reads, writes)
        self.streams[q].append(("d", out, in_, kw, self.dsems[key]))
        self._commit((key, tgt), reads, writes)

    def idma(self, fn, reads=(), writes=()):
        reads, writes = tuple(reads), tuple(writes)
        key, tgt = self._dma_common("pool", reads, writes)
        self.streams["pool"].append(("i", fn, self.dsems[key]))
        self._commit((key, tgt), reads, writes)

    def barrier(self):
        cur = {}
        for e in self.CENG:
            if self.cnt[e] > 0:
                cur[e] = self.cnt[e]
        for q in self.DMAQ:
            k = self.dcnt[q]
            for i in range(self.RING):
                n = (k - i + self.RING - 1) // self.RING if k > i else 0
                if n > 0:
                    cur[(q, i)] = 16 * n
        for s in self.streams:
            waits = {}
            for key, val in cur.items():
                if self.seen[s].get(key, 0) < val:
                    waits[key] = val
            self._emit_waits(s, waits)
        self.tiles = {}

    def emit(self):
        nc = self.nc
        streams = self.streams

        def run(e, lst):
            for it in lst:
                k = it[0]
                if k == "w":
                    e.wait_ge(it[1], it[2])
                elif k == "c":
                    it[1](e).then_inc(it[2], 1)
                elif k == "d":
                    e.dma_start(out=it[1], in_=it[2], **it[3]).then_inc(it[4], 16)
                elif k == "i":
                    it[1](e).then_inc(it[2], 16)

        with nc.Block() as block:
            @block.sync
            def _(e):
                run(e, streams["sp"])

            @block.tensor
            def _(e):
                run(e, streams["pe"])

            @block.scalar
            def _(e):
                run(e, streams["act"])

            @block.vector
            def _(e):
                run(e, streams["dve"])

            @block.gpsimd
            def _(e):
                run(e, streams["pool"])
        self.es.close()


def make_consts():
    c = {}
    c["ident"] = np.eye(128, dtype=np.float32)
    pos = np.arange(S, dtype=np.float32)

    def rope_tab(rot, p):
        half = rot // 2
        inv = (np.float32(THETA) ** (-np.arange(half, dtype=np.float32) / np.float32(half))).astype(np.float32)
        ang = (p.astype(np.float32)[None, :] * inv[:, None]).astype(np.float32)
        cos = np.cos(ang.astype(np.float64)).astype(np.float32)
        sin = np.sin(ang.astype(np.float64)).astype(np.float32)
        t = np.zeros((rot, 2, p.shape[0]), np.float32)
        t[:half, 0], t[half:, 0] = cos, cos
        t[:half, 1], t[half:, 1] = -sin, sin
        return t

    c["cs32"] = rope_tab(32, pos)
    c["cs64"] = rope_tab(64, pos)
    kc = np.zeros((32, 2, 128), np.float32)
    kc[:, :, :127] = rope_tab(32, (np.arange(127) * 16 + 31).astype(np.float32))
    c["cskc"] = kc

    def perm(n):
        m = np.zeros((n, n), np.float32)
        h = n // 2
        for j in range(n):
            m[(j + h) % n, j] = 1.0
        return m

    c["pm32"] = perm(32)
    c["pm64"] = perm(64)
    kk = np.arange(128)[:, None]
    qq = np.arange(128)[None, :]
    md = np.zeros((128, 16, 128), np.float32)
    for delta in range(16):
        d = 128 * delta + qq - kk
        m = ((d >= 0) & (d <= 128)).astype(np.float32) + ((d >= 0) & (d % 4 == 0) & (d <= 512)).astype(np.float32) \
            + ((d >= 0) & (d % 16 == 0)).astype(np.float32)
        md[:, 15 - delta, :] = m
    c["mdil"] = md.reshape(128, 2048)
    c["tri"] = (kk <= qq).astype(np.float32)
    c["upp"] = (kk > qq).astype(np.float32)
    cc = np.arange(128)[:, None]
    vcm = ((16 * cc + 31) <= np.arange(S)[None, :]).astype(np.float32)
    vcm[127] = 0
    c["vcm"] = vcm
    cs = np.arange(128) * 16
    ss = np.arange(32) * 64
    cover = ((cs[:, None] < ss[None, :] + 64) & (cs[:, None] + 32 > ss[None, :])).astype(np.float32)
    cover[127] = 0
    c["cover"] = cover
    p = np.arange(S)
    jj = np.arange(32)[None, :]
    qblk = (p // 64)[:, None]
    valid = (ss[None, :] <= p[:, None])
    forced = (jj == 0) | (jj == qblk) | (jj == qblk - 1)
    selA = (valid & ~forced).astype(np.float32)
    selB = np.where(valid, np.where(forced, 1e4, 0.0), -1.0).astype(np.float32)
    c["selA"] = selA.reshape(16, 128, 32).transpose(1, 0, 2).copy()
    c["selB"] = selB.reshape(16, 128, 32).transpose(1, 0, 2).copy()
    E = np.zeros((32, 16, 128), np.float32)
    for kt in range(16):
        E[2 * kt, kt, :64] = 1
        E[2 * kt + 1, kt, 64:] = 1
    c["emat"] = E.reshape(32, 2048)
    c["ltri"] = (kk < qq).astype(np.float32)
    c["ecap"] = np.tile((np.arange(32) * CAP).astype(np.float32)[None, :], (128, 1))
    return c


CONST_SHAPES = {"ident": (128, 128), "cs32": (32, 2, 2048), "cs64": (64, 2, 2048), "cskc": (32, 2, 128),
                "pm32": (32, 32), "pm64": (64, 64), "mdil": (128, 2048), "tri": (128, 128), "upp": (128, 128),
                "vcm": (128, 2048), "cover": (128, 32), "selA": (128, 16, 32), "selB": (128, 16, 32),
                "emat": (32, 2048), "ltri": (128, 128), "ecap": (128, 32)}

W_SHAPES = {"w_in": (2, 2048, 4434), "q_lat_norm": (2, 384), "w_q_up": (2, 384, 768), "kv_lat_norm": (2, 128),
            "w_kv_up": (2, 128, 1024), "cmp_pos_k": (2, 32, 128), "cmp_w1_k": (2, 4096, 256),
            "cmp_w2_k": (2, 256, 128), "cmp_pos_v": (2, 32, 128), "cmp_w1_v": (2, 4096, 256),
            "cmp_w2_v": (2, 256, 128), "w_out": (2, 2048, 2048), "ln1_g": (2, 2048), "ln1_b": (2, 2048),
            "w_grp": (2, 2048, 4), "w_exp": (2, 2048, 32), "w_gate": (2, 32, 2048, 512),
            "w_up": (2, 32, 2048, 512), "w_down": (2, 32, 512, 2048), "ln2_g": (2, 2048), "ln2_b": (2, 2048)}


def pe_group(mms):
    def fn(e):
        ins = None
        for (o, l, r, st, sp) in mms:
            ins = e.matmul(o, lhsT=l, rhs=r, start=st, stop=sp)
        return ins
    return fn


def build(n_layers=DEPTH, stage="full"):
    nc = bass.Bass("TRN2", target_bir_lowering=False)

    def din(name, shape, dt=F32):
        return nc.dram_tensor(name, list(shape), dt, kind="ExternalInput").ap()

    def dscr(name, shape, dt):
        return nc.dram_tensor(name, list(shape), dt, kind="Internal").ap()

    x_d = din("x", [S, D])
    W = {k: din(k, v) for k, v in W_SHAPES.items()}
    CD = {k: din("c_" + k, v) for k, v in CONST_SHAPES.items()}
    out_d = nc.dram_tensor("out", [S, D], F32, kind="ExternalOutput").ap()
    dbg_d = dbg2_d = None
    if stage != "full":
        dbg_d = nc.dram_tensor("dbg", [S, D], F32, kind="ExternalOutput").ap()
        dbg2_d = nc.dram_tensor("dbg2", [S, D], F32, kind="ExternalOutput").ap()
        dbg3_d = nc.dram_tensor("dbg3", [8, 128, S], BF16, kind="ExternalOutput").ap()

    o_scr = dscr("o_scr", [S, D], BF16)
    nq_scr = dscr("nq_scr", [6, 128, S], BF16)
    ocmp_scr = dscr("ocmp_scr", [6, S, 128], F32)
    x1_scr = dscr("x1_scr", [S, D], F32)
    x1b_scr = dscr("x1b_scr", [S, D], BF16)
    xres_scr = dscr("xres_scr", [S, D], F32)
    xg_scr = dscr("xg_scr", [NEXP * CAP, D], BF16)
    y_scr = dscr("y_scr", [NEXP * CAP, D], F32)

    P = Prog(nc)
    XT = P.sb("XT", [128, 16, S], BF16)
    ARENA = P.sb("ARENA", [128, 30 * 1024], F32)
    ident = P.sb("ident", [128, 128], F32)
    identb = P.sb("identb", [128, 128], BF16)
    cs32 = P.sb("cs32", [32, 2, S], F32)
    pm32 = P.sb("pm32", [32, 32], BF16)
    pm64 = P.sb("pm64", [64, 64], BF16)
    onesb = P.sb("onesb", [128, 128], BF16)
    tri = P.sb("tri", [128, 128], BF16)
    upp = P.sb("upp", [128, 128], BF16)
    ltri = P.sb("ltri", [128, 128], BF16)
    ecap = P.sb("ecap", [128, 32], F32)
    gates = P.sb("gates", [128, 16, 18], F32)
    slots = P.sb("slots", [128, 16, 2], I32)
    gatew = P.sb("gatew", [128, 16, 2], F32)
    mk = P.sb("mk", [128, 16, 32], BF16)
    oh = P.sb("oh", [128, 16, 2, 32], BF16)
    PS = [P.ps("ps%d" % i, [128, 512], F32) for i in range(8)]
    pA, pB, pS0, pS1, pO0, pO1, pM0, pM1 = PS
    PN = {id(t): "ps%d" % i for i, t in enumerate(PS)}

    def pn(t):
        return PN[id(t)]

    arena_off = [0]

    def carve(shape, dt):
        n = int(np.prod(shape[1:]))
        words = n if dt in (F32, I32) else (n + 1) // 2
        words = (words + 15) // 16 * 16
        o = arena_off[0]
        arena_off[0] += words
        assert arena_off[0] <= 30 * 1024, ("arena overflow", arena_off[0])
        v = ARENA[0:shape[0], o:o + words]
        if dt != F32:
            v = v.bitcast(dt)
        v = v[:, 0:n]
        if len(shape) == 3:
            v = v.rearrange("p (a b) -> p a b", a=shape[1])
        elif len(shape) == 4:
            v = v.rearrange("p (a b c) -> p a b c", a=shape[1], b=shape[2])
        return v

    def phase():
        P.barrier()
        arena_off[0] = 0

    P.dma("sp", ident[:], CD["ident"], writes=["ident"])
    P.dma("pool", identb[:], CD["ident"], writes=["identb"])
    P.dma("sp", cs32[:], CD["cs32"], writes=["cs32"])
    P.dma("pool", pm32[:], CD["pm32"], writes=["pm32"])
    P.dma("pool", pm64[:], CD["pm64"], writes=["pm64"])
    P.dma("pool", tri[:], CD["tri"], writes=["tri"])
    P.dma("pool", upp[:], CD["upp"], writes=["upp"])
    P.dma("pool", ltri[:], CD["ltri"], writes=["ltri"])
    P.dma("sp", ecap[:], CD["ecap"], writes=["ecap"])
    P.op("dve", lambda e: e.memset(onesb[:], 1.0), writes=["onesb"])
    epsr = P.sb("epsr", [128, 2], F32)
    P.op("dve", lambda e: e.memset(epsr[:, 0:1], RMS_EPS), writes=["epsr"])
    P.op("dve", lambda e: e.memset(epsr[:, 1:2], LN_EPS), writes=["epsr"])

    arena_off[0] = 0
    ztile = carve([128, 8192], BF16)
    P.op("pool", lambda e: e.memset(ztile, 0.0), writes=["ztile"])
    xg_flat = xg_scr.rearrange("(a p) d -> a p d", p=128)
    for a in range(NEXP * CAP // 128):
        P.dma("sp", xg_flat[a], ztile[:, 0:D], reads=["ztile"], writes=["xg_scr"])

    def layer(l, x_src, last):
        phase()
        QTS = [carve([128, S], BF16) for _ in range(2)]
        QR = carve([64, S], BF16)
        KT = [carve([128, S], BF16) for _ in range(2)]
        KR = carve([64, S], BF16)
        VA = carve([128, 16, 2, 129], BF16)
        QLT = carve([128, 3, S], BF16)
        KVLT = carve([128, S], BF16)
        SCR16 = carve([128, 4096], F32)
        WST = [carve([128, 16, 128], BF16) for _ in range(3)]
        PT = [carve([128, 512], BF16) for _ in range(3)]
        mdil = carve([128, S], BF16)
        vcm = mdil
        emat = mdil[0:32, :]
        nbT = carve([32, S], BF16)
        selA = carve([128, 16, 32], F32)
        selB = carve([128, 16, 32], F32)
        imp = carve([128, 16, 32], F32)
        cover = carve([128, 32], F32)
        rt12 = carve([128, 1024], F32)
        rt1 = rt12[0:64, 0:512]
        rt2 = rt12[0:64, 512:1024]
        sqb = carve([128, 3, 512], BF16)
        rstd = carve([128, 512], F32)
        wqu = carve([128, 3, 768], BF16)
        wkvu = carve([128, 1024], BF16)
        qg = carve([128, 4], F32)
        xin = [SCR16[:, 0:2048], SCR16[:, 2048:4096]]
        fin = carve([128, 8], F32)
        ofin = [carve([128, 128], BF16) for _ in range(2)]
        onsa = QLT[:, :, :].rearrange("p a b -> p (a b)")[:, 0:4096].bitcast(F32).rearrange("p (a b) -> p a b", a=16)
        ocl = [carve([128, 128], F32) for _ in range(2)]
        KCT = carve([128, 128], BF16)
        VCA = carve([128, 161], BF16)
        peT = carve([128, 32], F32)
        XPE = QLT[:, :, :].rearrange("p a b -> p (a b)")[:, 0:32 * 127].rearrange("p (a b) -> p a b", a=32)
        hT = carve([128, 2, 127], BF16)
        w2 = carve([128, 2, 128], BF16)
        cskc = carve([32, 2, 128], F32)
        hsc = [carve([128, 127], F32) for _ in range(3)]
        selw = rt12[:, :].rearrange("p (a b) -> p a b", a=32)
        selr = carve([128, 32], F32)

        P.dma("pool", mdil, CD["mdil"], writes=["msk"])
        P.dma("sp", selA, CD["selA"], writes=["selA"])
        P.dma("sp", selB, CD["selB"], writes=["selB"])
        P.dma("sp", cover, CD["cover"], writes=["cover"])
        P.dma("sp", cskc, CD["cskc"], writes=["cskc"])
        cs64 = SCR16[0:64, :].rearrange("p (a b) -> p a b", a=2)
        P.dma("pool", wqu, W["w_q_up"][l].rearrange("(kc p) n -> p kc n", p=128), writes=["wqu"])
        P.dma("pool", wkvu, W["w_kv_up"][l], writes=["wkvu"])
        P.dma("sp", qg[:, 0:3], W["q_lat_norm"][l].rearrange("(kc p) -> p kc", p=128), writes=["qg"], allow_slow_non_contiguous=True)
        P.dma("sp", qg[:, 3:4], W["kv_lat_norm"][l].rearrange("(kc p) -> p kc", p=128), writes=["qg"], allow_slow_non_contiguous=True)
        P.op("dve", lambda e: e.memset(VA[:, :, :, 128:129], 1.0), writes=["va0", "va1"])
        P.op("dve", lambda e: e.memset(imp, 0.0), writes=["imp"])

        for t in range(NT):
            xt_ = xin[t % 2]
            P.dma("sp", xt_, x_src[t * 128:(t + 1) * 128, :], writes=["xin%d" % (t % 2)])
            for g in range(4):
                pb = (pA, pB)[(t * 4 + g) % 2]
                P.op("pe", (lambda pb=pb, xt_=xt_, g=g: lambda e: [e.transpose(out=pb[:, j * 128:(j + 1) * 128], in_=xt_[:, (g * 4 + j) * 128:(g * 4 + j + 1) * 128], identity=ident[:]) for j in range(4)][-1])(),
                     reads=["xin%d" % (t % 2), "ident"], writes=[pn(pb)])
                eng = "act" if g % 2 == 0 else "dve"
                dst = XT[:, g * 4:(g + 1) * 4, t * 128:(t + 1) * 128]
                src = pb[:, :].rearrange("p (a b) -> p a b", a=4)
                if eng == "act":
                    P.op("act", lambda e, dst=dst, src=src: e.activation(out=dst, in_=src, func=AF.Copy), writes=[pn(pb), "XT"])
                else:
                    P.op("dve", lambda e, dst=dst, src=src: e.tensor_copy(out=dst, in_=src), writes=[pn(pb), "XT"])

        P.dma("sp", cs64, CD["cs64"], writes=["scr16", "xin0", "xin1"])
        wst_i = [0]
        pb_i = [0]

        def next_pb():
            pb_i[0] += 1
            return (pA, pB)[pb_i[0] % 2]

        def load_w(col0, ncols):
            s = wst_i[0] % 3
            wst_i[0] += 1
            P.dma("pool", WST[s][:, :, 0:ncols], W["w_in"][l, :, col0:col0 + ncols].rearrange("(kc p) n -> p kc n", p=128),
                  writes=["wst%d" % s])
            return s

        def proj_fm(col0, ncols, evac):
            s = load_w(col0, ncols)
            for tc in range(4):
                pb = next_pb()
                mms = [(pb[0:ncols, :], WST[s][:, kc, 0:ncols], XT[:, kc, tc * 512:(tc + 1) * 512], kc == 0, kc == 15) for kc in range(16)]
                P.op("pe", pe_group(mms), reads=["wst%d" % s, "XT"], writes=[pn(pb)])
                evac(pb, tc)

        def proj_tm(col0, ncols, evac):
            s = load_w(col0, ncols)
            for kt in range(NT):
                pb = next_pb()
                mms = [(pb[:, 0:ncols], XT[:, kc, kt * 128:(kt + 1) * 128], WST[s][:, kc, 0:ncols], kc == 0, kc == 15) for kc in range(16)]
                P.op("pe", pe_group(mms), reads=["wst%d" % s, "XT"], writes=[pn(pb)])
                evac(pb, kt)

        def rope_evac(dst, dname, nrows, R, cs, csname, pm, pmname):
            def ev(pb, tc, ncol=512, c0=None):
                c0 = tc * 512 if c0 is None else c0
                sl = slice(c0, c0 + ncol)
                tn = "%s.%d" % (dname, tc)
                P.op("act", lambda e: e.activation(out=dst[0:nrows, sl], in_=pb[0:nrows, 0:ncol], func=AF.Copy), writes=[pn(pb), tn])
                pm_ = pM0
                P.op("pe", lambda e: e.matmul(pm_[0:R, 0:ncol], lhsT=pm[0:R, 0:R], rhs=dst[0:R, sl], start=True, stop=True),
                     reads=[tn, pmname], writes=[pn(pm_)])
                P.op("dve", lambda e: e.tensor_tensor(out=rt1[0:R, 0:ncol], in0=dst[0:R, sl], in1=cs[0:R, 0, sl], op=ALU.mult),
                     reads=[tn, csname], writes=["rt1"])
                P.op("dve", lambda e: e.tensor_tensor(out=rt2[0:R, 0:ncol], in0=pm_[0:R, 0:ncol], in1=cs[0:R, 1, sl], op=ALU.mult),
                     reads=[csname], writes=["rt2", pn(pm_)])
                P.op("pool", lambda e: e.tensor_tensor(out=dst[0:R, sl], in0=rt1[0:R, 0:ncol], in1=rt2[0:R, 0:ncol], op=ALU.add),
                     reads=["rt1", "rt2"], writes=[tn])
            return ev

        def copy_evac(dst, dname, nrows):
            def ev(pb, tc):
                sl = slice(tc * 512, (tc + 1) * 512)
                P.op("act", lambda e: e.activation(out=dst[0:nrows, sl], in_=pb[0:nrows, :], func=AF.Copy),
                     writes=[pn(pb), "%s.%d" % (dname, tc)])
            return ev

        def v_evac(slot, ncols=128):
            def ev(pb, kt):
                P.op("dve", lambda e: e.tensor_copy(out=VA[:, kt, slot, 0:ncols], in_=pb[:, 0:ncols]), writes=[pn(pb), "va%d" % slot])
            return ev

        def names4(n):
            return ["%s.%d" % (n, i) for i in range(4)]

        pt_i = [0]
        ps_i = [0]
        po_i = [0]

        def attn(qparts, kparts, vslot, scale, kts_fn, mask_fn, W_out, finalize, nk=128, bias=False, vsrc=None, vname=None):
            qreads = [n for (_, _, nm) in qparts for n in nm]
            kreads = [n for (_, _, nm) in kparts for n in nm]
            vname_ = vname or ("va%d" % vslot)
            for qt in range(NT):
                kts = kts_fn(qt)
                po = (pO0, pO1)[po_i[0] % 2]
                po_i[0] += 1
                qs = slice(qt * 128, (qt + 1) * 128)
                groups = [kts[i:i + 4] for i in range(0, len(kts), 4)]
                for gi, grp in enumerate(groups):
                    psb = (pS0, pS1)[ps_i[0] % 2]
                    ps_i[0] += 1
                    pts = pt_i[0] % 3
                    pt_i[0] += 1
                    mms = []
                    for j, kt in enumerate(grp):
                        o = psb[0:nk, j * 128:(j + 1) * 128]
                        np_ = len(qparts)
                        for pi in range(np_):
                            qa, K, _ = qparts[pi]
                            ka, _, _ = kparts[pi]
                            mms.append((o, ka[0:K, kt * 128:kt * 128 + nk], qa[0:K, qs], pi == 0, (pi == np_ - 1) and not bias))
                        if bias:
                            mms.append((o, emat[0:32, kt * 128:(kt + 1) * 128], nbT[0:32, qs], False, True))
                    P.op("pe", pe_group(mms), reads=qreads + kreads + (["msk", "nbT"] if bias else []), writes=[pn(psb)])
                    n = len(grp) * 128
                    ptn = "pt%d" % pts
                    P.op("act", lambda e, psb=psb, pts=pts, n=n: e.activation(out=PT[pts][0:nk, 0:n], in_=psb[0:nk, 0:n], func=AF.Exp, scale=scale),
                         writes=[pn(psb), ptn])
                    for (eng, fn, rd) in mask_fn(qt, grp, PT[pts]):
                        P.op(eng, fn, reads=rd, writes=[ptn])
                    mms = []
                    for j, kt in enumerate(grp):
                        vv = VA[0:nk, kt, vslot, 0:W_out] if vsrc is None else vsrc
                        mms.append((po[:, 0:W_out], PT[pts][0:nk, j * 128:(j + 1) * 128], vv,
                                    gi == 0 and j == 0, gi == len(groups) - 1 and j == len(grp) - 1))
                    P.op("pe", pe_group(mms), reads=[ptn, vname_], writes=[pn(po)])
                finalize(qt, po)

        def causal_mask(qt, grp, pt):
            ops = []
            if grp[-1] == qt:
                j = len(grp) - 1
                ops.append(("dve", lambda e, j=j, pt=pt: e.tensor_tensor(out=pt[:, j * 128:(j + 1) * 128], in0=pt[:, j * 128:(j + 1) * 128], in1=tri[:], op=ALU.mult), ["tri"]))
            return ops

        def fin_simple(colbase):
            def f(qt, po):
                of = ofin[qt % 2]
                ofn = "ofin%d" % (qt % 2)
                P.op("dve", lambda e: e.tensor_scalar(out=fin[:, 0:1], in0=po[:, 128:129], scalar1=1e-30, scalar2=None, op0=ALU.max), writes=[pn(po), "fin"])
                P.op("dve", lambda e: e.reciprocal(out=fin[:, 1:2], in_=fin[:, 0:1]), writes=["fin"])
                P.op("dve", lambda e: e.tensor_scalar(out=of[:], in0=po[:, 0:128], scalar1=fin[:, 1:2], scalar2=None, op0=ALU.mult),
                     reads=["fin"], writes=[pn(po), ofn])
                P.dma("sp", o_scr[qt * 128:(qt + 1) * 128, colbase:colbase + 128], of[:], reads=[ofn], writes=["o_scr"])
            return f

        def qlat_evac(c):
            def ev(pb, tc):
                sl = slice(tc * 512, (tc + 1) * 512)
                P.op("act", lambda e: e.activation(out=QLT[:, c, sl], in_=pb[:, :], func=AF.Copy), writes=[pn(pb), "qlt.%d" % tc])
            return ev
        for c in range(3):
            proj_fm(C_QLAT + 128 * c, 128, qlat_evac(c))
        proj_fm(C_KVLAT, 128, copy_evac(KVLT, "kvlt", 128))
        proj_fm(C_KROPE, 64, rope_evac(KR, "kr", 64, 64, cs64, "scr16", pm64, "pm64"))

        def rms_apply(views, nfeat, gcols, tnames_fn):
            for tc in range(4):
                sl = slice(tc * 512, (tc + 1) * 512)
                n = len(views)
                for c in range(n):
                    P.op("dve", lambda e, c=c: e.tensor_tensor(out=sqb[:, c, :], in0=views[c][:, sl], in1=views[c][:, sl], op=ALU.mult),
                         reads=[tnames_fn(tc)], writes=["sqb"])
                mms = [(pM1[:, :], onesb[:, :], sqb[:, c, :], c == 0, c == n - 1) for c in range(n)]
                P.op("pe", pe_group(mms), reads=["sqb", "onesb"], writes=[pn(pM1)])
                P.op("act", lambda e: e.activation(out=rstd[:], in_=pM1[:, :], func=AF.Ln, scale=1.0 / nfeat, bias=epsr[:, 0:1]),
                     reads=["epsr"], writes=[pn(pM1), "rstd"])
                P.op("act", lambda e: e.activation(out=rstd[:], in_=rstd[:], func=AF.Exp, scale=-0.5), writes=["rstd"])
                for c in range(n):
                    P.op("dve", lambda e, c=c: e.scalar_tensor_tensor(out=views[c][:, sl], in0=views[c][:, sl], scalar=qg[:, gcols[c]:gcols[c] + 1], in1=rstd[:], op0=ALU.mult, op1=ALU.mult),
                         reads=["rstd", "qg"], writes=[tnames_fn(tc)])
        rms_apply([QLT[:, 0, :], QLT[:, 1, :], QLT[:, 2, :]], 384.0, [0, 1, 2], lambda tc: "qlt.%d" % tc)
        rms_apply([KVLT], 128.0, [3], lambda tc: "kvlt.%d" % tc)

        sc_mla = 192.0 ** -0.5
        for h in range(4):
            qs_, ks_ = h % 2, h % 2
            for tc in range(4):
                sl = slice(tc * 512, (tc + 1) * 512)
                pb = next_pb()
                mms = [(pb[:, :], wqu[:, c, h * 192:h * 192 + 128], QLT[:, c, sl], c == 0, c == 2) for c in range(3)]
                P.op("pe", pe_group(mms), reads=["wqu", "qlt.%d" % tc], writes=[pn(pb)])
                copy_evac(QTS[qs_], "qts%d" % qs_, 128)(pb, tc)
                pb = next_pb()
                mms = [(pb[0:64, :], wqu[:, c, h * 192 + 128:h * 192 + 192], QLT[:, c, sl], c == 0, c == 2) for c in range(3)]
                P.op("pe", pe_group(mms), reads=["wqu", "qlt.%d" % tc], writes=[pn(pb)])
                rope_evac(QR, "qr", 64, 64, cs64, "scr16", pm64, "pm64")(pb, tc)
                pb = next_pb()
                P.op("pe", pe_group([(pb[:, :], wkvu[:, h * 256:h * 256 + 128], KVLT[:, sl], True, True)]), reads=["wkvu", "kvlt.%d" % tc], writes=[pn(pb)])
                copy_evac(KT[ks_], "kt%d" % ks_, 128)(pb, tc)
            for kt in range(NT):
                pb = next_pb()
                P.op("pe", pe_group([(pb[:, 0:128], KVLT[:, kt * 128:(kt + 1) * 128], wkvu[:, h * 256 + 128:h * 256 + 256], True, True)]),
                     reads=["wkvu"] + names4("kvlt"), writes=[pn(pb)])
                v_evac(h % 2)(pb, kt)
            attn([(QTS[qs_], 128, names4("qts%d" % qs_)), (QR, 64, names4("qr"))],
                 [(KT[ks_], 128, names4("kt%d" % ks_)), (KR, 64, names4("kr"))],
                 h % 2, sc_mla, lambda qt: list(range(qt + 1)), causal_mask, 129, fin_simple(h * 128))

        sc = 128.0 ** -0.5

        def dil_mask(qt, grp, pt):
            n = len(grp) * 128
            i0 = (15 - qt + grp[0]) * 128
            return [("dve", lambda e: e.tensor_tensor(out=pt[:, 0:n], in0=pt[:, 0:n], in1=mdil[:, i0:i0 + n], op=ALU.mult), ["msk"])]
        for h in range(6):
            s_ = h % 2
            proj_fm(C_DQ + 128 * h, 128, rope_evac(QTS[s_], "qts%d" % s_, 128, 32, cs32, "cs32", pm32, "pm32"))
            proj_fm(C_DK + 128 * h, 128, rope_evac(KT[s_], "kt%d" % s_, 128, 32, cs32, "cs32", pm32, "pm32"))
            proj_tm(C_DV + 128 * h, 128, v_evac(s_))
            if stage == "pdbg":
                P.barrier()
                P.dma("sp", dbg3_d[0], XT[:, 0, :], writes=["dbg3"])
                P.dma("sp", dbg3_d[1], QTS[0], writes=["dbg3"])
                P.dma("sp", dbg3_d[2], KT[0], writes=["dbg3"])
                P.dma("sp", dbg3_d[3].rearrange("p (a b) -> p a b", a=16), VA[:, :, 0, 0:128], writes=["dbg3"])
                P.dma("sp", dbg3_d[4], KVLT, writes=["dbg3"])
                P.dma("sp", dbg3_d[5], QLT[:, 0, :], writes=["dbg3"])
                P.dma("sp", dbg3_d[6, 0:64], KR, writes=["dbg3"])
                P.dma("sp", dbg3_d[7].rearrange("p (a b) -> p a b", a=16), VA[:, :, 0, 1:129], writes=["dbg3"])
                return "stop"
            attn([(QTS[s_], 128, names4("qts%d" % s_))], [(KT[s_], 128, names4("kt%d" % s_))], s_, sc,
                 lambda qt: list(range(qt + 1)), dil_mask, 129, fin_simple(512 + h * 128))

        def gate_evac(pb, kt):
            P.op("act", lambda e: e.activation(out=gates[:, kt, :], in_=pb[:, 0:18], func=AF.Sigmoid), writes=[pn(pb), "gates"])
        proj_tm(C_GATE, 18, gate_evac)

        w1 = SCR16[:, :].bitcast(BF16).rearrange("p (a b) -> p a b", a=32)
        for which in range(2):
            col = (C_NKC, C_NVC)[which]
            src = KT[which]
            proj_fm(col, 128, copy_evac(src, "kt%d" % which, 128))
            P.dma("sp", peT, W[("cmp_pos_k", "cmp_pos_v")[which]][l].rearrange("i d -> d i"), writes=["peT"], allow_slow_non_contiguous=True)
            P.dma("pool", w1, W[("cmp_w1_k", "cmp_w1_v")[which]][l].rearrange("(i d) j -> d i j", d=128), writes=["scr16"])
            P.dma("pool", w2, W[("cmp_w2_k", "cmp_w2_v")[which]][l].rearrange("(jc p) d -> p jc d", p=128), writes=["w2"])
            ovl = bass.AP(src.tensor, src.offset, [list(src.ap[0]), [1, 32], [16, 127]])
            P.op("dve", lambda e, ovl=ovl: e.tensor_tensor(out=XPE, in0=ovl, in1=peT.unsqueeze(2).broadcast_to([128, 32, 127]), op=ALU.add),
                 reads=names4("kt%d" % which) + ["peT"], writes=["xpe"])
            for jc in range(2):
                pb = next_pb()
                mms = [(pb[:, 0:127], w1[:, i, jc * 128:(jc + 1) * 128], XPE[:, i, :], i == 0, i == 31) for i in range(32)]
                P.op("pe", pe_group(mms), reads=["scr16", "xpe"], writes=[pn(pb)])
                P.op("act", lambda e, pb=pb: e.activation(out=hsc[0], in_=pb[:, 0:127], func=AF.Square), writes=[pn(pb), "hsc0"])
                P.op("dve", lambda e: e.tensor_scalar(out=hsc[0], in0=hsc[0], scalar1=0.044715, scalar2=1.0, op0=ALU.mult, op1=ALU.add), writes=["hsc0"])
                P.op("dve", lambda e, pb=pb: e.tensor_tensor(out=hsc[0], in0=hsc[0], in1=pb[:, 0:127], op=ALU.mult), writes=["hsc0", pn(pb)])
                P.op("act", lambda e: e.activation(out=hsc[1], in_=hsc[0], func=AF.Sigmoid, scale=1.5957691216057308), reads=["hsc0"], writes=["hsc1"])
                P.op("dve", lambda e, pb=pb, jc=jc: e.tensor_tensor(out=hT[:, jc, :], in0=hsc[1], in1=pb[:, 0:127], op=ALU.mult), reads=["hsc1"], writes=["hT", pn(pb)])
            if which == 0:
                pb = next_pb()
                mms = [(pb[:, 0:127], w2[:, jc, :], hT[:, jc, :], jc == 0, jc == 1) for jc in range(2)]
                P.op("pe", pe_group(mms), reads=["w2", "hT"], writes=[pn(pb)])
                rope_evac(KCT, "kct", 128, 32, cskc, "cskc", pm32, "pm32")(pb, 0, ncol=127, c0=0)
            else:
                pb = next_pb()
                mms = [(pb[0:127, 0:128], hT[:, jc, :], w2[:, jc, :], jc == 0, jc == 1) for jc in range(2)]
                P.op("pe", pe_group(mms), reads=["w2", "hT"], writes=[pn(pb)])
                P.op("dve", lambda e, pb=pb: e.tensor_copy(out=VCA[0:127, 0:128], in_=pb[0:127, 0:128]), writes=[pn(pb), "vca"])
                P.op("dve", lambda e: e.memset(VCA[0:127, 128:129], 1.0), writes=["vca"])
                P.op("dve", lambda e: e.tensor_copy(out=VCA[0:127, 129:161], in_=cover[0:127, :]), reads=["cover"], writes=["vca"])

        P.dma("pool", vcm, CD["vcm"], writes=["msk"])
        def cmp_mask(qt, grp, pt):
            return [("dve", lambda e: e.tensor_tensor(out=pt[0:127, 0:128], in0=pt[0:127, 0:128], in1=vcm[0:127, qt * 128:(qt + 1) * 128], op=ALU.mult), ["msk"])]
        for h in range(6):
            s_ = h % 2
            proj_fm(C_NQ + 128 * h, 128, rope_evac(QTS[s_], "qts%d" % s_, 128, 32, cs32, "cs32", pm32, "pm32"))
            P.dma("sp", nq_scr[h], QTS[s_], reads=names4("qts%d" % s_), writes=["nq_scr%d" % h])

            def fin_cmp(qt, po, h=h):
                oc = ocl[qt % 2]
                ocn = "ocl%d" % (qt % 2)
                P.op("dve", lambda e: e.tensor_scalar(out=fin[:, 0:1], in0=po[:, 128:129], scalar1=1e-30, scalar2=None, op0=ALU.max), writes=[pn(po), "fin"])
                P.op("dve", lambda e: e.reciprocal(out=fin[:, 1:2], in_=fin[:, 0:1]), writes=["fin"])
                P.op("dve", lambda e: e.scalar_tensor_tensor(out=imp[:, qt, :], in0=po[:, 129:161], scalar=fin[:, 1:2], in1=imp[:, qt, :], op0=ALU.mult, op1=ALU.add),
                     reads=["fin"], writes=[pn(po), "imp"])
                P.op("dve", lambda e: e.tensor_tensor(out=fin[:, 2:3], in0=fin[:, 1:2], in1=gates[:, qt, 3 * h:3 * h + 1], op=ALU.mult), reads=["gates"], writes=["fin"])
                P.op("dve", lambda e: e.tensor_scalar(out=oc[:], in0=po[:, 0:128], scalar1=fin[:, 2:3], scalar2=None, op0=ALU.mult), reads=["fin"], writes=[pn(po), ocn])
                P.dma("sp", ocmp_scr[h, qt * 128:(qt + 1) * 128, :], oc[:], reads=[ocn], writes=["ocmp_scr%d" % h])
            attn([(QTS[s_], 128, names4("qts%d" % s_))], [(KCT, 128, ["kct.0"])], 0, sc, lambda qt: [0], cmp_mask, 161, fin_cmp,
                 nk=127, vsrc=VCA[0:127, 0:161], vname="vca")

        for qt in range(NT):
            P.op("dve", lambda e, qt=qt: e.tensor_tensor(out=selr[:], in0=imp[:, qt, :], in1=selA[:, qt, :], op=ALU.mult), reads=["imp", "selA"], writes=["selr"])
            P.op("dve", lambda e, qt=qt: e.tensor_tensor(out=selr[:], in0=selr[:], in1=selB[:, qt, :], op=ALU.add), reads=["selB"], writes=["selr"])
            P.op("dve", lambda e: e.tensor_tensor(out=selw, in0=selr[:].unsqueeze(1).broadcast_to([128, 32, 32]), in1=selr[:].unsqueeze(2).broadcast_to([128, 32, 32]), op=ALU.is_gt),
                 reads=["selr"], writes=["selw", "rt1", "rt2"])
            P.op("dve", lambda e: e.tensor_reduce(out=selr[:], in_=selw, axis=AX.X, op=ALU.add), reads=["selw"], writes=["selr"])
            P.op("dve", lambda e: e.tensor_scalar(out=selr[:], in0=selr[:], scalar1=15.5, scalar2=-30000.0, op0=ALU.is_gt, op1=ALU.mult), writes=["selr"])
            P.op("pe", lambda e: e.transpose(out=pM1[0:32, 0:128], in_=selr[:], identity=ident[:]), reads=["selr", "ident"], writes=[pn(pM1)])
            P.op("act", lambda e, qt=qt: e.activation(out=nbT[0:32, qt * 128:(qt + 1) * 128], in_=pM1[0:32, 0:128], func=AF.Copy), writes=[pn(pM1), "nbT"])

        P.dma("pool", emat, CD["emat"], writes=["msk"])
        proj_fm(C_NKS, 128, rope_evac(KT[0], "kt0", 128, 32, cs32, "cs32", pm32, "pm32"))
        proj_fm(C_NKW, 128, rope_evac(KT[1], "kt1", 128, 32, cs32, "cs32", pm32, "pm32"))
        proj_tm(C_NVS, 128, v_evac(0))
        proj_tm(C_NVW, 128, v_evac(1))

        def win_mask(qt, grp, pt):
            ops = []
            for j, kt in enumerate(grp):
                if kt == qt:
                    ops.append(("dve", lambda e, j=j: e.tensor_tensor(out=pt[:, j * 128:(j + 1) * 128], in0=pt[:, j * 128:(j + 1) * 128], in1=tri[:], op=ALU.mult), ["tri"]))
                elif kt == qt - 4:
                    ops.append(("dve", lambda e, j=j: e.tensor_tensor(out=pt[:, j * 128:(j + 1) * 128], in0=pt[:, j * 128:(j + 1) * 128], in1=upp[:], op=ALU.mult), ["upp"]))
            return ops
        for h in range(6):
            s_ = h % 2
            P.dma("sp", QTS[s_], nq_scr[h], reads=["nq_scr%d" % h], writes=names4("qts%d" % s_))

            def fin_slc(qt, po, h=h):
                P.op("dve", lambda e: e.tensor_scalar(out=fin[:, 0:1], in0=po[:, 128:129], scalar1=1e-30, scalar2=None, op0=ALU.max), writes=[pn(po), "fin"])
                P.op("dve", lambda e: e.reciprocal(out=fin[:, 1:2], in_=fin[:, 0:1]), writes=["fin"])
                P.op("dve", lambda e: e.tensor_tensor(out=fin[:, 2:3], in0=fin[:, 1:2], in1=gates[:, qt, 3 * h + 1:3 * h + 2], op=ALU.mult), reads=["gates"], writes=["fin"])
                P.op("dve", lambda e: e.tensor_scalar(out=onsa[:, qt, :], in0=po[:, 0:128], scalar1=fin[:, 2:3], scalar2=None, op0=ALU.mult), reads=["fin"], writes=[pn(po), "onsa"])
            attn([(QTS[s_], 128, names4("qts%d" % s_))], [(KT[0], 128, names4("kt0"))], 0, sc, lambda qt: list(range(qt + 1)), causal_mask, 129, fin_slc, bias=True)

            def fin_win(qt, po, h=h):
                oc = ocl[qt % 2]
                ocn = "ocl%d" % (qt % 2)
                of = ofin[qt % 2]
                ofn = "ofin%d" % (qt % 2)
                P.dma("sp", oc[:], ocmp_scr[h, qt * 128:(qt + 1) * 128, :], reads=["ocmp_scr%d" % h], writes=[ocn])
                P.op("dve", lambda e: e.tensor_scalar(out=fin[:, 0:1], in0=po[:, 128:129], scalar1=1e-30, scalar2=None, op0=ALU.max), writes=[pn(po), "fin"])
                P.op("dve", lambda e: e.reciprocal(out=fin[:, 1:2], in_=fin[:, 0:1]), writes=["fin"])
                P.op("dve", lambda e: e.tensor_tensor(out=fin[:, 2:3], in0=fin[:, 1:2], in1=gates[:, qt, 3 * h + 2:3 * h + 3], op=ALU.mult), reads=["gates"], writes=["fin"])
                P.op("dve", lambda e: e.scalar_tensor_tensor(out=onsa[:, qt, :], in0=po[:, 0:128], scalar=fin[:, 2:3], in1=onsa[:, qt, :], op0=ALU.mult, op1=ALU.add),
                     reads=["fin"], writes=[pn(po), "onsa"])
                P.op("dve", lambda e: e.tensor_tensor(out=of[:], in0=onsa[:, qt, :], in1=oc[:], op=ALU.add), reads=["onsa", ocn], writes=[ofn])
                P.dma("sp", o_scr[qt * 128:(qt + 1) * 128, 1280 + h * 128:1280 + (h + 1) * 128], of[:], reads=[ofn], writes=["o_scr"])
            attn([(QTS[s_], 128, names4("qts%d" % s_))], [(KT[1], 128, names4("kt1"))], 1, sc, lambda qt: list(range(max(0, qt - 4), qt + 1)), win_mask, 129, fin_win)

        phase()
        WO = XT
        P.dma("pool", WO[:, :, :], W["w_out"][l].rearrange("(kc p) n -> p kc n", p=128), writes=["WO"])
        lng = carve([128, D], F32)
        lnb = carve([128, D], F32)
        P.dma("sp", lng, W["ln1_g"][l:l + 1, :].broadcast_to([128, D]), writes=["lng"])
        P.dma("sp", lnb, W["ln1_b"][l:l + 1, :].broadcast_to([128, D]), writes=["lnb"])
        otl = [carve([128, D], BF16) for _ in range(2)]
        oT = [carve([128, 16, 128], BF16) for _ in range(2)]
        xtl = [carve([128, D], F32) for _ in range(2)]
        ytl = [carve([128, D], F32) for _ in range(2)]
        x1b = [carve([128, D], BF16) for _ in range(2)]
        x1T = carve([128, 16, 128], F32)
        wr = carve([128, 16, 36], F32)
        st = carve([128, 16], F32)
        rl = carve([128, 64], F32)
        P.dma("sp", wr[:, :, 0:4], W["w_grp"][l].rearrange("(kc p) n -> p kc n", p=128), writes=["wr"])
        P.dma("sp", wr[:, :, 4:36], W["w_exp"][l].rearrange("(kc p) n -> p kc n", p=128), writes=["wr"])
        psT = [pS0[:, :].bitcast(BF16), pS1[:, :].bitcast(BF16)]

        def layer_norm(y, yn, g, gname, b, bname, out, outn):
            P.op("dve", lambda e: e.tensor_reduce(out=st[:, 0:1], in_=y, axis=AX.X, op=ALU.add), reads=[yn], writes=["st"])
            P.op("dve", lambda e: e.tensor_scalar(out=st[:, 1:2], in0=st[:, 0:1], scalar1=-1.0 / D, scalar2=None, op0=ALU.mult), writes=["st"])
            P.op("act", lambda e: e.activation(out=y, in_=y, func=AF.Identity, bias=st[:, 1:2], scale=1.0), reads=["st"], writes=[yn])
            P.op("dve", lambda e: e.memset(st[:, 2:3], 0.0), writes=["st"])
            P.op("act", lambda e: e.activation(out=out, in_=y, func=AF.Square, accum_out=st[:, 2:3]), reads=[yn], writes=[outn, "st"])
            P.op("act", lambda e: e.activation(out=st[:, 3:4], in_=st[:, 2:3], func=AF.Ln, scale=1.0 / D, bias=epsr[:, 1:2]), reads=["epsr"], writes=["st"])
            P.op("act", lambda e: e.activation(out=st[:, 3:4], in_=st[:, 3:4], func=AF.Exp, scale=-0.5), writes=["st"])
            P.op("dve", lambda e: e.scalar_tensor_tensor(out=out, in0=y, scalar=st[:, 3:4], in1=g, op0=ALU.mult, op1=ALU.mult), reads=[yn, "st", gname], writes=[outn])
            P.op("pool", lambda e: e.tensor_tensor(out=out, in0=out, in1=b, op=ALU.add), reads=[bname], writes=[outn])

        for t in range(NT):
            i2 = t % 2
            rows = slice(t * 128, (t + 1) * 128)
            P.dma("sp", otl[i2], o_scr[rows, :], reads=["o_scr"], writes=["otl%d" % i2])
            P.dma("sp", xtl[i2], x_src[rows, :], writes=["xtl%d" % i2])
            if stage == "odbg":
                P.op("pool", lambda e, i2=i2: e.tensor_copy(out=ytl[i2], in_=otl[i2]), reads=["otl%d" % i2], writes=["ytl%d" % i2])
                P.dma("sp", dbg2_d[rows, :], ytl[i2], reads=["ytl%d" % i2], writes=["dbg2"])
            for g in range(2):
                pt_ = psT[g]
                pnm = pn((pS0, pS1)[g])
                P.op("pe", (lambda pt_=pt_, g=g, i2=i2: lambda e: [e.transpose(out=pt_[:, j * 128:(j + 1) * 128], in_=otl[i2][:, (g * 8 + j) * 128:(g * 8 + j + 1) * 128], identity=identb[:]) for j in range(8)][-1])(),
                     reads=["otl%d" % i2, "identb"], writes=[pnm])
                P.op("act", lambda e, pt_=pt_, g=g, i2=i2: e.activation(out=oT[i2][:, g * 8:(g + 1) * 8, :], in_=pt_[:, 0:1024].rearrange("p (a b) -> p a b", a=8), func=AF.Copy),
                     writes=[pnm, "oT%d" % i2])
            for cc in range(4):
                pb = next_pb()
                mms = [(pb[:, :], oT[i2][:, kc, :], WO[:, kc, cc * 512:(cc + 1) * 512], kc == 0, kc == 15) for kc in range(16)]
                P.op("pe", pe_group(mms), reads=["oT%d" % i2, "WO"], writes=[pn(pb)])
                P.op("dve", lambda e, pb=pb, cc=cc, i2=i2: e.scalar_tensor_tensor(out=ytl[i2][:, cc * 512:(cc + 1) * 512], in0=xtl[i2][:, cc * 512:(cc + 1) * 512], scalar=ALPHA, in1=pb[:, :], op0=ALU.mult, op1=ALU.add),
                     reads=["xtl%d" % i2], writes=[pn(pb), "ytl%d" % i2])
            layer_norm(ytl[i2], "ytl%d" % i2, lng, "lng", lnb, "lnb", xtl[i2], "xtl%d" % i2)
            P.dma("sp", x1_scr[rows, :], xtl[i2], reads=["xtl%d" % i2], writes=["x1_scr"])
            if stage != "full" and l == 0:
                P.dma("sp", dbg_d[rows, :], xtl[i2], reads=["xtl%d" % i2], writes=["dbg"])
            P.op("pool", lambda e, i2=i2: e.tensor_copy(out=x1b[i2], in_=xtl[i2]), reads=["xtl%d" % i2], writes=["x1b%d" % i2])
            P.dma("sp", x1b_scr[rows, :], x1b[i2], reads=["x1b%d" % i2], writes=["x1b_scr"])
            for g in range(4):
                pm_ = (pM0, pM1)[g % 2]
                P.op("pe", (lambda pm_=pm_, g=g, i2=i2: lambda e: [e.transpose(out=pm_[:, j * 128:(j + 1) * 128], in_=xtl[i2][:, (g * 4 + j) * 128:(g * 4 + j + 1) * 128], identity=ident[:]) for j in range(4)][-1])(),
                     reads=["xtl%d" % i2, "ident"], writes=[pn(pm_)])
                P.op("act", lambda e, pm_=pm_, g=g: e.activation(out=x1T[:, g * 4:(g + 1) * 4, :], in_=pm_[:, :].rearrange("p (a b) -> p a b", a=4), func=AF.Copy),
                     writes=[pn(pm_), "x1T"])
            mms = [(pO0[:, 0:36], x1T[:, kc, :], wr[:, kc, :], kc == 0, kc == 15) for kc in range(16)]
            P.op("pe", pe_group(mms), reads=["x1T", "wr"], writes=[pn(pO0)])
            router(t, pO0, rl)

    def router(t, pl, rl):
        V = lambda f, reads=(), writes=("rl",): P.op("dve", f, reads=reads, writes=writes)
        lg = rl[:, 0:36]
        V(lambda e: e.tensor_copy(out=lg, in_=pl[:, 0:36]), writes=["rl", pn(pl)])
        V(lambda e: e.tensor_reduce(out=rl[:, 36:37], in_=rl[:, 0:4], axis=AX.X, op=ALU.max))
        V(lambda e: e.tensor_scalar(out=rl[:, 40:44], in0=rl[:, 0:4], scalar1=rl[:, 36:37], scalar2=None, op0=ALU.subtract))
        V(lambda e: e.memset(rl[:, 37:38], 0.0))
        P.op("act", lambda e: e.activation(out=rl[:, 44:48], in_=rl[:, 40:44], func=AF.Exp, accum_out=rl[:, 37:38]), writes=["rl"])
        V(lambda e: e.reciprocal(out=rl[:, 38:39], in_=rl[:, 37:38]))
        V(lambda e: e.tensor_scalar(out=rl[:, 40:44], in0=rl[:, 40:44], scalar1=0.0, scalar2=1e30, op0=ALU.is_lt, op1=ALU.mult))
        em = rl[:, 4:36].rearrange("p (g k) -> p g k", g=4)
        V(lambda e: e.tensor_tensor(out=em, in0=em, in1=rl[:, 40:44].unsqueeze(2).broadcast_to([128, 4, 8]), op=ALU.subtract))
        V(lambda e: e.tensor_reduce(out=rl[:, 48:49], in_=rl[:, 4:36], axis=AX.X, op=ALU.max))
        V(lambda e: e.tensor_scalar(out=oh[:, t, 0, :], in0=rl[:, 4:36], scalar1=rl[:, 48:49], scalar2=None, op0=ALU.is_equal), writes=["rl", "oh"])
        V(lambda e: e.scalar_tensor_tensor(out=rl[:, 4:36], in0=oh[:, t, 0, :], scalar=-1e30, in1=rl[:, 4:36], op0=ALU.mult, op1=ALU.add), reads=["oh"])
        V(lambda e: e.tensor_reduce(out=rl[:, 49:50], in_=rl[:, 4:36], axis=AX.X, op=ALU.max))
        V(lambda e: e.tensor_scalar(out=oh[:, t, 1, :], in0=rl[:, 4:36], scalar1=rl[:, 49:50], scalar2=None, op0=ALU.is_equal), writes=["rl", "oh"])
        V(lambda e: e.tensor_tensor(out=mk[:, t, :], in0=oh[:, t, 0, :], in1=oh[:, t, 1, :], op=ALU.add), reads=["oh"], writes=["mk"])
        V(lambda e: e.tensor_tensor(out=rl[:, 50:51], in0=rl[:, 49:50], in1=rl[:, 48:49], op=ALU.subtract))
        P.op("act", lambda e: e.activation(out=rl[:, 51:52], in_=rl[:, 50:51], func=AF.Exp), writes=["rl"])
        V(lambda e: e.tensor_scalar(out=rl[:, 51:52], in0=rl[:, 51:52], scalar1=1.0, scalar2=None, op0=ALU.add))
        V(lambda e: e.reciprocal(out=rl[:, 52:53], in_=rl[:, 51:52]))
        V(lambda e: e.tensor_tensor(out=gatew[:, t, 0:1], in0=rl[:, 52:53], in1=rl[:, 38:39], op=ALU.mult), writes=["rl", "gatew"])
        V(lambda e: e.tensor_tensor(out=gatew[:, t, 1:2], in0=rl[:, 38:39], in1=gatew[:, t, 0:1], op=ALU.subtract), writes=["rl", "gatew"])

    def moe(l, last):
        phase()
        pos = carve([128, 32], F32)
        tmp = carve([128, 32], F32)
        sf = carve([128, 4], F32)
        xbt = [carve([128, D], BF16) for _ in range(2)]
        for t in range(NT):
            mms = [(pM0[:, 0:32], onesb[:, :], mk[:, tp, :], tp == 0, False) for tp in range(t)]
            mms.append((pM0[:, 0:32], ltri[:, :], mk[:, t, :], t == 0, True))
            P.op("pe", pe_group(mms), reads=["mk", "onesb", "ltri"], writes=[pn(pM0)])
            P.op("dve", lambda e: e.scalar_tensor_tensor(out=pos, in0=pM0[:, 0:32], scalar=float(CAP - 1), in1=ecap[:], op0=ALU.min, op1=ALU.add), reads=["ecap"], writes=[pn(pM0), "pos"])
            for k in range(2):
                P.op("dve", lambda e, k=k, t=t: e.tensor_tensor(out=tmp, in0=pos, in1=oh[:, t, k, :], op=ALU.mult), reads=["pos", "oh"], writes=["tmp"])
                P.op("dve", lambda e, k=k: e.tensor_reduce(out=sf[:, k:k + 1], in_=tmp, axis=AX.X, op=ALU.add), reads=["tmp"], writes=["sf"])
            P.op("dve", lambda e, t=t: e.tensor_copy(out=slots[:, t, :], in_=sf[:, 0:2]), reads=["sf"], writes=["slots"])
            i2 = t % 2
            P.dma("sp", xbt[i2], x1b_scr[t * 128:(t + 1) * 128, :], reads=["x1b_scr"], writes=["xbt%d" % i2])
            for k in range(2):
                P.idma(lambda g, t=t, k=k, i2=i2: g.indirect_dma_start(out=xg_scr[:, :], out_offset=bass.IndirectOffsetOnAxis(ap=slots[:, t, k:k + 1], axis=0),
                                                                      in_=xbt[i2], in_offset=None),
                       reads=["xbt%d" % i2, "slots"], writes=["xg_scr"])
        phase()
        wbuf = []
        e0 = XT[:, :, :].rearrange("p a b -> p (a b)")
        wbuf.append((e0[:, 0:8192].rearrange("p (a b) -> p a b", a=16), e0[:, 8192:16384].rearrange("p (a b) -> p a b", a=16),
                     e0[:, 16384:24576].rearrange("p (a b) -> p a b", a=4)))
        wbuf.append((carve([128, 16, 512], BF16), carve([128, 16, 512], BF16), carve([128, 4, D], BF16)))
        xg = [carve([128, D], BF16) for _ in range(2)]
        xgT = carve([128, 16, CAP], BF16)
        hTm = carve([128, 4, CAP], BF16)
        sg = carve([128, CAP], F32)
        ysb = [carve([128, D], F32) for _ in range(2)]
        psT = [pS0[:, :].bitcast(BF16), pS1[:, :].bitcast(BF16)]
        for ex in range(NEXP):
            wg, wu, wd = wbuf[ex % 2]
            wn = "wexp%d" % (ex % 2)
            P.dma("pool", wg, W["w_gate"][l, ex].rearrange("(kc p) f -> p kc f", p=128), writes=[wn])
            P.dma("pool", wu, W["w_up"][l, ex].rearrange("(kc p) f -> p kc f", p=128), writes=[wn])
            P.dma("pool", wd, W["w_down"][l, ex].rearrange("(kc p) f -> p kc f", p=128), writes=[wn])
            for s_ in range(CAP // 128):
                r0 = ex * CAP + s_ * 128
                P.dma("sp", xg[s_], xg_scr[r0:r0 + 128, :], reads=["xg_scr"], writes=["xg%d" % s_])
                for g in range(2):
                    pt_ = psT[g]
                    pnm = pn((pS0, pS1)[g])
                    P.op("pe", (lambda pt_=pt_, g=g, s_=s_: lambda e: [e.transpose(out=pt_[:, j * 128:(j + 1) * 128], in_=xg[s_][:, (g * 8 + j) * 128:(g * 8 + j + 1) * 128], identity=identb[:]) for j in range(8)][-1])(),
                         reads=["xg%d" % s_, "identb"], writes=[pnm])
                    P.op("act", lambda e, pt_=pt_, g=g, s_=s_: e.activation(out=xgT[:, g * 8:(g + 1) * 8, s_ * 128:(s_ + 1) * 128], in_=pt_[:, 0:1024].rearrange("p (a b) -> p a b", a=8), func=AF.Copy),
                         writes=[pnm, "xgT"])
            for fc in range(4):
                mms = [(pA[:, 0:CAP], wg[:, kc, fc * 128:(fc + 1) * 128], xgT[:, kc, :], kc == 0, kc == 15) for kc in range(16)]
                P.op("pe", pe_group(mms), reads=[wn, "xgT"], writes=[pn(pA)])
                mms = [(pB[:, 0:CAP], wu[:, kc, fc * 128:(fc + 1) * 128], xgT[:, kc, :], kc == 0, kc == 15) for kc in range(16)]
                P.op("pe", pe_group(mms), reads=[wn, "xgT"], writes=[pn(pB)])
                P.op("act", lambda e: e.activation(out=sg, in_=pA[:, 0:CAP], func=AF.Silu), writes=[pn(pA), "sg"])
                P.op("dve", lambda e, fc=fc: e.tensor_tensor(out=hTm[:, fc, :], in0=sg, in1=pB[:, 0:CAP], op=ALU.mult), reads=["sg"], writes=[pn(pB), "hTm"])
            for s_ in range(CAP // 128):
                for cc in range(4):
                    pb = (pO0, pO1, pM0, pM1)[cc]
                    mms = [(pb[:, :], hTm[:, kc, s_ * 128:(s_ + 1) * 128], wd[:, kc, cc * 512:(cc + 1) * 512], kc == 0, kc == 3) for kc in range(4)]
                    P.op("pe", pe_group(mms), reads=[wn, "hTm"], writes=[pn(pb)])
                    if cc % 2 == 0:
                        P.op("act", lambda e, pb=pb, cc=cc, s_=s_: e.activation(out=ysb[s_][:, cc * 512:(cc + 1) * 512], in_=pb[:, :], func=AF.Copy), writes=[pn(pb), "ysb%d" % s_])
                    else:
                        P.op("dve", lambda e, pb=pb, cc=cc, s_=s_: e.tensor_copy(out=ysb[s_][:, cc * 512:(cc + 1) * 512], in_=pb[:, :]), writes=[pn(pb), "ysb%d" % s_])
                r0 = ex * CAP + s_ * 128
                P.dma("sp", y_scr[r0:r0 + 128, :], ysb[s_], reads=["ysb%d" % s_], writes=["y_scr"])
        phase()
        lng = carve([128, D], F32)
        lnb = carve([128, D], F32)
        P.dma("sp", lng, W["ln2_g"][l:l + 1, :].broadcast_to([128, D]), writes=["lng"])
        P.dma("sp", lnb, W["ln2_b"][l:l + 1, :].broadcast_to([128, D]), writes=["lnb"])
        y0 = [carve([128, D], F32) for _ in range(2)]
        y1 = [carve([128, D], F32) for _ in range(2)]
        x1t = [carve([128, D], F32) for _ in range(2)]
        st = carve([128, 16], F32)
        dst = out_d if last else xres_scr
        for t in range(NT):
            i2 = t % 2
            rows = slice(t * 128, (t + 1) * 128)
            P.dma("sp", x1t[i2], x1_scr[rows, :], reads=["x1_scr"], writes=["x1t%d" % i2])
            P.idma(lambda g, t=t, i2=i2: g.indirect_dma_start(out=y0[i2], out_offset=None, in_=y_scr[:, :], in_offset=bass.IndirectOffsetOnAxis(ap=slots[:, t, 0:1], axis=0)), reads=["y_scr", "slots"], writes=["y0%d" % i2])
            P.idma(lambda g, t=t, i2=i2: g.indirect_dma_start(out=y1[i2], out_offset=None, in_=y_scr[:, :], in_offset=bass.IndirectOffsetOnAxis(ap=slots[:, t, 1:2], axis=0)), reads=["y_scr", "slots"], writes=["y1%d" % i2])
            P.op("dve", lambda e, t=t, i2=i2: e.tensor_scalar(out=y0[i2], in0=y0[i2], scalar1=gatew[:, t, 0:1], scalar2=None, op0=ALU.mult), reads=["gatew"], writes=["y0%d" % i2])
            P.op("dve", lambda e, t=t, i2=i2: e.scalar_tensor_tensor(out=y0[i2], in0=y1[i2], scalar=gatew[:, t, 1:2], in1=y0[i2], op0=ALU.mult, op1=ALU.add), reads=["gatew", "y1%d" % i2], writes=["y0%d" % i2])
            if stage != "full" and l == 0:
                P.dma("sp", dbg2_d[rows, :], y0[i2], reads=["y0%d" % i2], writes=["dbg2"])
            P.op("dve", lambda e, i2=i2: e.scalar_tensor_tensor(out=y0[i2], in0=x1t[i2], scalar=ALPHA, in1=y0[i2], op0=ALU.mult, op1=ALU.add), reads=["x1t%d" % i2], writes=["y0%d" % i2])
            y, yn, out, outn = y0[i2], "y0%d" % i2, x1t[i2], "x1t%d" % i2
            P.op("dve", lambda e, y=y: e.tensor_reduce(out=st[:, 0:1], in_=y, axis=AX.X, op=ALU.add), reads=[yn], writes=["st"])
            P.op("dve", lambda e: e.tensor_scalar(out=st[:, 1:2], in0=st[:, 0:1], scalar1=-1.0 / D, scalar2=None, op0=ALU.mult), writes=["st"])
            P.op("act", lambda e, y=y: e.activation(out=y, in_=y, func=AF.Identity, bias=st[:, 1:2], scale=1.0), reads=["st"], writes=[yn])
            P.op("dve", lambda e: e.memset(st[:, 2:3], 0.0), writes=["st"])
            P.op("act", lambda e, y=y, out=out: e.activation(out=out, in_=y, func=AF.Square, accum_out=st[:, 2:3]), reads=[yn], writes=[outn, "st"])
            P.op("act", lambda e: e.activation(out=st[:, 3:4], in_=st[:, 2:3], func=AF.Ln, scale=1.0 / D, bias=epsr[:, 1:2]), reads=["epsr"], writes=["st"])
            P.op("act", lambda e: e.activation(out=st[:, 3:4], in_=st[:, 3:4], func=AF.Exp, scale=-0.5), writes=["st"])
            P.op("dve", lambda e, y=y, out=out: e.scalar_tensor_tensor(out=out, in0=y, scalar=st[:, 3:4], in1=lng, op0=ALU.mult, op1=ALU.mult), reads=[yn, "st", "lng"], writes=[outn])
            P.op("pool", lambda e, out=out: e.tensor_tensor(out=out, in0=out, in1=lnb, op=ALU.add), reads=["lnb"], writes=[outn])
            P.dma("sp", dst[rows, :], out, reads=[outn], writes=["dst"])

    x_src = x_d
    for l in range(n_layers):
        last = (l == n_layers - 1)
        if layer(l, x_src, last) == "stop":
            break
        if stage in ("ln1", "odbg"):
            break
        moe(l, last)
        x_src = xres_scr
    P.barrier()
    P.emit()
    return nc


_CONSTS = None


def kernel(**inputs):
    global _CONSTS
    if _CONSTS is None:
        _CONSTS = make_consts()
    nc = build()
    x = np.ascontiguousarray(inputs["x"], dtype=np.float32)
    shared = {k: np.ascontiguousarray(inputs[k], dtype=np.float32) for k in W_SHAPES}
    for k, v in _CONSTS.items():
        shared["c_" + k] = np.ascontiguousarray(v.reshape(CONST_SHAPES[k]), dtype=np.float32)
    in_maps = []
    for b in range(4):
        m = dict(shared)
        m["x"] = x[b]
        in_maps.append(m)
    res = run_bass_kernel_spmd(nc, in_maps, core_ids=list(range(4)))
    return np.stack([np.asarray(r["out"], dtype=np.float32) for r in res.results], axis=0)
```

```python
import contextlib
import numpy as np
import concourse.bass as bass
import concourse.mybir as mybir
from concourse.bass_utils import run_bass_kernel_spmd

F32 = mybir.dt.float32
BF16 = mybir.dt.bfloat16
I32 = mybir.dt.int32
ALU = mybir.AluOpType
AF = mybir.ActivationFunctionType
AX = mybir.AxisListType

S = 2048
D = 2048
NT = 16
DEPTH = 2
IN_COLS = 4434
CAP = 256
NEXP = 32
THETA = 500000.0
ALPHA = (2 * DEPTH) ** 0.25
LN_EPS = 1e-5
RMS_EPS = 1e-6
C_QLAT, C_KVLAT, C_KROPE = 0, 384, 512
C_DQ, C_DK, C_DV = 576, 1344, 2112
C_NQ = 2880
C_NKC, C_NVC, C_NKS, C_NVS, C_NKW, C_NVW = 3648, 3776, 3904, 4032, 4160, 4288
C_GATE = 4416


class Prog:
    CENG = ("pe", "act", "dve", "pool")
    DMAQ = ("sp", "act", "pool")
    RING = 8

    def __init__(self, nc):
        self.nc = nc
        self.es = contextlib.ExitStack()
        self.streams = {e: [] for e in ("pe", "act", "dve", "pool", "sp")}
        self.cnt = {e: 0 for e in self.CENG}
        self.sems = {}
        for e in self.CENG:
            self.sems[e] = self.es.enter_context(nc.semaphore("s_" + e))
        self.dsems = {}
        self.dcnt = {}
        for q in self.DMAQ:
            self.dcnt[q] = 0
            for i in range(self.RING):
                self.dsems[(q, i)] = self.es.enter_context(nc.semaphore("d_%s%d" % (q, i)))
        self.seen = {s: {} for s in self.streams}
        self.tiles = {}

    def sb(self, name, shape, dt):
        return self.es.enter_context(self.nc.sbuf_tensor(name, list(shape), dt))

    def ps(self, name, shape, dt=F32):
        return self.es.enter_context(self.nc.psum_tensor(name, list(shape), dt))

    def _need(self, stream, ev, waits, is_dma=False):
        if ev is None:
            return
        key, val = ev
        if key == stream and key == "pe" and not is_dma:
            return
        if self.seen[stream].get(key, 0) >= val:
            return
        if waits.get(key, 0) < val:
            waits[key] = val

    def _deps(self, stream, reads, writes, is_dma=False):
        waits = {}
        for t in reads:
            st = self.tiles.setdefault(t, {"w": None, "r": []})
            self._need(stream, st["w"], waits, is_dma)
        for t in writes:
            st = self.tiles.setdefault(t, {"w": None, "r": []})
            self._need(stream, st["w"], waits, is_dma)
            for ev in st["r"]:
                self._need(stream, ev, waits, is_dma)
        return waits

    def _commit(self, ev, reads, writes):
        for t in reads:
            if t in writes:
                continue
            r = self.tiles[t]["r"]
            r.append(ev)
            if len(r) > 48:
                best = {}
                for k, v in r:
                    if best.get(k, 0) < v:
                        best[k] = v
                self.tiles[t]["r"] = list(best.items())
        for t in writes:
            self.tiles[t]["w"] = ev
            self.tiles[t]["r"] = []

    def _sem(self, key):
        return self.sems[key] if key in self.sems else self.dsems[key]

    def _emit_waits(self, stream, waits):
        for key, val in waits.items():
            self.streams[stream].append(("w", self._sem(key), val))
            self.seen[stream][key] = val

    def op(self, eng, fn, reads=(), writes=()):
        reads, writes = tuple(reads), tuple(writes)
        self._emit_waits(eng, self._deps(eng, reads, writes))
        self.cnt[eng] += 1
        ev = (eng, self.cnt[eng])
        self.streams[eng].append(("c", fn, self.sems[eng]))
        self._commit(ev, reads, writes)
        return ev

    def _dma_common(self, q, reads, writes):
        waits = self._deps(q, reads, writes, True)
        k = self.dcnt[q]
        self.dcnt[q] += 1
        key = (q, k % self.RING)
        tgt = 16 * (k // self.RING + 1)
        if tgt > 16:
            prev = tgt - 16
            if self.seen[q].get(key, 0) < prev and waits.get(key, 0) < prev:
                waits[key] = prev
        self._emit_waits(q, waits)
        return key, tgt

    def dma(self, q, out, in_, reads=(), writes=(), **kw):
        reads, writes = tuple(reads), tuple(writes)
        key, tgt = self._dma_common(q, reads, writes)
        self.streams[q].append(("d", out, in_, kw, self.dsems[key]))
        self._commit((key, tgt), reads, writes)

    def idma(self, fn, reads=(), writes=()):
        reads, writes = tuple(reads), tuple(writes)
        key, tgt = self._dma_common("pool", reads, writes)
        self.streams["pool"].append(("i", fn, self.dsems[key]))
        self._commit((key, tgt), reads, writes)

    def barrier(self):
        cur = {}
        for e in self.CENG:
            if self.cnt[e] > 0:
                cur[e] = self.cnt[e]
        for q in self.DMAQ:
            k = self.dcnt[q]
            for i in range(self.RING):
                n = (k - i + self.RING - 1) // self.RING if k > i else 0
                if n > 0:
                    cur[(q, i)] = 16 * n
        for s in self.streams:
            waits = {}
            for key, val in cur.items():
                if self.seen[s].get(key, 0) < val:
                    waits[key] = val
            self._emit_waits(s, waits)
        self.tiles = {}

    def emit(self):
        nc = self.nc
        streams = self.streams

        def run(e, lst):
            for it in lst:
                k = it[0]
                if k == "w":
                    e.wait_ge(it[1], it[2])
                elif k == "c":
                    it[1](e).then_inc(it[2], 1)
                elif k == "d":
                    e.dma_start(out=it[1], in_=it[2], **it[3]).then_inc(it[4], 16)
                elif k == "i":
                    it[1](e).then_inc(it[2], 16)

        with nc.Block() as block:
            @block.sync
            def _(e):
                run(e, streams["sp"])

            @block.tensor
            def _(e):
                run(e, streams["pe"])

            @block.scalar
            def _(e):
                run(e, streams["act"])

            @block.vector
            def _(e):
                run(e, streams["dve"])

            @block.gpsimd
            def _(e):
                run(e, streams["pool"])
        self.es.close()


def make_consts():
    c = {}
    c["ident"] = np.eye(128, dtype=np.float32)
    pos = np.arange(S, dtype=np.float32)

    def rope_tab(rot, p):
        half = rot // 2
        inv = (np.float32(THETA) ** (-np.arange(half, dtype=np.float32) / np.float32(half))).astype(np.float32)
        ang = (p.astype(np.float32)[None, :] * inv[:, None]).astype(np.float32)
        cos = np.cos(ang.astype(np.float64)).astype(np.float32)
        sin = np.sin(ang.astype(np.float64)).astype(np.float32)
        t = np.zeros((rot, 2, p.shape[0]), np.float32)
        t[:half, 0], t[half:, 0] = cos, cos
        t[:half, 1], t[half:, 1] = -sin, sin
        return t

    c["cs32"] = rope_tab(32, pos)
    c["cs64"] = rope_tab(64, pos)
    kc = np.zeros((32, 2, 128), np.float32)
    kc[:, :, :127] = rope_tab(32, (np.arange(127) * 16 + 31).astype(np.float32))
    c["cskc"] = kc

    def perm(n):
        m = np.zeros((n, n), np.float32)
        h = n // 2
        for j in range(n):
            m[(j + h) % n, j] = 1.0
        return m

    c["pm32"] = perm(32)
    c["pm64"] = perm(64)
    kk = np.arange(128)[:, None]
    qq = np.arange(128)[None, :]
    md = np.zeros((128, 16, 128), np.float32)
    for delta in range(16):
        d = 128 * delta + qq - kk
        m = ((d >= 0) & (d <= 128)).astype(np.float32) + ((d >= 0) & (d % 4 == 0) & (d <= 512)).astype(np.float32) \
            + ((d >= 0) & (d % 16 == 0)).astype(np.float32)
        md[:, 15 - delta, :] = m
    c["mdil"] = md.reshape(128, 2048)
    c["tri"] = (kk <= qq).astype(np.float32)
    c["upp"] = (kk > qq).astype(np.float32)
    cc = np.arange(128)[:, None]
    vcm = ((16 * cc + 31) <= np.arange(S)[None, :]).astype(np.float32)
    vcm[127] = 0
    c["vcm"] = vcm
    cs = np.arange(128) * 16
    ss = np.arange(32) * 64
    cover = ((cs[:, None] < ss[None, :] + 64) & (cs[:, None] + 32 > ss[None, :])).astype(np.float32)
    cover[127] = 0
    c["cover"] = cover
    p = np.arange(S)
    jj = np.arange(32)[None, :]
    qblk = (p // 64)[:, None]
    valid = (ss[None, :] <= p[:, None])
    forced = (jj == 0) | (jj == qblk) | (jj == qblk - 1)
    selA = (valid & ~forced).astype(np.float32)
    selB = np.where(valid, np.where(forced, 1e4, 0.0), -1.0).astype(np.float32)
    c["selA"] = selA.reshape(16, 128, 32).transpose(1, 0, 2).copy()
    c["selB"] = selB.reshape(16, 128, 32).transpose(1, 0, 2).copy()
    E = np.zeros((32, 16, 128), np.float32)
    for kt in range(16):
        E[2 * kt, kt, :64] = 1
        E[2 * kt + 1, kt, 64:] = 1
    c["emat"] = E.reshape(32, 2048)
    c["ltri"] = (kk < qq).astype(np.float32)
    c["ecap"] = np.tile((np.arange(32) * CAP).astype(np.float32)[None, :], (128, 1))
    return c


CONST_SHAPES = {"ident": (128, 128), "cs32": (32, 2, 2048), "cs64": (64, 2, 2048), "cskc": (32, 2, 128),
                "pm32": (32, 32), "pm64": (64, 64), "mdil": (128, 2048), "tri": (128, 128), "upp": (128, 128),
                "vcm": (128, 2048), "cover": (128, 32), "selA": (128, 16, 32), "selB": (128, 16, 32),
                "emat": (32, 2048), "ltri": (128, 128), "ecap": (128, 32)}

W_SHAPES = {"w_in": (2, 36, 128, 2048), "q_lat_norm": (2, 384), "w_q_up": (2, 384, 768), "kv_lat_norm": (2, 128),
            "w_kv_up": (2, 128, 1024), "cmp_pos_k": (2, 32, 128), "cmp_w1_k": (2, 4096, 256),
            "cmp_w2_k": (2, 256, 128), "cmp_pos_v": (2, 32, 128), "cmp_w1_v": (2, 4096, 256),
            "cmp_w2_v": (2, 256, 128), "w_out": (2, 2048, 2048), "ln1_g": (2, 2048), "ln1_b": (2, 2048),
            "w_grp": (2, 2048, 4), "w_exp": (2, 2048, 32), "w_gate": (2, 32, 2048, 512),
            "w_up": (2, 32, 2048, 512), "w_down": (2, 32, 512, 2048), "ln2_g": (2, 2048), "ln2_b": (2, 2048)}


W_CHUNKS = ([(C_QLAT + 128 * c, 128) for c in range(3)] + [(C_KVLAT, 128), (C_KROPE, 64)]
            + [(C_DQ + 128 * h, 128) for h in range(6)] + [(C_DK + 128 * h, 128) for h in range(6)]
            + [(C_DV + 128 * h, 128) for h in range(6)] + [(C_GATE, 18), (C_NKC, 128), (C_NVC, 128)]
            + [(C_NQ + 128 * h, 128) for h in range(6)] + [(C_NKS, 128), (C_NKW, 128), (C_NVS, 128), (C_NVW, 128)])
W_CHUNK_IDX = {c: i for i, c in enumerate(W_CHUNKS)}


def relayout_w_in(w_in):
    L = w_in.shape[0]
    out = np.zeros((L, len(W_CHUNKS), 128, 16, 128), np.float32)
    for i, (c0, n) in enumerate(W_CHUNKS):
        out[:, i, :, :, 0:n] = w_in[:, :, c0:c0 + n].reshape(L, 16, 128, n).transpose(0, 2, 1, 3)
    return out.reshape(L, len(W_CHUNKS), 128, 2048)


def pe_group(mms):
    def fn(e):
        ins = None
        for (o, l, r, st, sp) in mms:
            ins = e.matmul(o, lhsT=l, rhs=r, start=st, stop=sp)
        return ins
    return fn


def build(n_layers=DEPTH, stage="full"):
    nc = bass.Bass("TRN2", target_bir_lowering=False)

    def din(name, shape, dt=F32):
        return nc.dram_tensor(name, list(shape), dt, kind="ExternalInput").ap()

    def dscr(name, shape, dt):
        return nc.dram_tensor(name, list(shape), dt, kind="Internal").ap()

    x_d = din("x", [S, D])
    W = {k: din(k, v) for k, v in W_SHAPES.items()}
    CD = {k: din("c_" + k, v) for k, v in CONST_SHAPES.items()}
    out_d = nc.dram_tensor("out", [S, D], F32, kind="ExternalOutput").ap()
    dbg_d = dbg2_d = None
    if stage != "full":
        dbg_d = nc.dram_tensor("dbg", [S, D], F32, kind="ExternalOutput").ap()
        dbg2_d = nc.dram_tensor("dbg2", [S, D], F32, kind="ExternalOutput").ap()
        dbg3_d = nc.dram_tensor("dbg3", [8, 128, S], BF16, kind="ExternalOutput").ap()

    o_scr = dscr("o_scr", [S, D], BF16)
    nq_scr = dscr("nq_scr", [6, 128, S], BF16)
    ocmp_scr = dscr("ocmp_scr", [6, S, 128], F32)
    x1_scr = dscr("x1_scr", [S, D], F32)
    x1b_scr = dscr("x1b_scr", [S, D], BF16)
    xres_scr = dscr("xres_scr", [S, D], F32)
    xg_scr = dscr("xg_scr", [NEXP * CAP, D], BF16)
    y_scr = dscr("y_scr", [NEXP * CAP, D], F32)

    P = Prog(nc)
    XT = P.sb("XT", [128, 16, S], BF16)
    ARENA = P.sb("ARENA", [128, 30 * 1024], F32)
    ident = P.sb("ident", [128, 128], F32)
    identb = P.sb("identb", [128, 128], BF16)
    cs32 = P.sb("cs32", [32, 2, S], F32)
    pm32 = P.sb("pm32", [32, 32], BF16)
    pm64 = P.sb("pm64", [64, 64], BF16)
    onesb = P.sb("onesb", [128, 128], BF16)
    tri = P.sb("tri", [128, 128], BF16)
    upp = P.sb("upp", [128, 128], BF16)
    ltri = P.sb("ltri", [128, 128], BF16)
    ecap = P.sb("ecap", [128, 32], F32)
    gates = P.sb("gates", [128, 16, 18], F32)
    slots = P.sb("slots", [128, 16, 2], I32)
    gatew = P.sb("gatew", [128, 16, 2], F32)
    mk = P.sb("mk", [128, 16, 32], BF16)
    oh = P.sb("oh", [128, 16, 2, 32], BF16)
    PS = [P.ps("ps%d" % i, [128, 512], F32) for i in range(8)]
    pA, pB, pS0, pS1, pO0, pO1, pM0, pM1 = PS
    PN = {id(t): "ps%d" % i for i, t in enumerate(PS)}

    def pn(t):
        return PN[id(t)]

    arena_off = [0]

    def carve(shape, dt):
        n = int(np.prod(shape[1:]))
        words = n if dt in (F32, I32) else (n + 1) // 2
        words = (words + 15) // 16 * 16
        o = arena_off[0]
        arena_off[0] += words
        assert arena_off[0] <= 30 * 1024, ("arena overflow", arena_off[0])
        v = ARENA[0:shape[0], o:o + words]
        if dt != F32:
            v = v.bitcast(dt)
        v = v[:, 0:n]
        if len(shape) == 3:
            v = v.rearrange("p (a b) -> p a b", a=shape[1])
        elif len(shape) == 4:
            v = v.rearrange("p (a b c) -> p a b c", a=shape[1], b=shape[2])
        return v

    def phase():
        P.barrier()
        arena_off[0] = 0

    P.dma("sp", ident[:], CD["ident"], writes=["ident"])
    P.dma("pool", identb[:], CD["ident"], writes=["identb"])
    P.dma("sp", cs32[:], CD["cs32"], writes=["cs32"])
    P.dma("pool", pm32[:], CD["pm32"], writes=["pm32"])
    P.dma("pool", pm64[:], CD["pm64"], writes=["pm64"])
    P.dma("pool", tri[:], CD["tri"], writes=["tri"])
    P.dma("pool", upp[:], CD["upp"], writes=["upp"])
    P.dma("pool", ltri[:], CD["ltri"], writes=["ltri"])
    P.dma("sp", ecap[:], CD["ecap"], writes=["ecap"])
    P.op("dve", lambda e: e.memset(onesb[:], 1.0), writes=["onesb"])
    epsr = P.sb("epsr", [128, 2], F32)
    P.op("dve", lambda e: e.memset(epsr[:, 0:1], RMS_EPS), writes=["epsr"])
    P.op("dve", lambda e: e.memset(epsr[:, 1:2], LN_EPS), writes=["epsr"])

    arena_off[0] = 0
    ztile = carve([128, 8192], BF16)
    P.op("pool", lambda e: e.memset(ztile, 0.0), writes=["ztile"])
    xg_flat = xg_scr.rearrange("(a p) d -> a p d", p=128)
    for a in range(NEXP * CAP // 128):
        P.dma("sp", xg_flat[a], ztile[:, 0:D], reads=["ztile"], writes=["xg_scr"])

    def layer(l, x_src, last):
        phase()
        QTS = [carve([128, S], BF16) for _ in range(2)]
        QR = carve([64, S], BF16)
        KT = [carve([128, S], BF16) for _ in range(2)]
        KR = carve([64, S], BF16)
        VA = carve([128, 16, 2, 129], BF16)
        QLT = carve([128, 3, S], BF16)
        KVLT = carve([128, S], BF16)
        SCR16 = carve([128, 4096], F32)
        WST = [carve([128, 16, 128], BF16) for _ in range(3)]
        PT = [carve([128, 512], BF16) for _ in range(4)]
        mdil = carve([128, S], BF16)
        vcm = mdil
        emat = mdil[0:32, :]
        nbT = carve([32, S], BF16)
        selA = carve([128, 16, 32], F32)
        selB = carve([128, 16, 32], F32)
        imp = carve([128, 16, 32], F32)
        cover = carve([128, 32], F32)
        rt12 = carve([128, 1024], F32)
        rt1 = rt12[0:64, 0:512]
        rt2 = rt12[0:64, 512:1024]
        sqb = carve([128, 3, 512], BF16)
        rstd = carve([128, 512], F32)
        wqu = carve([128, 3, 768], BF16)
        wkvu = carve([128, 1024], BF16)
        qg = carve([128, 4], F32)
        xin = [SCR16[:, 0:2048], SCR16[:, 2048:4096]]
        fin = carve([128, 8], F32)
        ofin = [carve([128, 128], BF16) for _ in range(2)]
        onsa = QLT[:, :, :].rearrange("p a b -> p (a b)")[:, 0:4096].bitcast(F32).rearrange("p (a b) -> p a b", a=16)
        ocl = [carve([128, 128], F32) for _ in range(2)]
        KCT = carve([128, 128], BF16)
        VCA = carve([128, 161], BF16)
        peT = carve([128, 32], F32)
        XPE = QLT[:, :, :].rearrange("p a b -> p (a b)")[:, 0:32 * 127].rearrange("p (a b) -> p a b", a=32)
        hT = carve([128, 2, 127], BF16)
        w2 = carve([128, 2, 128], BF16)
        cskc = carve([32, 2, 128], F32)
        hsc = [carve([128, 127], F32) for _ in range(3)]
        selw = rt12[:, :].rearrange("p (a b) -> p a b", a=32)
        selr = carve([128, 32], F32)

        P.dma("pool", mdil, CD["mdil"], writes=["msk"])
        P.dma("sp", selA, CD["selA"], writes=["selA"])
        P.dma("sp", selB, CD["selB"], writes=["selB"])
        P.dma("sp", cover, CD["cover"], writes=["cover"])
        P.dma("sp", cskc, CD["cskc"], writes=["cskc"])
        cs64 = SCR16[0:64, :].rearrange("p (a b) -> p a b", a=2)
        P.dma("pool", wqu, W["w_q_up"][l].rearrange("(kc p) n -> p kc n", p=128), writes=["wqu"])
        P.dma("pool", wkvu, W["w_kv_up"][l], writes=["wkvu"])
        P.dma("sp", qg[:, 0:3], W["q_lat_norm"][l].rearrange("(kc p) -> p kc", p=128), writes=["qg"], allow_slow_non_contiguous=True)
        P.dma("sp", qg[:, 3:4], W["kv_lat_norm"][l].rearrange("(kc p) -> p kc", p=128), writes=["qg"], allow_slow_non_contiguous=True)
        P.op("dve", lambda e: e.memset(VA[:, :, :, 128:129], 1.0), writes=["va0", "va1"])
        P.op("dve", lambda e: e.memset(imp, 0.0), writes=["imp"])

        for t in range(NT):
            xt_ = xin[t % 2]
            P.dma("sp", xt_, x_src[t * 128:(t + 1) * 128, :], writes=["xin%d" % (t % 2)])
            for g in range(4):
                pb = (pA, pB)[(t * 4 + g) % 2]
                P.op("pe", (lambda pb=pb, xt_=xt_, g=g: lambda e: [e.transpose(out=pb[:, j * 128:(j + 1) * 128], in_=xt_[:, (g * 4 + j) * 128:(g * 4 + j + 1) * 128], identity=ident[:]) for j in range(4)][-1])(),
                     reads=["xin%d" % (t % 2), "ident"], writes=[pn(pb)])
                eng = "act" if g % 2 == 0 else "dve"
                dst = XT[:, g * 4:(g + 1) * 4, t * 128:(t + 1) * 128]
                src = pb[:, :].rearrange("p (a b) -> p a b", a=4)
                if eng == "act":
                    P.op("act", lambda e, dst=dst, src=src: e.activation(out=dst, in_=src, func=AF.Copy), writes=[pn(pb), "XT"])
                else:
                    P.op("dve", lambda e, dst=dst, src=src: e.tensor_copy(out=dst, in_=src), writes=[pn(pb), "XT"])

        P.dma("sp", cs64, CD["cs64"], writes=["scr16", "xin0", "xin1"])
        wst_i = [0]
        pb_i = [0]

        def next_pb():
            pb_i[0] += 1
            return (pA, pB)[pb_i[0] % 2]

        def load_w(col0, ncols):
            s = wst_i[0] % 3
            wst_i[0] += 1
            P.dma("pool", WST[s], W["w_in"][l, W_CHUNK_IDX[(col0, ncols)]].rearrange("p (kc n) -> p kc n", kc=16),
                  writes=["wst%d" % s])
            return s

        def proj_fm(col0, ncols, evac):
            s = load_w(col0, ncols)
            for tc in range(4):
                pb = next_pb()
                mms = [(pb[0:ncols, :], WST[s][:, kc, 0:ncols], XT[:, kc, tc * 512:(tc + 1) * 512], kc == 0, kc == 15) for kc in range(16)]
                P.op("pe", pe_group(mms), reads=["wst%d" % s, "XT"], writes=[pn(pb)])
                evac(pb, tc)

        def proj_tm(col0, ncols, evac):
            s = load_w(col0, ncols)
            for kt in range(NT):
                pb = next_pb()
                mms = [(pb[:, 0:ncols], XT[:, kc, kt * 128:(kt + 1) * 128], WST[s][:, kc, 0:ncols], kc == 0, kc == 15) for kc in range(16)]
                P.op("pe", pe_group(mms), reads=["wst%d" % s, "XT"], writes=[pn(pb)])
                evac(pb, kt)

        def rope_evac(dst, dname, nrows, R, cs, csname, pm, pmname):
            def ev(pb, tc, ncol=512, c0=None):
                c0 = tc * 512 if c0 is None else c0
                sl = slice(c0, c0 + ncol)
                tn = "%s.%d" % (dname, tc)
                P.op("act", lambda e: e.activation(out=dst[0:nrows, sl], in_=pb[0:nrows, 0:ncol], func=AF.Copy), writes=[pn(pb), tn])
                pm_ = pM0
                P.op("pe", lambda e: e.matmul(pm_[0:R, 0:ncol], lhsT=pm[0:R, 0:R], rhs=dst[0:R, sl], start=True, stop=True),
                     reads=[tn, pmname], writes=[pn(pm_)])
                P.op("dve", lambda e: e.tensor_tensor(out=rt1[0:R, 0:ncol], in0=dst[0:R, sl], in1=cs[0:R, 0, sl], op=ALU.mult),
                     reads=[tn, csname], writes=["rt1"])
                P.op("dve", lambda e: e.tensor_tensor(out=rt2[0:R, 0:ncol], in0=pm_[0:R, 0:ncol], in1=cs[0:R, 1, sl], op=ALU.mult),
                     reads=[csname], writes=["rt2", pn(pm_)])
                P.op("pool", lambda e: e.tensor_tensor(out=dst[0:R, sl], in0=rt1[0:R, 0:ncol], in1=rt2[0:R, 0:ncol], op=ALU.add),
                     reads=["rt1", "rt2"], writes=[tn])
            return ev

        def copy_evac(dst, dname, nrows):
            def ev(pb, tc):
                sl = slice(tc * 512, (tc + 1) * 512)
                P.op("act", lambda e: e.activation(out=dst[0:nrows, sl], in_=pb[0:nrows, :], func=AF.Copy),
                     writes=[pn(pb), "%s.%d" % (dname, tc)])
            return ev

        def v_evac(slot, ncols=128):
            def ev(pb, kt):
                P.op("dve", lambda e: e.tensor_copy(out=VA[:, kt, slot, 0:ncols], in_=pb[:, 0:ncols]), writes=[pn(pb), "va%d" % slot])
            return ev

        def names4(n):
            return ["%s.%d" % (n, i) for i in range(4)]

        pt_i = [0]
        ps_i = [0]
        po_i = [0]

        def attn(qparts, kparts, vslot, scale, kts_fn, mask_fn, W_out, finalize, nk=128, bias=False, vsrc=None, vname=None):
            qreads = [n for (_, _, nm) in qparts for n in nm]
            kreads = [n for (_, _, nm) in kparts for n in nm]
            vname_ = vname or ("va%d" % vslot)
            items = []
            for qt in range(NT):
                kts = kts_fn(qt)
                po = (pO0, pO1)[po_i[0] % 2]
                po_i[0] += 1
                groups = [kts[i:i + 4] for i in range(0, len(kts), 4)]
                for gi, grp in enumerate(groups):
                    psb = (pS0, pS1, pM1)[ps_i[0] % 3]
                    ps_i[0] += 1
                    pts = pt_i[0] % 4
                    pt_i[0] += 1
                    items.append((qt, gi, grp, len(groups), po, psb, pts))

            def stage1(it):
                qt, gi, grp, ng, po, psb, pts = it
                qs = slice(qt * 128, (qt + 1) * 128)
                mms = []
                for j, kt in enumerate(grp):
                    o = psb[0:nk, j * 128:(j + 1) * 128]
                    np_ = len(qparts)
                    for pi in range(np_):
                        qa, K, _ = qparts[pi]
                        ka, _, _ = kparts[pi]
                        mms.append((o, ka[0:K, kt * 128:kt * 128 + nk], qa[0:K, qs], pi == 0, (pi == np_ - 1) and not bias))
                    if bias:
                        mms.append((o, emat[0:32, kt * 128:(kt + 1) * 128], nbT[0:32, qs], False, True))
                P.op("pe", pe_group(mms), reads=qreads + kreads + (["msk", "nbT"] if bias else []), writes=[pn(psb)])
                n = len(grp) * 128
                ptn = "pt%d" % pts
                P.op("act", lambda e: e.activation(out=PT[pts][0:nk, 0:n], in_=psb[0:nk, 0:n], func=AF.Exp, scale=scale),
                     writes=[pn(psb), ptn])
                for (eng, fn, rd) in mask_fn(qt, grp, PT[pts]):
                    P.op(eng, fn, reads=rd, writes=[ptn])

            def stage2(it):
                qt, gi, grp, ng, po, psb, pts = it
                ptn = "pt%d" % pts
                mms = []
                for j, kt in enumerate(grp):
                    vv = VA[0:nk, kt, vslot, 0:W_out] if vsrc is None else vsrc
                    mms.append((po[:, 0:W_out], PT[pts][0:nk, j * 128:(j + 1) * 128], vv,
                                gi == 0 and j == 0, gi == ng - 1 and j == len(grp) - 1))
                P.op("pe", pe_group(mms), reads=[ptn, vname_], writes=[pn(po)])
                if gi == ng - 1:
                    finalize(qt, po)

            SK = 2
            for i in range(len(items) + SK):
                if i < len(items):
                    stage1(items[i])
                if i >= SK:
                    stage2(items[i - SK])

        def causal_mask(qt, grp, pt):
            ops = []
            if grp[-1] == qt:
                j = len(grp) - 1
                ops.append(("dve", lambda e, j=j, pt=pt: e.tensor_tensor(out=pt[:, j * 128:(j + 1) * 128], in0=pt[:, j * 128:(j + 1) * 128], in1=tri[:], op=ALU.mult), ["tri"]))
            return ops

        def fin_simple(colbase):
            def f(qt, po):
                of = ofin[qt % 2]
                ofn = "ofin%d" % (qt % 2)
                P.op("dve", lambda e: e.tensor_scalar(out=fin[:, 0:1], in0=po[:, 128:129], scalar1=1e-30, scalar2=None, op0=ALU.max), writes=[pn(po), "fin"])
                P.op("dve", lambda e: e.reciprocal(out=fin[:, 1:2], in_=fin[:, 0:1]), writes=["fin"])
                P.op("dve", lambda e: e.tensor_scalar(out=of[:], in0=po[:, 0:128], scalar1=fin[:, 1:2], scalar2=None, op0=ALU.mult),
                     reads=["fin"], writes=[pn(po), ofn])
                P.dma("sp", o_scr[qt * 128:(qt + 1) * 128, colbase:colbase + 128], of[:], reads=[ofn], writes=["o_scr"])
            return f

        def qlat_evac(c):
            def ev(pb, tc):
                sl = slice(tc * 512, (tc + 1) * 512)
                P.op("act", lambda e: e.activation(out=QLT[:, c, sl], in_=pb[:, :], func=AF.Copy), writes=[pn(pb), "qlt.%d" % tc])
            return ev
        for c in range(3):
            proj_fm(C_QLAT + 128 * c, 128, qlat_evac(c))
        proj_fm(C_KVLAT, 128, copy_evac(KVLT, "kvlt", 128))
        proj_fm(C_KROPE, 64, rope_evac(KR, "kr", 64, 64, cs64, "scr16", pm64, "pm64"))

        def rms_apply(views, nfeat, gcols, tnames_fn):
            for tc in range(4):
                sl = slice(tc * 512, (tc + 1) * 512)
                n = len(views)
                for c in range(n):
                    P.op("dve", lambda e, c=c: e.tensor_tensor(out=sqb[:, c, :], in0=views[c][:, sl], in1=views[c][:, sl], op=ALU.mult),
                         reads=[tnames_fn(tc)], writes=["sqb"])
                mms = [(pM1[:, :], onesb[:, :], sqb[:, c, :], c == 0, c == n - 1) for c in range(n)]
                P.op("pe", pe_group(mms), reads=["sqb", "onesb"], writes=[pn(pM1)])
                P.op("act", lambda e: e.activation(out=rstd[:], in_=pM1[:, :], func=AF.Ln, scale=1.0 / nfeat, bias=epsr[:, 0:1]),
                     reads=["epsr"], writes=[pn(pM1), "rstd"])
                P.op("act", lambda e: e.activation(out=rstd[:], in_=rstd[:], func=AF.Exp, scale=-0.5), writes=["rstd"])
                for c in range(n):
                    P.op("dve", lambda e, c=c: e.scalar_tensor_tensor(out=views[c][:, sl], in0=views[c][:, sl], scalar=qg[:, gcols[c]:gcols[c] + 1], in1=rstd[:], op0=ALU.mult, op1=ALU.mult),
                         reads=["rstd", "qg"], writes=[tnames_fn(tc)])
        rms_apply([QLT[:, 0, :], QLT[:, 1, :], QLT[:, 2, :]], 384.0, [0, 1, 2], lambda tc: "qlt.%d" % tc)
        rms_apply([KVLT], 128.0, [3], lambda tc: "kvlt.%d" % tc)

        sc_mla = 192.0 ** -0.5
        for h in range(4):
            qs_, ks_ = h % 2, h % 2
            for tc in range(4):
                sl = slice(tc * 512, (tc + 1) * 512)
                pb = next_pb()
                mms = [(pb[:, :], wqu[:, c, h * 192:h * 192 + 128], QLT[:, c, sl], c == 0, c == 2) for c in range(3)]
                P.op("pe", pe_group(mms), reads=["wqu", "qlt.%d" % tc], writes=[pn(pb)])
                copy_evac(QTS[qs_], "qts%d" % qs_, 128)(pb, tc)
                pb = next_pb()
                mms = [(pb[0:64, :], wqu[:, c, h * 192 + 128:h * 192 + 192], QLT[:, c, sl], c == 0, c == 2) for c in range(3)]
                P.op("pe", pe_group(mms), reads=["wqu", "qlt.%d" % tc], writes=[pn(pb)])
                rope_evac(QR, "qr", 64, 64, cs64, "scr16", pm64, "pm64")(pb, tc)
                pb = next_pb()
                P.op("pe", pe_group([(pb[:, :], wkvu[:, h * 256:h * 256 + 128], KVLT[:, sl], True, True)]), reads=["wkvu", "kvlt.%d" % tc], writes=[pn(pb)])
                copy_evac(KT[ks_], "kt%d" % ks_, 128)(pb, tc)
            for kt in range(NT):
                pb = next_pb()
                P.op("pe", pe_group([(pb[:, 0:128], KVLT[:, kt * 128:(kt + 1) * 128], wkvu[:, h * 256 + 128:h * 256 + 256], True, True)]),
                     reads=["wkvu"] + names4("kvlt"), writes=[pn(pb)])
                v_evac(h % 2)(pb, kt)
            attn([(QTS[qs_], 128, names4("qts%d" % qs_)), (QR, 64, names4("qr"))],
                 [(KT[ks_], 128, names4("kt%d" % ks_)), (KR, 64, names4("kr"))],
                 h % 2, sc_mla, lambda qt: list(range(qt + 1)), causal_mask, 129, fin_simple(h * 128))

        sc = 128.0 ** -0.5

        def dil_mask(qt, grp, pt):
            n = len(grp) * 128
            i0 = (15 - qt + grp[0]) * 128
            return [("dve", lambda e: e.tensor_tensor(out=pt[:, 0:n], in0=pt[:, 0:n], in1=mdil[:, i0:i0 + n], op=ALU.mult), ["msk"])]
        for h in range(6):
            s_ = h % 2
            proj_fm(C_DQ + 128 * h, 128, rope_evac(QTS[s_], "qts%d" % s_, 128, 32, cs32, "cs32", pm32, "pm32"))
            proj_fm(C_DK + 128 * h, 128, rope_evac(KT[s_], "kt%d" % s_, 128, 32, cs32, "cs32", pm32, "pm32"))
            proj_tm(C_DV + 128 * h, 128, v_evac(s_))
            if stage == "pdbg":
                P.barrier()
                P.dma("sp", dbg3_d[0], XT[:, 0, :], writes=["dbg3"])
                P.dma("sp", dbg3_d[1], QTS[0], writes=["dbg3"])
                P.dma("sp", dbg3_d[2], KT[0], writes=["dbg3"])
                P.dma("sp", dbg3_d[3].rearrange("p (a b) -> p a b", a=16), VA[:, :, 0, 0:128], writes=["dbg3"])
                P.dma("sp", dbg3_d[4], KVLT, writes=["dbg3"])
                P.dma("sp", dbg3_d[5], QLT[:, 0, :], writes=["dbg3"])
                P.dma("sp", dbg3_d[6, 0:64], KR, writes=["dbg3"])
                P.dma("sp", dbg3_d[7].rearrange("p (a b) -> p a b", a=16), VA[:, :, 0, 1:129], writes=["dbg3"])
                return "stop"
            attn([(QTS[s_], 128, names4("qts%d" % s_))], [(KT[s_], 128, names4("kt%d" % s_))], s_, sc,
                 lambda qt: list(range(qt + 1)), dil_mask, 129, fin_simple(512 + h * 128))

        def gate_evac(pb, kt):
            P.op("act", lambda e: e.activation(out=gates[:, kt, :], in_=pb[:, 0:18], func=AF.Sigmoid), writes=[pn(pb), "gates"])
        proj_tm(C_GATE, 18, gate_evac)

        w1 = SCR16[:, :].bitcast(BF16).rearrange("p (a b) -> p a b", a=32)
        for which in range(2):
            col = (C_NKC, C_NVC)[which]
            src = KT[which]
            proj_fm(col, 128, copy_evac(src, "kt%d" % which, 128))
            P.dma("sp", peT, W[("cmp_pos_k", "cmp_pos_v")[which]][l].rearrange("i d -> d i"), writes=["peT"], allow_slow_non_contiguous=True)
            P.dma("pool", w1, W[("cmp_w1_k", "cmp_w1_v")[which]][l].rearrange("(i d) j -> d i j", d=128), writes=["scr16"])
            P.dma("pool", w2, W[("cmp_w2_k", "cmp_w2_v")[which]][l].rearrange("(jc p) d -> p jc d", p=128), writes=["w2"])
            ovl = bass.AP(src.tensor, src.offset, [list(src.ap[0]), [1, 32], [16, 127]])
            P.op("dve", lambda e, ovl=ovl: e.tensor_tensor(out=XPE, in0=ovl, in1=peT.unsqueeze(2).broadcast_to([128, 32, 127]), op=ALU.add),
                 reads=names4("kt%d" % which) + ["peT"], writes=["xpe"])
            for jc in range(2):
                pb = next_pb()
                mms = [(pb[:, 0:127], w1[:, i, jc * 128:(jc + 1) * 128], XPE[:, i, :], i == 0, i == 31) for i in range(32)]
                P.op("pe", pe_group(mms), reads=["scr16", "xpe"], writes=[pn(pb)])
                P.op("act", lambda e, pb=pb: e.activation(out=hsc[0], in_=pb[:, 0:127], func=AF.Square), writes=[pn(pb), "hsc0"])
                P.op("dve", lambda e: e.tensor_scalar(out=hsc[0], in0=hsc[0], scalar1=0.044715, scalar2=1.0, op0=ALU.mult, op1=ALU.add), writes=["hsc0"])
                P.op("dve", lambda e, pb=pb: e.tensor_tensor(out=hsc[0], in0=hsc[0], in1=pb[:, 0:127], op=ALU.mult), writes=["hsc0", pn(pb)])
                P.op("act", lambda e: e.activation(out=hsc[1], in_=hsc[0], func=AF.Sigmoid, scale=1.5957691216057308), reads=["hsc0"], writes=["hsc1"])
                P.op("dve", lambda e, pb=pb, jc=jc: e.tensor_tensor(out=hT[:, jc, :], in0=hsc[1], in1=pb[:, 0:127], op=ALU.mult), reads=["hsc1"], writes=["hT", pn(pb)])
            if which == 0:
                pb = next_pb()
                mms = [(pb[:, 0:127], w2[:, jc, :], hT[:, jc, :], jc == 0, jc == 1) for jc in range(2)]
                P.op("pe", pe_group(mms), reads=["w2", "hT"], writes=[pn(pb)])
                rope_evac(KCT, "kct", 128, 32, cskc, "cskc", pm32, "pm32")(pb, 0, ncol=127, c0=0)
            else:
                pb = next_pb()
                mms = [(pb[0:127, 0:128], hT[:, jc, :], w2[:, jc, :], jc == 0, jc == 1) for jc in range(2)]
                P.op("pe", pe_group(mms), reads=["w2", "hT"], writes=[pn(pb)])
                P.op("dve", lambda e, pb=pb: e.tensor_copy(out=VCA[0:127, 0:128], in_=pb[0:127, 0:128]), writes=[pn(pb), "vca"])
                P.op("dve", lambda e: e.memset(VCA[0:127, 128:129], 1.0), writes=["vca"])
                P.op("dve", lambda e: e.tensor_copy(out=VCA[0:127, 129:161], in_=cover[0:127, :]), reads=["cover"], writes=["vca"])

        P.dma("pool", vcm, CD["vcm"], writes=["msk"])
        def cmp_mask(qt, grp, pt):
            return [("dve", lambda e: e.tensor_tensor(out=pt[0:127, 0:128], in0=pt[0:127, 0:128], in1=vcm[0:127, qt * 128:(qt + 1) * 128], op=ALU.mult), ["msk"])]
        for h in range(6):
            s_ = h % 2
            proj_fm(C_NQ + 128 * h, 128, rope_evac(QTS[s_], "qts%d" % s_, 128, 32, cs32, "cs32", pm32, "pm32"))
            P.dma("sp", nq_scr[h], QTS[s_], reads=names4("qts%d" % s_), writes=["nq_scr%d" % h])

            def fin_cmp(qt, po, h=h):
                oc = ocl[qt % 2]
                ocn = "ocl%d" % (qt % 2)
                P.op("dve", lambda e: e.tensor_scalar(out=fin[:, 0:1], in0=po[:, 128:129], scalar1=1e-30, scalar2=None, op0=ALU.max), writes=[pn(po), "fin"])
                P.op("dve", lambda e: e.reciprocal(out=fin[:, 1:2], in_=fin[:, 0:1]), writes=["fin"])
                P.op("dve", lambda e: e.scalar_tensor_tensor(out=imp[:, qt, :], in0=po[:, 129:161], scalar=fin[:, 1:2], in1=imp[:, qt, :], op0=ALU.mult, op1=ALU.add),
                     reads=["fin"], writes=[pn(po), "imp"])
                P.op("dve", lambda e: e.tensor_tensor(out=fin[:, 2:3], in0=fin[:, 1:2], in1=gates[:, qt, 3 * h:3 * h + 1], op=ALU.mult), reads=["gates"], writes=["fin"])
                P.op("dve", lambda e: e.tensor_scalar(out=oc[:], in0=po[:, 0:128], scalar1=fin[:, 2:3], scalar2=None, op0=ALU.mult), reads=["fin"], writes=[pn(po), ocn])
                P.dma("sp", ocmp_scr[h, qt * 128:(qt + 1) * 128, :], oc[:], reads=[ocn], writes=["ocmp_scr%d" % h])
            attn([(QTS[s_], 128, names4("qts%d" % s_))], [(KCT, 128, ["kct.0"])], 0, sc, lambda qt: [0], cmp_mask, 161, fin_cmp,
                 nk=127, vsrc=VCA[0:127, 0:161], vname="vca")

        for qt in range(NT):
            P.op("dve", lambda e, qt=qt: e.tensor_tensor(out=selr[:], in0=imp[:, qt, :], in1=selA[:, qt, :], op=ALU.mult), reads=["imp", "selA"], writes=["selr"])
            P.op("dve", lambda e, qt=qt: e.tensor_tensor(out=selr[:], in0=selr[:], in1=selB[:, qt, :], op=ALU.add), reads=["selB"], writes=["selr"])
            P.op("dve", lambda e: e.tensor_tensor(out=selw, in0=selr[:].unsqueeze(1).broadcast_to([128, 32, 32]), in1=selr[:].unsqueeze(2).broadcast_to([128, 32, 32]), op=ALU.is_gt),
                 reads=["selr"], writes=["selw", "rt1", "rt2"])
            P.op("dve", lambda e: e.tensor_reduce(out=selr[:], in_=selw, axis=AX.X, op=ALU.add), reads=["selw"], writes=["selr"])
            P.op("dve", lambda e: e.tensor_scalar(out=selr[:], in0=selr[:], scalar1=15.5, scalar2=-30000.0, op0=ALU.is_gt, op1=ALU.mult), writes=["selr"])
            P.op("pe", lambda e: e.transpose(out=pM1[0:32, 0:128], in_=selr[:], identity=ident[:]), reads=["selr", "ident"], writes=[pn(pM1)])
            P.op("act", lambda e, qt=qt: e.activation(out=nbT[0:32, qt * 128:(qt + 1) * 128], in_=pM1[0:32, 0:128], func=AF.Copy), writes=[pn(pM1), "nbT"])

        P.dma("pool", emat, CD["emat"], writes=["msk"])
        proj_fm(C_NKS, 128, rope_evac(KT[0], "kt0", 128, 32, cs32, "cs32", pm32, "pm32"))
        proj_fm(C_NKW, 128, rope_evac(KT[1], "kt1", 128, 32, cs32, "cs32", pm32, "pm32"))
        proj_tm(C_NVS, 128, v_evac(0))
        proj_tm(C_NVW, 128, v_evac(1))

        def win_mask(qt, grp, pt):
            ops = []
            for j, kt in enumerate(grp):
                if kt == qt:
                    ops.append(("dve", lambda e, j=j: e.tensor_tensor(out=pt[:, j * 128:(j + 1) * 128], in0=pt[:, j * 128:(j + 1) * 128], in1=tri[:], op=ALU.mult), ["tri"]))
                elif kt == qt - 4:
                    ops.append(("dve", lambda e, j=j: e.tensor_tensor(out=pt[:, j * 128:(j + 1) * 128], in0=pt[:, j * 128:(j + 1) * 128], in1=upp[:], op=ALU.mult), ["upp"]))
            return ops
        for h in range(6):
            s_ = h % 2
            P.dma("sp", QTS[s_], nq_scr[h], reads=["nq_scr%d" % h], writes=names4("qts%d" % s_))

            def fin_slc(qt, po, h=h):
                P.op("dve", lambda e: e.tensor_scalar(out=fin[:, 0:1], in0=po[:, 128:129], scalar1=1e-30, scalar2=None, op0=ALU.max), writes=[pn(po), "fin"])
                P.op("dve", lambda e: e.reciprocal(out=fin[:, 1:2], in_=fin[:, 0:1]), writes=["fin"])
                P.op("dve", lambda e: e.tensor_tensor(out=fin[:, 2:3], in0=fin[:, 1:2], in1=gates[:, qt, 3 * h + 1:3 * h + 2], op=ALU.mult), reads=["gates"], writes=["fin"])
                P.op("dve", lambda e: e.tensor_scalar(out=onsa[:, qt, :], in0=po[:, 0:128], scalar1=fin[:, 2:3], scalar2=None, op0=ALU.mult), reads=["fin"], writes=[pn(po), "onsa"])
            attn([(QTS[s_], 128, names4("qts%d" % s_))], [(KT[0], 128, names4("kt0"))], 0, sc, lambda qt: list(range(qt + 1)), causal_mask, 129, fin_slc, bias=True)

            def fin_win(qt, po, h=h):
                oc = ocl[qt % 2]
                ocn = "ocl%d" % (qt % 2)
                of = ofin[qt % 2]
                ofn = "ofin%d" % (qt % 2)
                P.dma("sp", oc[:], ocmp_scr[h, qt * 128:(qt + 1) * 128, :], reads=["ocmp_scr%d" % h], writes=[ocn])
                P.op("dve", lambda e: e.tensor_scalar(out=fin[:, 0:1], in0=po[:, 128:129], scalar1=1e-30, scalar2=None, op0=ALU.max), writes=[pn(po), "fin"])
                P.op("dve", lambda e: e.reciprocal(out=fin[:, 1:2], in_=fin[:, 0:1]), writes=["fin"])
                P.op("dve", lambda e: e.tensor_tensor(out=fin[:, 2:3], in0=fin[:, 1:2], in1=gates[:, qt, 3 * h + 2:3 * h + 3], op=ALU.mult), reads=["gates"], writes=["fin"])
                P.op("dve", lambda e: e.scalar_tensor_tensor(out=onsa[:, qt, :], in0=po[:, 0:128], scalar=fin[:, 2:3], in1=onsa[:, qt, :], op0=ALU.mult, op1=ALU.add),
                     reads=["fin"], writes=[pn(po), "onsa"])
                P.op("dve", lambda e: e.tensor_tensor(out=of[:], in0=onsa[:, qt, :], in1=oc[:], op=ALU.add), reads=["onsa", ocn], writes=[ofn])
                P.dma("sp", o_scr[qt * 128:(qt + 1) * 128, 1280 + h * 128:1280 + (h + 1) * 128], of[:], reads=[ofn], writes=["o_scr"])
            attn([(QTS[s_], 128, names4("qts%d" % s_))], [(KT[1], 128, names4("kt1"))], 1, sc, lambda qt: list(range(max(0, qt - 4), qt + 1)), win_mask, 129, fin_win)

        phase()
        WO = XT
        P.dma("pool", WO[:, :, :], W["w_out"][l].rearrange("(kc p) n -> p kc n", p=128), writes=["WO"])
        lng = carve([128, D], F32)
        lnb = carve([128, D], F32)
        P.dma("sp", lng, W["ln1_g"][l:l + 1, :].broadcast_to([128, D]), writes=["lng"])
        P.dma("sp", lnb, W["ln1_b"][l:l + 1, :].broadcast_to([128, D]), writes=["lnb"])
        otl = [carve([128, D], BF16) for _ in range(2)]
        oT = [carve([128, 16, 128], BF16) for _ in range(2)]
        xtl = [carve([128, D], F32) for _ in range(2)]
        ytl = [carve([128, D], F32) for _ in range(2)]
        x1b = [carve([128, D], BF16) for _ in range(2)]
        x1T = carve([128, 16, 128], F32)
        wr = carve([128, 16, 36], F32)
        st = carve([128, 16], F32)
        rl = carve([128, 64], F32)
        P.dma("sp", wr[:, :, 0:4], W["w_grp"][l].rearrange("(kc p) n -> p kc n", p=128), writes=["wr"])
        P.dma("sp", wr[:, :, 4:36], W["w_exp"][l].rearrange("(kc p) n -> p kc n", p=128), writes=["wr"])
        psT = [pS0[:, :].bitcast(BF16), pS1[:, :].bitcast(BF16)]

        def layer_norm(y, yn, g, gname, b, bname, out, outn):
            P.op("dve", lambda e: e.tensor_reduce(out=st[:, 0:1], in_=y, axis=AX.X, op=ALU.add), reads=[yn], writes=["st"])
            P.op("dve", lambda e: e.tensor_scalar(out=st[:, 1:2], in0=st[:, 0:1], scalar1=-1.0 / D, scalar2=None, op0=ALU.mult), writes=["st"])
            P.op("act", lambda e: e.activation(out=y, in_=y, func=AF.Identity, bias=st[:, 1:2], scale=1.0), reads=["st"], writes=[yn])
            P.op("dve", lambda e: e.memset(st[:, 2:3], 0.0), writes=["st"])
            P.op("act", lambda e: e.activation(out=out, in_=y, func=AF.Square, accum_out=st[:, 2:3]), reads=[yn], writes=[outn, "st"])
            P.op("act", lambda e: e.activation(out=st[:, 3:4], in_=st[:, 2:3], func=AF.Ln, scale=1.0 / D, bias=epsr[:, 1:2]), reads=["epsr"], writes=["st"])
            P.op("act", lambda e: e.activation(out=st[:, 3:4], in_=st[:, 3:4], func=AF.Exp, scale=-0.5), writes=["st"])
            P.op("dve", lambda e: e.scalar_tensor_tensor(out=out, in0=y, scalar=st[:, 3:4], in1=g, op0=ALU.mult, op1=ALU.mult), reads=[yn, "st", gname], writes=[outn])
            P.op("pool", lambda e: e.tensor_tensor(out=out, in0=out, in1=b, op=ALU.add), reads=[bname], writes=[outn])

        for t in range(NT):
            i2 = t % 2
            rows = slice(t * 128, (t + 1) * 128)
            P.dma("sp", otl[i2], o_scr[rows, :], reads=["o_scr"], writes=["otl%d" % i2])
            P.dma("sp", xtl[i2], x_src[rows, :], writes=["xtl%d" % i2])
            if stage == "odbg":
                P.op("pool", lambda e, i2=i2: e.tensor_copy(out=ytl[i2], in_=otl[i2]), reads=["otl%d" % i2], writes=["ytl%d" % i2])
                P.dma("sp", dbg2_d[rows, :], ytl[i2], reads=["ytl%d" % i2], writes=["dbg2"])
            for g in range(2):
                pt_ = psT[g]
                pnm = pn((pS0, pS1)[g])
                P.op("pe", (lambda pt_=pt_, g=g, i2=i2: lambda e: [e.transpose(out=pt_[:, j * 128:(j + 1) * 128], in_=otl[i2][:, (g * 8 + j) * 128:(g * 8 + j + 1) * 128], identity=identb[:]) for j in range(8)][-1])(),
                     reads=["otl%d" % i2, "identb"], writes=[pnm])
                P.op("act", lambda e, pt_=pt_, g=g, i2=i2: e.activation(out=oT[i2][:, g * 8:(g + 1) * 8, :], in_=pt_[:, 0:1024].rearrange("p (a b) -> p a b", a=8), func=AF.Copy),
                     writes=[pnm, "oT%d" % i2])
            for cc in range(4):
                pb = next_pb()
                mms = [(pb[:, :], oT[i2][:, kc, :], WO[:, kc, cc * 512:(cc + 1) * 512], kc == 0, kc == 15) for kc in range(16)]
                P.op("pe", pe_group(mms), reads=["oT%d" % i2, "WO"], writes=[pn(pb)])
                P.op("dve", lambda e, pb=pb, cc=cc, i2=i2: e.scalar_tensor_tensor(out=ytl[i2][:, cc * 512:(cc + 1) * 512], in0=xtl[i2][:, cc * 512:(cc + 1) * 512], scalar=ALPHA, in1=pb[:, :], op0=ALU.mult, op1=ALU.add),
                     reads=["xtl%d" % i2], writes=[pn(pb), "ytl%d" % i2])
            layer_norm(ytl[i2], "ytl%d" % i2, lng, "lng", lnb, "lnb", xtl[i2], "xtl%d" % i2)
            P.dma("sp", x1_scr[rows, :], xtl[i2], reads=["xtl%d" % i2], writes=["x1_scr"])
            if stage != "full" and l == 0:
                P.dma("sp", dbg_d[rows, :], xtl[i2], reads=["xtl%d" % i2], writes=["dbg"])
            P.op("pool", lambda e, i2=i2: e.tensor_copy(out=x1b[i2], in_=xtl[i2]), reads=["xtl%d" % i2], writes=["x1b%d" % i2])
            P.dma("sp", x1b_scr[rows, :], x1b[i2], reads=["x1b%d" % i2], writes=["x1b_scr"])
            for g in range(4):
                pm_ = (pM0, pM1)[g % 2]
                P.op("pe", (lambda pm_=pm_, g=g, i2=i2: lambda e: [e.transpose(out=pm_[:, j * 128:(j + 1) * 128], in_=xtl[i2][:, (g * 4 + j) * 128:(g * 4 + j + 1) * 128], identity=ident[:]) for j in range(4)][-1])(),
                     reads=["xtl%d" % i2, "ident"], writes=[pn(pm_)])
                P.op("act", lambda e, pm_=pm_, g=g: e.activation(out=x1T[:, g * 4:(g + 1) * 4, :], in_=pm_[:, :].rearrange("p (a b) -> p a b", a=4), func=AF.Copy),
                     writes=[pn(pm_), "x1T"])
            mms = [(pO0[:, 0:36], x1T[:, kc, :], wr[:, kc, :], kc == 0, kc == 15) for kc in range(16)]
            P.op("pe", pe_group(mms), reads=["x1T", "wr"], writes=[pn(pO0)])
            router(t, pO0, rl)

    def router(t, pl, rl):
        V = lambda f, reads=(), writes=("rl",): P.op("dve", f, reads=reads, writes=writes)
        lg = rl[:, 0:36]
        V(lambda e: e.tensor_copy(out=lg, in_=pl[:, 0:36]), writes=["rl", pn(pl)])
        V(lambda e: e.tensor_reduce(out=rl[:, 36:37], in_=rl[:, 0:4], axis=AX.X, op=ALU.max))
        V(lambda e: e.tensor_scalar(out=rl[:, 40:44], in0=rl[:, 0:4], scalar1=rl[:, 36:37], scalar2=None, op0=ALU.subtract))
        V(lambda e: e.memset(rl[:, 37:38], 0.0))
        P.op("act", lambda e: e.activation(out=rl[:, 44:48], in_=rl[:, 40:44], func=AF.Exp, accum_out=rl[:, 37:38]), writes=["rl"])
        V(lambda e: e.reciprocal(out=rl[:, 38:39], in_=rl[:, 37:38]))
        V(lambda e: e.tensor_scalar(out=rl[:, 40:44], in0=rl[:, 40:44], scalar1=0.0, scalar2=1e30, op0=ALU.is_lt, op1=ALU.mult))
        em = rl[:, 4:36].rearrange("p (g k) -> p g k", g=4)
        V(lambda e: e.tensor_tensor(out=em, in0=em, in1=rl[:, 40:44].unsqueeze(2).broadcast_to([128, 4, 8]), op=ALU.subtract))
        V(lambda e: e.tensor_reduce(out=rl[:, 48:49], in_=rl[:, 4:36], axis=AX.X, op=ALU.max))
        V(lambda e: e.tensor_scalar(out=oh[:, t, 0, :], in0=rl[:, 4:36], scalar1=rl[:, 48:49], scalar2=None, op0=ALU.is_equal), writes=["rl", "oh"])
        V(lambda e: e.scalar_tensor_tensor(out=rl[:, 4:36], in0=oh[:, t, 0, :], scalar=-1e30, in1=rl[:, 4:36], op0=ALU.mult, op1=ALU.add), reads=["oh"])
        V(lambda e: e.tensor_reduce(out=rl[:, 49:50], in_=rl[:, 4:36], axis=AX.X, op=ALU.max))
        V(lambda e: e.tensor_scalar(out=oh[:, t, 1, :], in0=rl[:, 4:36], scalar1=rl[:, 49:50], scalar2=None, op0=ALU.is_equal), writes=["rl", "oh"])
        V(lambda e: e.tensor_tensor(out=mk[:, t, :], in0=oh[:, t, 0, :], in1=oh[:, t, 1, :], op=ALU.add), reads=["oh"], writes=["mk"])
        V(lambda e: e.tensor_tensor(out=rl[:, 50:51], in0=rl[:, 49:50], in1=rl[:, 48:49], op=ALU.subtract))
        P.op("act", lambda e: e.activation(out=rl[:, 51:52], in_=rl[:, 50:51], func=AF.Exp), writes=["rl"])
        V(lambda e: e.tensor_scalar(out=rl[:, 51:52], in0=rl[:, 51:52], scalar1=1.0, scalar2=None, op0=ALU.add))
        V(lambda e: e.reciprocal(out=rl[:, 52:53], in_=rl[:, 51:52]))
        V(lambda e: e.tensor_tensor(out=gatew[:, t, 0:1], in0=rl[:, 52:53], in1=rl[:, 38:39], op=ALU.mult), writes=["rl", "gatew"])
        V(lambda e: e.tensor_tensor(out=gatew[:, t, 1:2], in0=rl[:, 38:39], in1=gatew[:, t, 0:1], op=ALU.subtract), writes=["rl", "gatew"])

    def moe(l, last):
        phase()
        pos = carve([128, 32], F32)
        tmp = carve([128, 32], F32)
        sf = carve([128, 4], F32)
        xbt = [carve([128, D], BF16) for _ in range(2)]
        for t in range(NT):
            mms = [(pM0[:, 0:32], onesb[:, :], mk[:, tp, :], tp == 0, False) for tp in range(t)]
            mms.append((pM0[:, 0:32], ltri[:, :], mk[:, t, :], t == 0, True))
            P.op("pe", pe_group(mms), reads=["mk", "onesb", "ltri"], writes=[pn(pM0)])
            P.op("dve", lambda e: e.scalar_tensor_tensor(out=pos, in0=pM0[:, 0:32], scalar=float(CAP - 1), in1=ecap[:], op0=ALU.min, op1=ALU.add), reads=["ecap"], writes=[pn(pM0), "pos"])
            for k in range(2):
                P.op("dve", lambda e, k=k, t=t: e.tensor_tensor(out=tmp, in0=pos, in1=oh[:, t, k, :], op=ALU.mult), reads=["pos", "oh"], writes=["tmp"])
                P.op("dve", lambda e, k=k: e.tensor_reduce(out=sf[:, k:k + 1], in_=tmp, axis=AX.X, op=ALU.add), reads=["tmp"], writes=["sf"])
            P.op("dve", lambda e, t=t: e.tensor_copy(out=slots[:, t, :], in_=sf[:, 0:2]), reads=["sf"], writes=["slots"])
            i2 = t % 2
            P.dma("sp", xbt[i2], x1b_scr[t * 128:(t + 1) * 128, :], reads=["x1b_scr"], writes=["xbt%d" % i2])
            for k in range(2):
                P.idma(lambda g, t=t, k=k, i2=i2: g.indirect_dma_start(out=xg_scr[:, :], out_offset=bass.IndirectOffsetOnAxis(ap=slots[:, t, k:k + 1], axis=0),
                                                                      in_=xbt[i2], in_offset=None),
                       reads=["xbt%d" % i2, "slots"], writes=["xg_scr"])
        phase()
        wbuf = []
        e0 = XT[:, :, :].rearrange("p a b -> p (a b)")
        wbuf.append((e0[:, 0:8192].rearrange("p (a b) -> p a b", a=16), e0[:, 8192:16384].rearrange("p (a b) -> p a b", a=16),
                     e0[:, 16384:24576].rearrange("p (a b) -> p a b", a=4)))
        wbuf.append((carve([128, 16, 512], BF16), carve([128, 16, 512], BF16), carve([128, 4, D], BF16)))
        xg = [carve([128, D], BF16) for _ in range(2)]
        xgT = carve([128, 16, CAP], BF16)
        hTm = carve([128, 4, CAP], BF16)
        sg = carve([128, CAP], F32)
        ysb = [carve([128, D], F32) for _ in range(2)]
        psT = [pS0[:, :].bitcast(BF16), pS1[:, :].bitcast(BF16)]
        for ex in range(NEXP):
            wg, wu, wd = wbuf[ex % 2]
            wn = "wexp%d" % (ex % 2)
            P.dma("pool", wg, W["w_gate"][l, ex].rearrange("(kc p) f -> p kc f", p=128), writes=[wn])
            P.dma("pool", wu, W["w_up"][l, ex].rearrange("(kc p) f -> p kc f", p=128), writes=[wn])
            P.dma("pool", wd, W["w_down"][l, ex].rearrange("(kc p) f -> p kc f", p=128), writes=[wn])
            for s_ in range(CAP // 128):
                r0 = ex * CAP + s_ * 128
                P.dma("sp", xg[s_], xg_scr[r0:r0 + 128, :], reads=["xg_scr"], writes=["xg%d" % s_])
                for g in range(2):
                    pt_ = psT[g]
                    pnm = pn((pS0, pS1)[g])
                    P.op("pe", (lambda pt_=pt_, g=g, s_=s_: lambda e: [e.transpose(out=pt_[:, j * 128:(j + 1) * 128], in_=xg[s_][:, (g * 8 + j) * 128:(g * 8 + j + 1) * 128], identity=identb[:]) for j in range(8)][-1])(),
                         reads=["xg%d" % s_, "identb"], writes=[pnm])
                    P.op("act", lambda e, pt_=pt_, g=g, s_=s_: e.activation(out=xgT[:, g * 8:(g + 1) * 8, s_ * 128:(s_ + 1) * 128], in_=pt_[:, 0:1024].rearrange("p (a b) -> p a b", a=8), func=AF.Copy),
                         writes=[pnm, "xgT"])
            for fc in range(4):
                mms = [(pA[:, 0:CAP], wg[:, kc, fc * 128:(fc + 1) * 128], xgT[:, kc, :], kc == 0, kc == 15) for kc in range(16)]
                P.op("pe", pe_group(mms), reads=[wn, "xgT"], writes=[pn(pA)])
                mms = [(pB[:, 0:CAP], wu[:, kc, fc * 128:(fc + 1) * 128], xgT[:, kc, :], kc == 0, kc == 15) for kc in range(16)]
                P.op("pe", pe_group(mms), reads=[wn, "xgT"], writes=[pn(pB)])
                P.op("act", lambda e: e.activation(out=sg, in_=pA[:, 0:CAP], func=AF.Silu), writes=[pn(pA), "sg"])
                P.op("dve", lambda e, fc=fc: e.tensor_tensor(out=hTm[:, fc, :], in0=sg, in1=pB[:, 0:CAP], op=ALU.mult), reads=["sg"], writes=[pn(pB), "hTm"])
            for s_ in range(CAP // 128):
                for cc in range(4):
                    pb = (pO0, pO1, pM0, pM1)[cc]
                    mms = [(pb[:, :], hTm[:, kc, s_ * 128:(s_ + 1) * 128], wd[:, kc, cc * 512:(cc + 1) * 512], kc == 0, kc == 3) for kc in range(4)]
                    P.op("pe", pe_group(mms), reads=[wn, "hTm"], writes=[pn(pb)])
                    if cc % 2 == 0:
                        P.op("act", lambda e, pb=pb, cc=cc, s_=s_: e.activation(out=ysb[s_][:, cc * 512:(cc + 1) * 512], in_=pb[:, :], func=AF.Copy), writes=[pn(pb), "ysb%d" % s_])
                    else:
                        P.op("dve", lambda e, pb=pb, cc=cc, s_=s_: e.tensor_copy(out=ysb[s_][:, cc * 512:(cc + 1) * 512], in_=pb[:, :]), writes=[pn(pb), "ysb%d" % s_])
                r0 = ex * CAP + s_ * 128
                P.dma("sp", y_scr[r0:r0 + 128, :], ysb[s_], reads=["ysb%d" % s_], writes=["y_scr"])
        phase()
        lng = carve([128, D], F32)
        lnb = carve([128, D], F32)
        P.dma("sp", lng, W["ln2_g"][l:l + 1, :].broadcast_to([128, D]), writes=["lng"])
        P.dma("sp", lnb, W["ln2_b"][l:l + 1, :].broadcast_to([128, D]), writes=["lnb"])
        y0 = [carve([128, D], F32) for _ in range(2)]
        y1 = [carve([128, D], F32) for _ in range(2)]
        x1t = [carve([128, D], F32) for _ in range(2)]
        st = carve([128, 16], F32)
        dst = out_d if last else xres_scr
        for t in range(NT):
            i2 = t % 2
            rows = slice(t * 128, (t + 1) * 128)
            P.dma("sp", x1t[i2], x1_scr[rows, :], reads=["x1_scr"], writes=["x1t%d" % i2])
            P.idma(lambda g, t=t, i2=i2: g.indirect_dma_start(out=y0[i2], out_offset=None, in_=y_scr[:, :], in_offset=bass.IndirectOffsetOnAxis(ap=slots[:, t, 0:1], axis=0)), reads=["y_scr", "slots"], writes=["y0%d" % i2])
            P.idma(lambda g, t=t, i2=i2: g.indirect_dma_start(out=y1[i2], out_offset=None, in_=y_scr[:, :], in_offset=bass.IndirectOffsetOnAxis(ap=slots[:, t, 1:2], axis=0)), reads=["y_scr", "slots"], writes=["y1%d" % i2])
            P.op("dve", lambda e, t=t, i2=i2: e.tensor_scalar(out=y0[i2], in0=y0[i2], scalar1=gatew[:, t, 0:1], scalar2=None, op0=ALU.mult), reads=["gatew"], writes=["y0%d" % i2])
            P.op("dve", lambda e, t=t, i2=i2: e.scalar_tensor_tensor(out=y0[i2], in0=y1[i2], scalar=gatew[:, t, 1:2], in1=y0[i2], op0=ALU.mult, op1=ALU.add), reads=["gatew", "y1%d" % i2], writes=["y0%d" % i2])
            if stage != "full" and l == 0:
                P.dma("sp", dbg2_d[rows, :], y0[i2], reads=["y0%d" % i2], writes=["dbg2"])
            P.op("dve", lambda e, i2=i2: e.scalar_tensor_tensor(out=y0[i2], in0=x1t[i2], scalar=ALPHA, in1=y0[i2], op0=ALU.mult, op1=ALU.add), reads=["x1t%d" % i2], writes=["y0%d" % i2])
            y, yn, out, outn = y0[i2], "y0%d" % i2, x1t[i2], "x1t%d" % i2
            P.op("dve", lambda e, y=y: e.tensor_reduce(out=st[:, 0:1], in_=y, axis=AX.X, op=ALU.add), reads=[yn], writes=["st"])
            P.op("dve", lambda e: e.tensor_scalar(out=st[:, 1:2], in0=st[:, 0:1], scalar1=-1.0 / D, scalar2=None, op0=ALU.mult), writes=["st"])
            P.op("act", lambda e, y=y: e.activation(out=y, in_=y, func=AF.Identity, bias=st[:, 1:2], scale=1.0), reads=["st"], writes=[yn])
            P.op("dve", lambda e: e.memset(st[:, 2:3], 0.0), writes=["st"])
            P.op("act", lambda e, y=y, out=out: e.activation(out=out, in_=y, func=AF.Square, accum_out=st[:, 2:3]), reads=[yn], writes=[outn, "st"])
            P.op("act", lambda e: e.activation(out=st[:, 3:4], in_=st[:, 2:3], func=AF.Ln, scale=1.0 / D, bias=epsr[:, 1:2]), reads=["epsr"], writes=["st"])
            P.op("act", lambda e: e.activation(out=st[:, 3:4], in_=st[:, 3:4], func=AF.Exp, scale=-0.5), writes=["st"])
            P.op("dve", lambda e, y=y, out=out: e.scalar_tensor_tensor(out=out, in0=y, scalar=st[:, 3:4], in1=lng, op0=ALU.mult, op1=ALU.mult), reads=[yn, "st", "lng"], writes=[outn])
            P.op("pool", lambda e, out=out: e.tensor_tensor(out=out, in0=out, in1=lnb, op=ALU.add), reads=["lnb"], writes=[outn])
            P.dma("sp", dst[rows, :], out, reads=[outn], writes=["dst"])

    x_src = x_d
    for l in range(n_layers):
        last = (l == n_layers - 1)
        if layer(l, x_src, last) == "stop":
            break
        if stage in ("ln1", "odbg"):
            break
        moe(l, last)
        x_src = xres_scr
    P.barrier()
    P.emit()
    return nc


_CONSTS = None


def kernel(**inputs):
    global _CONSTS
    if _CONSTS is None:
        _CONSTS = make_consts()
    nc = build()
    x = np.ascontiguousarray(inputs["x"], dtype=np.float32)
    shared = {k: np.ascontiguousarray(inputs[k], dtype=np.float32) for k in W_SHAPES if k != "w_in"}
    shared["w_in"] = relayout_w_in(np.asarray(inputs["w_in"], dtype=np.float32))
    for k, v in _CONSTS.items():
        shared["c_" + k] = np.ascontiguousarray(v.reshape(CONST_SHAPES[k]), dtype=np.float32)
    in_maps = []
    for b in range(4):
        m = dict(shared)
        m["x"] = x[b]
        in_maps.append(m)
    res = run_bass_kernel_spmd(nc, in_maps, core_ids=list(range(4)))
    return np.stack([np.asarray(r["out"], dtype=np.float32) for r in res.results], axis=0)
```

```python
import contextlib
import numpy as np
import concourse.bass as bass
import concourse.mybir as mybir
from concourse.bass_utils import run_bass_kernel_spmd

F32 = mybir.dt.float32
BF16 = mybir.dt.bfloat16
I32 = mybir.dt.int32
ALU = mybir.AluOpType
AF = mybir.ActivationFunctionType
AX = mybir.AxisListType

S = 2048
D = 2048
NT = 16
DEPTH = 2
IN_COLS = 4434
CAP = 256
NEXP = 32
THETA = 500000.0
ALPHA = (2 * DEPTH) ** 0.25
LN_EPS = 1e-5
RMS_EPS = 1e-6
C_QLAT, C_KVLAT, C_KROPE = 0, 384, 512
C_DQ, C_DK, C_DV = 576, 1344, 2112
C_NQ = 2880
C_NKC, C_NVC, C_NKS, C_NVS, C_NKW, C_NVW = 3648, 3776, 3904, 4032, 4160, 4288
C_GATE = 4416


class Prog:
    CENG = ("pe", "act", "dve", "pool")
    DMAQ = ("sp", "act", "pool")
    RING = 8

    def __init__(self, nc):
        self.nc = nc
        self.es = contextlib.ExitStack()
        self.streams = {e: [] for e in ("pe", "act", "dve", "pool", "sp")}
        self.cnt = {e: 0 for e in self.CENG}
        self.sems = {}
        for e in self.CENG:
            self.sems[e] = self.es.enter_context(nc.semaphore("s_" + e))
        self.dsems = {}
        self.dcnt = {}
        for q in self.DMAQ:
            self.dcnt[q] = 0
            for i in range(self.RING):
                self.dsems[(q, i)] = self.es.enter_context(nc.semaphore("d_%s%d" % (q, i)))
        self.seen = {s: {} for s in self.streams}
        self.tiles = {}

    def sb(self, name, shape, dt):
        return self.es.enter_context(self.nc.sbuf_tensor(name, list(shape), dt))

    def ps(self, name, shape, dt=F32):
        return self.es.enter_context(self.nc.psum_tensor(name, list(shape), dt))

    def _need(self, stream, ev, waits, is_dma=False):
        if ev is None:
            return
        key, val = ev
        if key == stream and key == "pe" and not is_dma:
            return
        if self.seen[stream].get(key, 0) >= val:
            return
        if waits.get(key, 0) < val:
            waits[key] = val

    def _deps(self, stream, reads, writes, is_dma=False):
        waits = {}
        for t in reads:
            st = self.tiles.setdefault(t, {"w": None, "r": []})
            self._need(stream, st["w"], waits, is_dma)
        for t in writes:
            st = self.tiles.setdefault(t, {"w": None, "r": []})
            self._need(stream, st["w"], waits, is_dma)
            for ev in st["r"]:
                self._need(stream, ev, waits, is_dma)
        return waits

    def _commit(self, ev, reads, writes):
        for t in reads:
            if t in writes:
                continue
            r = self.tiles[t]["r"]
            r.append(ev)
            if len(r) > 48:
                best = {}
                for k, v in r:
                    if best.get(k, 0) < v:
                        best[k] = v
                self.tiles[t]["r"] = list(best.items())
        for t in writes:
            self.tiles[t]["w"] = ev
            self.tiles[t]["r"] = []

    def _sem(self, key):
        return self.sems[key] if key in self.sems else self.dsems[key]

    def _emit_waits(self, stream, waits):
        for key, val in waits.items():
            self.streams[stream].append(("w", self._sem(key), val))
            self.seen[stream][key] = val

    def op(self, eng, fn, reads=(), writes=()):
        reads, writes = tuple(reads), tuple(writes)
        self._emit_waits(eng, self._deps(eng, reads, writes))
        self.cnt[eng] += 1
        ev = (eng, self.cnt[eng])
        self.streams[eng].append(("c", fn, self.sems[eng]))
        self._commit(ev, reads, writes)
        return ev

    def _dma_common(self, q, reads, writes):
        waits = self._deps(q, reads, writes, True)
        k = self.dcnt[q]
        self.dcnt[q] += 1
        key = (q, k % self.RING)
        tgt = 16 * (k // self.RING + 1)
        if tgt > 16:
            prev = tgt - 16
            if self.seen[q].get(key, 0) < prev and waits.get(key, 0) < prev:
                waits[key] = prev
        self._emit_waits(q, waits)
        return key, tgt

    def dma(self, q, out, in_, reads=(), writes=(), **kw):
        reads, writes = tuple(reads), tuple(writes)
        key, tgt = self._dma_common(q, reads, writes)
        self.streams[q].append(("d", out, in_, kw, self.dsems[key]))
        self._commit((key, tgt), reads, writes)

    def idma(self, fn, reads=(), writes=()):
        reads, writes = tuple(reads), tuple(writes)
        key, tgt = self._dma_common("pool", reads, writes)
        self.streams["pool"].append(("i", fn, self.dsems[key]))
        self._commit((key, tgt), reads, writes)

    def barrier(self):
        cur = {}
        for e in self.CENG:
            if self.cnt[e] > 0:
                cur[e] = self.cnt[e]
        for q in self.DMAQ:
            k = self.dcnt[q]
            for i in range(self.RING):
                n = (k - i + self.RING - 1) // self.RING if k > i else 0
                if n > 0:
                    cur[(q, i)] = 16 * n
        for s in self.streams:
            waits = {}
            for key, val in cur.items():
                if self.seen[s].get(key, 0) < val:
                    waits[key] = val
            self._emit_waits(s, waits)
        self.tiles = {}

    def emit(self):
        nc = self.nc
        streams = self.streams

        def run(e, lst):
            for it in lst:
                k = it[0]
                if k == "w":
                    e.wait_ge(it[1], it[2])
                elif k == "c":
                    it[1](e).then_inc(it[2], 1)
                elif k == "d":
                    e.dma_start(out=it[1], in_=it[2], **it[3]).then_inc(it[4], 16)
                elif k == "i":
                    it[1](e).then_inc(it[2], 16)

        with nc.Block() as block:
            @block.sync
            def _(e):
                run(e, streams["sp"])

            @block.tensor
            def _(e):
                run(e, streams["pe"])

            @block.scalar
            def _(e):
                run(e, streams["act"])

            @block.vector
            def _(e):
                run(e, streams["dve"])

            @block.gpsimd
            def _(e):
                run(e, streams["pool"])
        self.es.close()


def make_consts():
    c = {}
    c["ident"] = np.eye(128, dtype=np.float32)
    pos = np.arange(S, dtype=np.float32)

    def rope_tab(rot, p):
        half = rot // 2
        inv = (np.float32(THETA) ** (-np.arange(half, dtype=np.float32) / np.float32(half))).astype(np.float32)
        ang = (p.astype(np.float32)[None, :] * inv[:, None]).astype(np.float32)
        cos = np.cos(ang.astype(np.float64)).astype(np.float32)
        sin = np.sin(ang.astype(np.float64)).astype(np.float32)
        t = np.zeros((rot, 2, p.shape[0]), np.float32)
        t[:half, 0], t[half:, 0] = cos, cos
        t[:half, 1], t[half:, 1] = -sin, sin
        return t

    c["cs32"] = rope_tab(32, pos)
    c["cs64"] = rope_tab(64, pos)
    kc = np.zeros((32, 2, 128), np.float32)
    kc[:, :, :127] = rope_tab(32, (np.arange(127) * 16 + 31).astype(np.float32))
    c["cskc"] = kc

    def perm(n):
        m = np.zeros((n, n), np.float32)
        h = n // 2
        for j in range(n):
            m[(j + h) % n, j] = 1.0
        return m

    c["pm32"] = perm(32)
    c["pm64"] = perm(64)
    kk = np.arange(128)[:, None]
    qq = np.arange(128)[None, :]
    md = np.zeros((128, 16, 128), np.float32)
    for delta in range(16):
        d = 128 * delta + qq - kk
        m = ((d >= 0) & (d <= 128)).astype(np.float32) + ((d >= 0) & (d % 4 == 0) & (d <= 512)).astype(np.float32) \
            + ((d >= 0) & (d % 16 == 0)).astype(np.float32)
        md[:, 15 - delta, :] = m
    c["mdil"] = md.reshape(128, 2048)
    c["tri"] = (kk <= qq).astype(np.float32)
    c["upp"] = (kk > qq).astype(np.float32)
    cc = np.arange(128)[:, None]
    vcm = ((16 * cc + 31) <= np.arange(S)[None, :]).astype(np.float32)
    vcm[127] = 0
    c["vcm"] = vcm
    cs = np.arange(128) * 16
    ss = np.arange(32) * 64
    cover = ((cs[:, None] < ss[None, :] + 64) & (cs[:, None] + 32 > ss[None, :])).astype(np.float32)
    cover[127] = 0
    c["cover"] = cover
    p = np.arange(S)
    jj = np.arange(32)[None, :]
    qblk = (p // 64)[:, None]
    valid = (ss[None, :] <= p[:, None])
    forced = (jj == 0) | (jj == qblk) | (jj == qblk - 1)
    selA = (valid & ~forced).astype(np.float32)
    selB = np.where(valid, np.where(forced, 1e4, 0.0), -1.0).astype(np.float32)
    c["selA"] = selA.reshape(16, 128, 32).transpose(1, 0, 2).copy()
    c["selB"] = selB.reshape(16, 128, 32).transpose(1, 0, 2).copy()
    E = np.zeros((32, 16, 128), np.float32)
    for kt in range(16):
        E[2 * kt, kt, :64] = 1
        E[2 * kt + 1, kt, 64:] = 1
    c["emat"] = E.reshape(32, 2048)
    c["ltri"] = (kk < qq).astype(np.float32)
    c["ecap"] = np.tile((np.arange(32) * CAP).astype(np.float32)[None, :], (128, 1))
    return c


CONST_SHAPES = {"ident": (128, 128), "cs32": (32, 2, 2048), "cs64": (64, 2, 2048), "cskc": (32, 2, 128),
                "pm32": (32, 32), "pm64": (64, 64), "mdil": (128, 2048), "tri": (128, 128), "upp": (128, 128),
                "vcm": (128, 2048), "cover": (128, 32), "selA": (128, 16, 32), "selB": (128, 16, 32),
                "emat": (32, 2048), "ltri": (128, 128), "ecap": (128, 32)}

W_SHAPES = {"w_in": (2, 36, 128, 2048), "q_lat_norm": (2, 384), "w_q_up": (2, 384, 768), "kv_lat_norm": (2, 128),
            "w_kv_up": (2, 128, 1024), "cmp_pos_k": (2, 32, 128), "cmp_w1_k": (2, 4096, 256),
            "cmp_w2_k": (2, 256, 128), "cmp_pos_v": (2, 32, 128), "cmp_w1_v": (2, 4096, 256),
            "cmp_w2_v": (2, 256, 128), "w_out": (2, 2048, 2048), "ln1_g": (2, 2048), "ln1_b": (2, 2048),
            "w_grp": (2, 2048, 4), "w_exp": (2, 2048, 32), "w_gate": (2, 32, 2048, 512),
            "w_up": (2, 32, 2048, 512), "w_down": (2, 32, 512, 2048), "ln2_g": (2, 2048), "ln2_b": (2, 2048)}


W_CHUNKS = ([(C_QLAT + 128 * c, 128) for c in range(3)] + [(C_KVLAT, 128), (C_KROPE, 64)]
            + [(C_DQ + 128 * h, 128) for h in range(6)] + [(C_DK + 128 * h, 128) for h in range(6)]
            + [(C_DV + 128 * h, 128) for h in range(6)] + [(C_GATE, 18), (C_NKC, 128), (C_NVC, 128)]
            + [(C_NQ + 128 * h, 128) for h in range(6)] + [(C_NKS, 128), (C_NKW, 128), (C_NVS, 128), (C_NVW, 128)])
W_CHUNK_IDX = {c: i for i, c in enumerate(W_CHUNKS)}


def relayout_w_in(w_in):
    L = w_in.shape[0]
    out = np.zeros((L, len(W_CHUNKS), 128, 16, 128), np.float32)
    for i, (c0, n) in enumerate(W_CHUNKS):
        out[:, i, :, :, 0:n] = w_in[:, :, c0:c0 + n].reshape(L, 16, 128, n).transpose(0, 2, 1, 3)
    return out.reshape(L, len(W_CHUNKS), 128, 2048)


def pe_group(mms):
    def fn(e):
        ins = None
        for (o, l, r, st, sp) in mms:
            ins = e.matmul(o, lhsT=l, rhs=r, start=st, stop=sp)
        return ins
    return fn


def build(n_layers=DEPTH, stage="full"):
    nc = bass.Bass("TRN2", target_bir_lowering=False)

    def din(name, shape, dt=F32):
        return nc.dram_tensor(name, list(shape), dt, kind="ExternalInput").ap()

    def dscr(name, shape, dt):
        return nc.dram_tensor(name, list(shape), dt, kind="Internal").ap()

    x_d = din("x", [S, D])
    W = {k: din(k, v) for k, v in W_SHAPES.items()}
    CD = {k: din("c_" + k, v) for k, v in CONST_SHAPES.items()}
    out_d = nc.dram_tensor("out", [S, D], F32, kind="ExternalOutput").ap()
    dbg_d = dbg2_d = None
    if stage != "full":
        dbg_d = nc.dram_tensor("dbg", [S, D], F32, kind="ExternalOutput").ap()
        dbg2_d = nc.dram_tensor("dbg2", [S, D], F32, kind="ExternalOutput").ap()
        dbg3_d = nc.dram_tensor("dbg3", [8, 128, S], BF16, kind="ExternalOutput").ap()

    o_scr = dscr("o_scr", [S, D], BF16)
    nq_scr = dscr("nq_scr", [6, 128, S], BF16)
    ocmp_scr = dscr("ocmp_scr", [6, S, 128], F32)
    x1_scr = dscr("x1_scr", [S, D], F32)
    x1b_scr = dscr("x1b_scr", [S, D], BF16)
    xres_scr = dscr("xres_scr", [S, D], F32)
    xg_scr = dscr("xg_scr", [NEXP * CAP, D], BF16)
    y_scr = dscr("y_scr", [NEXP * CAP, D], F32)

    P = Prog(nc)
    XT = P.sb("XT", [128, 16, S], BF16)
    ARENA = P.sb("ARENA", [128, 30 * 1024], F32)
    ident = P.sb("ident", [128, 128], F32)
    identb = P.sb("identb", [128, 128], BF16)
    cs32 = P.sb("cs32", [32, 2, S], F32)
    pm32 = P.sb("pm32", [32, 32], BF16)
    pm64 = P.sb("pm64", [64, 64], BF16)
    onesb = P.sb("onesb", [128, 128], BF16)
    tri = P.sb("tri", [128, 128], BF16)
    upp = P.sb("upp", [128, 128], BF16)
    ltri = P.sb("ltri", [128, 128], BF16)
    ecap = P.sb("ecap", [128, 32], F32)
    gates = P.sb("gates", [128, 16, 18], F32)
    slots = P.sb("slots", [128, 16, 2], I32)
    gatew = P.sb("gatew", [128, 16, 2], F32)
    mk = P.sb("mk", [128, 16, 32], BF16)
    oh = P.sb("oh", [128, 16, 2, 32], BF16)
    PS = [P.ps("ps%d" % i, [128, 512], F32) for i in range(8)]
    pA, pB, pS0, pS1, pO0, pO1, pM0, pM1 = PS
    PN = {id(t): "ps%d" % i for i, t in enumerate(PS)}

    def pn(t):
        return PN[id(t)]

    arena_off = [0]

    def carve(shape, dt):
        n = int(np.prod(shape[1:]))
        words = n if dt in (F32, I32) else (n + 1) // 2
        words = (words + 15) // 16 * 16
        o = arena_off[0]
        arena_off[0] += words
        assert arena_off[0] <= 30 * 1024, ("arena overflow", arena_off[0])
        v = ARENA[0:shape[0], o:o + words]
        if dt != F32:
            v = v.bitcast(dt)
        v = v[:, 0:n]
        if len(shape) == 3:
            v = v.rearrange("p (a b) -> p a b", a=shape[1])
        elif len(shape) == 4:
            v = v.rearrange("p (a b c) -> p a b c", a=shape[1], b=shape[2])
        return v

    def phase():
        P.barrier()
        arena_off[0] = 0

    P.dma("sp", ident[:], CD["ident"], writes=["ident"])
    P.dma("pool", identb[:], CD["ident"], writes=["identb"])
    P.dma("sp", cs32[:], CD["cs32"], writes=["cs32"])
    P.dma("pool", pm32[:], CD["pm32"], writes=["pm32"])
    P.dma("pool", pm64[:], CD["pm64"], writes=["pm64"])
    P.dma("pool", tri[:], CD["tri"], writes=["tri"])
    P.dma("pool", upp[:], CD["upp"], writes=["upp"])
    P.dma("pool", ltri[:], CD["ltri"], writes=["ltri"])
    P.dma("sp", ecap[:], CD["ecap"], writes=["ecap"])
    P.op("dve", lambda e: e.memset(onesb[:], 1.0), writes=["onesb"])
    epsr = P.sb("epsr", [128, 2], F32)
    P.op("dve", lambda e: e.memset(epsr[:, 0:1], RMS_EPS), writes=["epsr"])
    P.op("dve", lambda e: e.memset(epsr[:, 1:2], LN_EPS), writes=["epsr"])

    arena_off[0] = 0
    ztile = carve([128, 8192], BF16)
    P.op("pool", lambda e: e.memset(ztile, 0.0), writes=["ztile"])
    xg_flat = xg_scr.rearrange("(a p) d -> a p d", p=128)
    for a in range(NEXP * CAP // 128):
        P.dma("sp", xg_flat[a], ztile[:, 0:D], reads=["ztile"], writes=["xg_scr"])

    def layer(l, x_src, last):
        phase()
        QTS = [carve([128, S], BF16) for _ in range(2)]
        QR = carve([64, S], BF16)
        KT = [carve([128, S], BF16) for _ in range(2)]
        KR = carve([64, S], BF16)
        VA = carve([128, 16, 2, 129], BF16)
        QLT = carve([128, 3, S], BF16)
        KVLT = carve([128, S], BF16)
        SCR16 = carve([128, 4096], F32)
        WST = [carve([128, 16, 128], BF16) for _ in range(3)]
        PT = [carve([128, 512], BF16) for _ in range(5)]
        mdil = carve([128, S], BF16)
        vcm = mdil
        emat = mdil[0:32, :]
        nbT = carve([32, S], BF16)
        selA = carve([128, 16, 32], F32)
        selB = carve([128, 16, 32], F32)
        imp = carve([128, 16, 32], F32)
        cover = carve([128, 32], F32)
        rt12 = carve([128, 1024], F32)
        rt1 = rt12[0:64, 0:512]
        rt2 = rt12[0:64, 512:1024]
        sqb = carve([128, 3, 512], BF16)
        rstd = carve([128, 512], F32)
        wqu = carve([128, 3, 768], BF16)
        wkvu = carve([128, 1024], BF16)
        qg = carve([128, 4], F32)
        xin = [SCR16[:, 0:2048], SCR16[:, 2048:4096]]
        fin = carve([128, 8], F32)
        ofin = [carve([128, 128], BF16) for _ in range(2)]
        onsa = QLT[:, :, :].rearrange("p a b -> p (a b)")[:, 0:4096].bitcast(F32).rearrange("p (a b) -> p a b", a=16)
        ocl = [carve([128, 128], F32) for _ in range(2)]
        KCT = carve([128, 128], BF16)
        VCA = carve([128, 161], BF16)
        peT = carve([128, 32], F32)
        XPE = QLT[:, :, :].rearrange("p a b -> p (a b)")[:, 0:32 * 127].rearrange("p (a b) -> p a b", a=32)
        hT = carve([128, 2, 127], BF16)
        w2 = carve([128, 2, 128], BF16)
        cskc = carve([32, 2, 128], F32)
        hsc = [carve([128, 127], F32) for _ in range(3)]
        selw = rt12[:, :].rearrange("p (a b) -> p a b", a=32)
        selr = carve([128, 32], F32)

        P.dma("pool", mdil, CD["mdil"], writes=["msk"])
        P.dma("sp", selA, CD["selA"], writes=["selA"])
        P.dma("sp", selB, CD["selB"], writes=["selB"])
        P.dma("sp", cover, CD["cover"], writes=["cover"])
        P.dma("sp", cskc, CD["cskc"], writes=["cskc"])
        cs64 = SCR16[0:64, :].rearrange("p (a b) -> p a b", a=2)
        P.dma("pool", wqu, W["w_q_up"][l].rearrange("(kc p) n -> p kc n", p=128), writes=["wqu"])
        P.dma("pool", wkvu, W["w_kv_up"][l], writes=["wkvu"])
        P.dma("sp", qg[:, 0:3], W["q_lat_norm"][l].rearrange("(kc p) -> p kc", p=128), writes=["qg"], allow_slow_non_contiguous=True)
        P.dma("sp", qg[:, 3:4], W["kv_lat_norm"][l].rearrange("(kc p) -> p kc", p=128), writes=["qg"], allow_slow_non_contiguous=True)
        P.op("dve", lambda e: e.memset(VA[:, :, :, 128:129], 1.0), writes=["va0", "va1"])
        P.op("dve", lambda e: e.memset(imp, 0.0), writes=["imp"])

        for t in range(NT):
            xt_ = xin[t % 2]
            P.dma("sp", xt_, x_src[t * 128:(t + 1) * 128, :], writes=["xin%d" % (t % 2)])
            for g in range(4):
                pb = (pA, pB)[(t * 4 + g) % 2]
                P.op("pe", (lambda pb=pb, xt_=xt_, g=g: lambda e: [e.transpose(out=pb[:, j * 128:(j + 1) * 128], in_=xt_[:, (g * 4 + j) * 128:(g * 4 + j + 1) * 128], identity=ident[:]) for j in range(4)][-1])(),
                     reads=["xin%d" % (t % 2), "ident"], writes=[pn(pb)])
                eng = "act" if g % 2 == 0 else "dve"
                dst = XT[:, g * 4:(g + 1) * 4, t * 128:(t + 1) * 128]
                src = pb[:, :].rearrange("p (a b) -> p a b", a=4)
                if eng == "act":
                    P.op("act", lambda e, dst=dst, src=src: e.activation(out=dst, in_=src, func=AF.Copy), writes=[pn(pb), "XT"])
                else:
                    P.op("dve", lambda e, dst=dst, src=src: e.tensor_copy(out=dst, in_=src), writes=[pn(pb), "XT"])

        P.dma("sp", cs64, CD["cs64"], writes=["scr16", "xin0", "xin1"])
        wst_i = [0]
        pb_i = [0]

        def next_pb():
            pb_i[0] += 1
            return (pA, pB)[pb_i[0] % 2]

        def load_w(col0, ncols):
            s = wst_i[0] % 3
            wst_i[0] += 1
            P.dma("pool", WST[s], W["w_in"][l, W_CHUNK_IDX[(col0, ncols)]].rearrange("p (kc n) -> p kc n", kc=16),
                  writes=["wst%d" % s])
            return s

        def proj_fm(col0, ncols, evac):
            s = load_w(col0, ncols)
            for tc in range(4):
                pb = next_pb()
                mms = [(pb[0:ncols, :], WST[s][:, kc, 0:ncols], XT[:, kc, tc * 512:(tc + 1) * 512], kc == 0, kc == 15) for kc in range(16)]
                P.op("pe", pe_group(mms), reads=["wst%d" % s, "XT"], writes=[pn(pb)])
                evac(pb, tc)

        def proj_tm(col0, ncols, evac):
            s = load_w(col0, ncols)
            for kt in range(NT):
                pb = next_pb()
                mms = [(pb[:, 0:ncols], XT[:, kc, kt * 128:(kt + 1) * 128], WST[s][:, kc, 0:ncols], kc == 0, kc == 15) for kc in range(16)]
                P.op("pe", pe_group(mms), reads=["wst%d" % s, "XT"], writes=[pn(pb)])
                evac(pb, kt)

        def rope_evac(dst, dname, nrows, R, cs, csname, pm, pmname):
            def ev(pb, tc, ncol=512, c0=None):
                c0 = tc * 512 if c0 is None else c0
                sl = slice(c0, c0 + ncol)
                tn = "%s.%d" % (dname, tc)
                P.op("act", lambda e: e.activation(out=dst[0:nrows, sl], in_=pb[0:nrows, 0:ncol], func=AF.Copy), writes=[pn(pb), tn])
                pm_ = pM0
                P.op("pe", lambda e: e.matmul(pm_[0:R, 0:ncol], lhsT=pm[0:R, 0:R], rhs=dst[0:R, sl], start=True, stop=True),
                     reads=[tn, pmname], writes=[pn(pm_)])
                P.op("dve", lambda e: e.tensor_tensor(out=rt1[0:R, 0:ncol], in0=dst[0:R, sl], in1=cs[0:R, 0, sl], op=ALU.mult),
                     reads=[tn, csname], writes=["rt1"])
                P.op("dve", lambda e: e.tensor_tensor(out=rt2[0:R, 0:ncol], in0=pm_[0:R, 0:ncol], in1=cs[0:R, 1, sl], op=ALU.mult),
                     reads=[csname], writes=["rt2", pn(pm_)])
                P.op("pool", lambda e: e.tensor_tensor(out=dst[0:R, sl], in0=rt1[0:R, 0:ncol], in1=rt2[0:R, 0:ncol], op=ALU.add),
                     reads=["rt1", "rt2"], writes=[tn])
            return ev

        def copy_evac(dst, dname, nrows):
            def ev(pb, tc):
                sl = slice(tc * 512, (tc + 1) * 512)
                P.op("act", lambda e: e.activation(out=dst[0:nrows, sl], in_=pb[0:nrows, :], func=AF.Copy),
                     writes=[pn(pb), "%s.%d" % (dname, tc)])
            return ev

        def v_evac(slot, ncols=128):
            def ev(pb, kt):
                P.op("dve", lambda e: e.tensor_copy(out=VA[:, kt, slot, 0:ncols], in_=pb[:, 0:ncols]), writes=[pn(pb), "va%d" % slot])
            return ev

        def names4(n):
            return ["%s.%d" % (n, i) for i in range(4)]

        pt_i = [0]
        ps_i = [0]
        po_i = [0]

        def attn(qparts, kparts, vslot, scale, kts_fn, mask_fn, W_out, finalize, nk=128, bias=False, vsrc=None, vname=None):
            qreads = [n for (_, _, nm) in qparts for n in nm]
            kreads = [n for (_, _, nm) in kparts for n in nm]
            vname_ = vname or ("va%d" % vslot)
            items = []
            for qt in range(NT):
                kts = kts_fn(qt)
                po = (pO0, pO1)[po_i[0] % 2]
                po_i[0] += 1
                groups = [kts[i:i + 4] for i in range(0, len(kts), 4)]
                for gi, grp in enumerate(groups):
                    psb = (pS0, pS1, pM1, pM0)[ps_i[0] % 4]
                    ps_i[0] += 1
                    pts = pt_i[0] % 5
                    pt_i[0] += 1
                    items.append((qt, gi, grp, len(groups), po, psb, pts))

            def stage1(it):
                qt, gi, grp, ng, po, psb, pts = it
                qs = slice(qt * 128, (qt + 1) * 128)
                mms = []
                for j, kt in enumerate(grp):
                    o = psb[0:nk, j * 128:(j + 1) * 128]
                    np_ = len(qparts)
                    for pi in range(np_):
                        qa, K, _ = qparts[pi]
                        ka, _, _ = kparts[pi]
                        mms.append((o, ka[0:K, kt * 128:kt * 128 + nk], qa[0:K, qs], pi == 0, (pi == np_ - 1) and not bias))
                    if bias:
                        mms.append((o, emat[0:32, kt * 128:(kt + 1) * 128], nbT[0:32, qs], False, True))
                P.op("pe", pe_group(mms), reads=qreads + kreads + (["msk", "nbT"] if bias else []), writes=[pn(psb)])
                n = len(grp) * 128
                ptn = "pt%d" % pts
                P.op("act", lambda e: e.activation(out=PT[pts][0:nk, 0:n], in_=psb[0:nk, 0:n], func=AF.Exp, scale=scale),
                     writes=[pn(psb), ptn])
                for (eng, fn, rd) in mask_fn(qt, grp, PT[pts]):
                    P.op(eng, fn, reads=rd, writes=[ptn])

            def stage2(it):
                qt, gi, grp, ng, po, psb, pts = it
                ptn = "pt%d" % pts
                mms = []
                for j, kt in enumerate(grp):
                    vv = VA[0:nk, kt, vslot, 0:W_out] if vsrc is None else vsrc
                    mms.append((po[:, 0:W_out], PT[pts][0:nk, j * 128:(j + 1) * 128], vv,
                                gi == 0 and j == 0, gi == ng - 1 and j == len(grp) - 1))
                P.op("pe", pe_group(mms), reads=[ptn, vname_], writes=[pn(po)])
                if gi == ng - 1:
                    finalize(qt, po)

            SK = 3
            for i in range(len(items) + SK):
                if i < len(items):
                    stage1(items[i])
                if i >= SK:
                    stage2(items[i - SK])

        def causal_mask(qt, grp, pt):
            ops = []
            if grp[-1] == qt:
                j = len(grp) - 1
                ops.append(("dve", lambda e, j=j, pt=pt: e.tensor_tensor(out=pt[:, j * 128:(j + 1) * 128], in0=pt[:, j * 128:(j + 1) * 128], in1=tri[:], op=ALU.mult), ["tri"]))
            return ops

        def fin_simple(colbase):
            def f(qt, po):
                of = ofin[qt % 2]
                ofn = "ofin%d" % (qt % 2)
                fc_ = fin[:, 4 + (qt % 2):5 + (qt % 2)]
                fcn = "fin%d" % (qt % 2)
                P.op("dve", lambda e: e.reciprocal(out=fc_, in_=po[:, 128:129]), writes=[pn(po), fcn])
                P.op("dve", lambda e: e.tensor_scalar(out=of[:], in0=po[:, 0:128], scalar1=fc_, scalar2=None, op0=ALU.mult),
                     reads=[fcn], writes=[pn(po), ofn])
                P.dma("sp", o_scr[qt * 128:(qt + 1) * 128, colbase:colbase + 128], of[:], reads=[ofn], writes=["o_scr"])
            return f

        def qlat_evac(c):
            def ev(pb, tc):
                sl = slice(tc * 512, (tc + 1) * 512)
                P.op("act", lambda e: e.activation(out=QLT[:, c, sl], in_=pb[:, :], func=AF.Copy), writes=[pn(pb), "qlt.%d" % tc])
            return ev
        for c in range(3):
            proj_fm(C_QLAT + 128 * c, 128, qlat_evac(c))
        proj_fm(C_KVLAT, 128, copy_evac(KVLT, "kvlt", 128))
        proj_fm(C_KROPE, 64, rope_evac(KR, "kr", 64, 64, cs64, "scr16", pm64, "pm64"))

        def rms_apply(views, nfeat, gcols, tnames_fn):
            for tc in range(4):
                sl = slice(tc * 512, (tc + 1) * 512)
                n = len(views)
                for c in range(n):
                    P.op("dve", lambda e, c=c, sl=sl: e.tensor_tensor(out=sqb[:, c, :], in0=views[c][:, sl], in1=views[c][:, sl], op=ALU.mult),
                         reads=[tnames_fn(tc)], writes=["sqb"])
                mms = [(pM1[:, :], onesb[:, :], sqb[:, c, :], c == 0, c == n - 1) for c in range(n)]
                P.op("pe", pe_group(mms), reads=["sqb", "onesb"], writes=[pn(pM1)])
                P.op("act", lambda e: e.activation(out=rstd[:], in_=pM1[:, :], func=AF.Ln, scale=1.0 / nfeat, bias=epsr[:, 0:1]),
                     reads=["epsr"], writes=[pn(pM1), "rstd"])
                P.op("act", lambda e: e.activation(out=rstd[:], in_=rstd[:], func=AF.Exp, scale=-0.5), writes=["rstd"])
                for c in range(n):
                    P.op("dve", lambda e, c=c, sl=sl: e.scalar_tensor_tensor(out=views[c][:, sl], in0=views[c][:, sl], scalar=qg[:, gcols[c]:gcols[c] + 1], in1=rstd[:], op0=ALU.mult, op1=ALU.mult),
                         reads=["rstd", "qg"], writes=[tnames_fn(tc)])
        rms_apply([QLT[:, 0, :], QLT[:, 1, :], QLT[:, 2, :]], 384.0, [0, 1, 2], lambda tc: "qlt.%d" % tc)
        rms_apply([KVLT], 128.0, [3], lambda tc: "kvlt.%d" % tc)

        sc_mla = 192.0 ** -0.5
        for h in range(4):
            qs_, ks_ = h % 2, h % 2
            for tc in range(4):
                sl = slice(tc * 512, (tc + 1) * 512)
                pb = next_pb()
                mms = [(pb[:, :], wqu[:, c, h * 192:h * 192 + 128], QLT[:, c, sl], c == 0, c == 2) for c in range(3)]
                P.op("pe", pe_group(mms), reads=["wqu", "qlt.%d" % tc], writes=[pn(pb)])
                copy_evac(QTS[qs_], "qts%d" % qs_, 128)(pb, tc)
                pb = next_pb()
                mms = [(pb[0:64, :], wqu[:, c, h * 192 + 128:h * 192 + 192], QLT[:, c, sl], c == 0, c == 2) for c in range(3)]
                P.op("pe", pe_group(mms), reads=["wqu", "qlt.%d" % tc], writes=[pn(pb)])
                rope_evac(QR, "qr", 64, 64, cs64, "scr16", pm64, "pm64")(pb, tc)
                pb = next_pb()
                P.op("pe", pe_group([(pb[:, :], wkvu[:, h * 256:h * 256 + 128], KVLT[:, sl], True, True)]), reads=["wkvu", "kvlt.%d" % tc], writes=[pn(pb)])
                copy_evac(KT[ks_], "kt%d" % ks_, 128)(pb, tc)
            for kt in range(NT):
                pb = next_pb()
                P.op("pe", pe_group([(pb[:, 0:128], KVLT[:, kt * 128:(kt + 1) * 128], wkvu[:, h * 256 + 128:h * 256 + 256], True, True)]),
                     reads=["wkvu"] + names4("kvlt"), writes=[pn(pb)])
                v_evac(h % 2)(pb, kt)
            attn([(QTS[qs_], 128, names4("qts%d" % qs_)), (QR, 64, names4("qr"))],
                 [(KT[ks_], 128, names4("kt%d" % ks_)), (KR, 64, names4("kr"))],
                 h % 2, sc_mla, lambda qt: list(range(qt + 1)), causal_mask, 129, fin_simple(h * 128))

        sc = 128.0 ** -0.5

        def dil_mask(qt, grp, pt):
            n = len(grp) * 128
            i0 = (15 - qt + grp[0]) * 128
            return [("dve", lambda e: e.tensor_tensor(out=pt[:, 0:n], in0=pt[:, 0:n], in1=mdil[:, i0:i0 + n], op=ALU.mult), ["msk"])]
        for h in range(6):
            s_ = h % 2
            proj_fm(C_DQ + 128 * h, 128, rope_evac(QTS[s_], "qts%d" % s_, 128, 32, cs32, "cs32", pm32, "pm32"))
            proj_fm(C_DK + 128 * h, 128, rope_evac(KT[s_], "kt%d" % s_, 128, 32, cs32, "cs32", pm32, "pm32"))
            proj_tm(C_DV + 128 * h, 128, v_evac(s_))
            if stage == "pdbg":
                P.barrier()
                P.dma("sp", dbg3_d[0], XT[:, 0, :], writes=["dbg3"])
                P.dma("sp", dbg3_d[1], QTS[0], writes=["dbg3"])
                P.dma("sp", dbg3_d[2], KT[0], writes=["dbg3"])
                P.dma("sp", dbg3_d[3].rearrange("p (a b) -> p a b", a=16), VA[:, :, 0, 0:128], writes=["dbg3"])
                P.dma("sp", dbg3_d[4], KVLT, writes=["dbg3"])
                P.dma("sp", dbg3_d[5], QLT[:, 0, :], writes=["dbg3"])
                P.dma("sp", dbg3_d[6, 0:64], KR, writes=["dbg3"])
                P.dma("sp", dbg3_d[7].rearrange("p (a b) -> p a b", a=16), VA[:, :, 0, 1:129], writes=["dbg3"])
                return "stop"
            attn([(QTS[s_], 128, names4("qts%d" % s_))], [(KT[s_], 128, names4("kt%d" % s_))], s_, sc,
                 lambda qt: list(range(qt + 1)), dil_mask, 129, fin_simple(512 + h * 128))

        def gate_evac(pb, kt):
            P.op("act", lambda e: e.activation(out=gates[:, kt, :], in_=pb[:, 0:18], func=AF.Sigmoid), writes=[pn(pb), "gates"])
        proj_tm(C_GATE, 18, gate_evac)

        w1 = SCR16[:, :].bitcast(BF16).rearrange("p (a b) -> p a b", a=32)
        for which in range(2):
            col = (C_NKC, C_NVC)[which]
            src = KT[which]
            proj_fm(col, 128, copy_evac(src, "kt%d" % which, 128))
            P.dma("sp", peT, W[("cmp_pos_k", "cmp_pos_v")[which]][l].rearrange("i d -> d i"), writes=["peT"], allow_slow_non_contiguous=True)
            P.dma("pool", w1, W[("cmp_w1_k", "cmp_w1_v")[which]][l].rearrange("(i d) j -> d i j", d=128), writes=["scr16"])
            P.dma("pool", w2, W[("cmp_w2_k", "cmp_w2_v")[which]][l].rearrange("(jc p) d -> p jc d", p=128), writes=["w2"])
            ovl = bass.AP(src.tensor, src.offset, [list(src.ap[0]), [1, 32], [16, 127]])
            P.op("dve", lambda e, ovl=ovl: e.tensor_tensor(out=XPE, in0=ovl, in1=peT.unsqueeze(2).broadcast_to([128, 32, 127]), op=ALU.add),
                 reads=names4("kt%d" % which) + ["peT"], writes=["xpe"])
            for jc in range(2):
                pb = next_pb()
                mms = [(pb[:, 0:127], w1[:, i, jc * 128:(jc + 1) * 128], XPE[:, i, :], i == 0, i == 31) for i in range(32)]
                P.op("pe", pe_group(mms), reads=["scr16", "xpe"], writes=[pn(pb)])
                P.op("act", lambda e, pb=pb: e.activation(out=hsc[0], in_=pb[:, 0:127], func=AF.Square), writes=[pn(pb), "hsc0"])
                P.op("dve", lambda e: e.tensor_scalar(out=hsc[0], in0=hsc[0], scalar1=0.044715, scalar2=1.0, op0=ALU.mult, op1=ALU.add), writes=["hsc0"])
                P.op("dve", lambda e, pb=pb: e.tensor_tensor(out=hsc[0], in0=hsc[0], in1=pb[:, 0:127], op=ALU.mult), writes=["hsc0", pn(pb)])
                P.op("act", lambda e: e.activation(out=hsc[1], in_=hsc[0], func=AF.Sigmoid, scale=1.5957691216057308), reads=["hsc0"], writes=["hsc1"])
                P.op("dve", lambda e, pb=pb, jc=jc: e.tensor_tensor(out=hT[:, jc, :], in0=hsc[1], in1=pb[:, 0:127], op=ALU.mult), reads=["hsc1"], writes=["hT", pn(pb)])
            if which == 0:
                pb = next_pb()
                mms = [(pb[:, 0:127], w2[:, jc, :], hT[:, jc, :], jc == 0, jc == 1) for jc in range(2)]
                P.op("pe", pe_group(mms), reads=["w2", "hT"], writes=[pn(pb)])
                rope_evac(KCT, "kct", 128, 32, cskc, "cskc", pm32, "pm32")(pb, 0, ncol=127, c0=0)
            else:
                pb = next_pb()
                mms = [(pb[0:127, 0:128], hT[:, jc, :], w2[:, jc, :], jc == 0, jc == 1) for jc in range(2)]
                P.op("pe", pe_group(mms), reads=["w2", "hT"], writes=[pn(pb)])
                P.op("dve", lambda e, pb=pb: e.tensor_copy(out=VCA[0:127, 0:128], in_=pb[0:127, 0:128]), writes=[pn(pb), "vca"])
                P.op("dve", lambda e: e.memset(VCA[0:127, 128:129], 1.0), writes=["vca"])
                P.op("dve", lambda e: e.tensor_copy(out=VCA[0:127, 129:161], in_=cover[0:127, :]), reads=["cover"], writes=["vca"])

        P.dma("pool", vcm, CD["vcm"], writes=["msk"])
        def cmp_mask(qt, grp, pt):
            return [("dve", lambda e: e.tensor_tensor(out=pt[0:127, 0:128], in0=pt[0:127, 0:128], in1=vcm[0:127, qt * 128:(qt + 1) * 128], op=ALU.mult), ["msk"])]
        for h in range(6):
            s_ = h % 2
            proj_fm(C_NQ + 128 * h, 128, rope_evac(QTS[s_], "qts%d" % s_, 128, 32, cs32, "cs32", pm32, "pm32"))
            P.dma("sp", nq_scr[h], QTS[s_], reads=names4("qts%d" % s_), writes=["nq_scr%d" % h])

            def fin_cmp(qt, po, h=h):
                oc = ocl[qt % 2]
                ocn = "ocl%d" % (qt % 2)
                P.op("dve", lambda e: e.tensor_scalar(out=fin[:, 0:1], in0=po[:, 128:129], scalar1=1e-30, scalar2=None, op0=ALU.max), writes=[pn(po), "fin"])
                P.op("dve", lambda e: e.reciprocal(out=fin[:, 1:2], in_=fin[:, 0:1]), writes=["fin"])
                P.op("dve", lambda e: e.scalar_tensor_tensor(out=imp[:, qt, :], in0=po[:, 129:161], scalar=fin[:, 1:2], in1=imp[:, qt, :], op0=ALU.mult, op1=ALU.add),
                     reads=["fin"], writes=[pn(po), "imp"])
                P.op("dve", lambda e: e.tensor_tensor(out=fin[:, 2:3], in0=fin[:, 1:2], in1=gates[:, qt, 3 * h:3 * h + 1], op=ALU.mult), reads=["gates"], writes=["fin"])
                P.op("dve", lambda e: e.tensor_scalar(out=oc[:], in0=po[:, 0:128], scalar1=fin[:, 2:3], scalar2=None, op0=ALU.mult), reads=["fin"], writes=[pn(po), ocn])
                P.dma("sp", ocmp_scr[h, qt * 128:(qt + 1) * 128, :], oc[:], reads=[ocn], writes=["ocmp_scr%d" % h])
            attn([(QTS[s_], 128, names4("qts%d" % s_))], [(KCT, 128, ["kct.0"])], 0, sc, lambda qt: [0], cmp_mask, 161, fin_cmp,
                 nk=127, vsrc=VCA[0:127, 0:161], vname="vca")

        for qt in range(NT):
            P.op("dve", lambda e, qt=qt: e.tensor_tensor(out=selr[:], in0=imp[:, qt, :], in1=selA[:, qt, :], op=ALU.mult), reads=["imp", "selA"], writes=["selr"])
            P.op("dve", lambda e, qt=qt: e.tensor_tensor(out=selr[:], in0=selr[:], in1=selB[:, qt, :], op=ALU.add), reads=["selB"], writes=["selr"])
            P.op("dve", lambda e: e.tensor_tensor(out=selw, in0=selr[:].unsqueeze(1).broadcast_to([128, 32, 32]), in1=selr[:].unsqueeze(2).broadcast_to([128, 32, 32]), op=ALU.is_gt),
                 reads=["selr"], writes=["selw", "rt1", "rt2"])
            P.op("dve", lambda e: e.tensor_reduce(out=selr[:], in_=selw, axis=AX.X, op=ALU.add), reads=["selw"], writes=["selr"])
            P.op("dve", lambda e: e.tensor_scalar(out=selr[:], in0=selr[:], scalar1=15.5, scalar2=-30000.0, op0=ALU.is_gt, op1=ALU.mult), writes=["selr"])
            P.op("pe", lambda e: e.transpose(out=pM1[0:32, 0:128], in_=selr[:], identity=ident[:]), reads=["selr", "ident"], writes=[pn(pM1)])
            P.op("act", lambda e, qt=qt: e.activation(out=nbT[0:32, qt * 128:(qt + 1) * 128], in_=pM1[0:32, 0:128], func=AF.Copy), writes=[pn(pM1), "nbT"])

        P.dma("pool", emat, CD["emat"], writes=["msk"])
        proj_fm(C_NKS, 128, rope_evac(KT[0], "kt0", 128, 32, cs32, "cs32", pm32, "pm32"))
        proj_fm(C_NKW, 128, rope_evac(KT[1], "kt1", 128, 32, cs32, "cs32", pm32, "pm32"))
        proj_tm(C_NVS, 128, v_evac(0))
        proj_tm(C_NVW, 128, v_evac(1))

        def win_mask(qt, grp, pt):
            ops = []
            for j, kt in enumerate(grp):
                if kt == qt:
                    ops.append(("dve", lambda e, j=j: e.tensor_tensor(out=pt[:, j * 128:(j + 1) * 128], in0=pt[:, j * 128:(j + 1) * 128], in1=tri[:], op=ALU.mult), ["tri"]))
                elif kt == qt - 4:
                    ops.append(("dve", lambda e, j=j: e.tensor_tensor(out=pt[:, j * 128:(j + 1) * 128], in0=pt[:, j * 128:(j + 1) * 128], in1=upp[:], op=ALU.mult), ["upp"]))
            return ops
        for h in range(6):
            s_ = h % 2
            P.dma("sp", QTS[s_], nq_scr[h], reads=["nq_scr%d" % h], writes=names4("qts%d" % s_))

            def fin_slc(qt, po, h=h):
                P.op("dve", lambda e: e.tensor_scalar(out=fin[:, 0:1], in0=po[:, 128:129], scalar1=1e-30, scalar2=None, op0=ALU.max), writes=[pn(po), "fin"])
                P.op("dve", lambda e: e.reciprocal(out=fin[:, 1:2], in_=fin[:, 0:1]), writes=["fin"])
                P.op("dve", lambda e: e.tensor_tensor(out=fin[:, 2:3], in0=fin[:, 1:2], in1=gates[:, qt, 3 * h + 1:3 * h + 2], op=ALU.mult), reads=["gates"], writes=["fin"])
                P.op("dve", lambda e: e.tensor_scalar(out=onsa[:, qt, :], in0=po[:, 0:128], scalar1=fin[:, 2:3], scalar2=None, op0=ALU.mult), reads=["fin"], writes=[pn(po), "onsa"])
            attn([(QTS[s_], 128, names4("qts%d" % s_))], [(KT[0], 128, names4("kt0"))], 0, sc, lambda qt: list(range(qt + 1)), causal_mask, 129, fin_slc, bias=True)

            def fin_win(qt, po, h=h):
                oc = ocl[qt % 2]
                ocn = "ocl%d" % (qt % 2)
                of = ofin[qt % 2]
                ofn = "ofin%d" % (qt % 2)
                P.dma("sp", oc[:], ocmp_scr[h, qt * 128:(qt + 1) * 128, :], reads=["ocmp_scr%d" % h], writes=[ocn])
                P.op("dve", lambda e: e.tensor_scalar(out=fin[:, 0:1], in0=po[:, 128:129], scalar1=1e-30, scalar2=None, op0=ALU.max), writes=[pn(po), "fin"])
                P.op("dve", lambda e: e.reciprocal(out=fin[:, 1:2], in_=fin[:, 0:1]), writes=["fin"])
                P.op("dve", lambda e: e.tensor_tensor(out=fin[:, 2:3], in0=fin[:, 1:2], in1=gates[:, qt, 3 * h + 2:3 * h + 3], op=ALU.mult), reads=["gates"], writes=["fin"])
                P.op("dve", lambda e: e.scalar_tensor_tensor(out=onsa[:, qt, :], in0=po[:, 0:128], scalar=fin[:, 2:3], in1=onsa[:, qt, :], op0=ALU.mult, op1=ALU.add),
                     reads=["fin"], writes=[pn(po), "onsa"])
                P.op("dve", lambda e: e.tensor_tensor(out=of[:], in0=onsa[:, qt, :], in1=oc[:], op=ALU.add), reads=["onsa", ocn], writes=[ofn])
                P.dma("sp", o_scr[qt * 128:(qt + 1) * 128, 1280 + h * 128:1280 + (h + 1) * 128], of[:], reads=[ofn], writes=["o_scr"])
            attn([(QTS[s_], 128, names4("qts%d" % s_))], [(KT[1], 128, names4("kt1"))], 1, sc, lambda qt: list(range(max(0, qt - 4), qt + 1)), win_mask, 129, fin_win)

        phase()
        WO = XT
        P.dma("pool", WO[:, :, :], W["w_out"][l].rearrange("(kc p) n -> p kc n", p=128), writes=["WO"])
        lng = carve([128, D], F32)
        lnb = carve([128, D], F32)
        P.dma("sp", lng, W["ln1_g"][l:l + 1, :].broadcast_to([128, D]), writes=["lng"])
        P.dma("sp", lnb, W["ln1_b"][l:l + 1, :].broadcast_to([128, D]), writes=["lnb"])
        otl = [carve([128, D], BF16) for _ in range(2)]
        oT = [carve([128, 16, 128], BF16) for _ in range(2)]
        xtl = [carve([128, D], F32) for _ in range(2)]
        ytl = [carve([128, D], F32) for _ in range(2)]
        x1b = [carve([128, D], BF16) for _ in range(2)]
        x1T = carve([128, 16, 128], F32)
        wr = carve([128, 16, 36], F32)
        st = carve([128, 16], F32)
        rl = carve([128, 64], F32)
        P.dma("sp", wr[:, :, 0:4], W["w_grp"][l].rearrange("(kc p) n -> p kc n", p=128), writes=["wr"])
        P.dma("sp", wr[:, :, 4:36], W["w_exp"][l].rearrange("(kc p) n -> p kc n", p=128), writes=["wr"])
        psT = [pS0[:, :].bitcast(BF16), pS1[:, :].bitcast(BF16)]

        def layer_norm(y, yn, g, gname, b, bname, out, outn):
            P.op("dve", lambda e: e.tensor_reduce(out=st[:, 0:1], in_=y, axis=AX.X, op=ALU.add), reads=[yn], writes=["st"])
            P.op("dve", lambda e: e.tensor_scalar(out=st[:, 1:2], in0=st[:, 0:1], scalar1=-1.0 / D, scalar2=None, op0=ALU.mult), writes=["st"])
            P.op("act", lambda e: e.activation(out=y, in_=y, func=AF.Identity, bias=st[:, 1:2], scale=1.0), reads=["st"], writes=[yn])
            P.op("dve", lambda e: e.memset(st[:, 2:3], 0.0), writes=["st"])
            P.op("act", lambda e: e.activation(out=out, in_=y, func=AF.Square, accum_out=st[:, 2:3]), reads=[yn], writes=[outn, "st"])
            P.op("act", lambda e: e.activation(out=st[:, 3:4], in_=st[:, 2:3], func=AF.Ln, scale=1.0 / D, bias=epsr[:, 1:2]), reads=["epsr"], writes=["st"])
            P.op("act", lambda e: e.activation(out=st[:, 3:4], in_=st[:, 3:4], func=AF.Exp, scale=-0.5), writes=["st"])
            P.op("dve", lambda e: e.scalar_tensor_tensor(out=out, in0=y, scalar=st[:, 3:4], in1=g, op0=ALU.mult, op1=ALU.mult), reads=[yn, "st", gname], writes=[outn])
            P.op("pool", lambda e: e.tensor_tensor(out=out, in0=out, in1=b, op=ALU.add), reads=[bname], writes=[outn])

        for t in range(NT):
            i2 = t % 2
            rows = slice(t * 128, (t + 1) * 128)
            P.dma("sp", otl[i2], o_scr[rows, :], reads=["o_scr"], writes=["otl%d" % i2])
            P.dma("sp", xtl[i2], x_src[rows, :], writes=["xtl%d" % i2])
            if stage == "odbg":
                P.op("pool", lambda e, i2=i2: e.tensor_copy(out=ytl[i2], in_=otl[i2]), reads=["otl%d" % i2], writes=["ytl%d" % i2])
                P.dma("sp", dbg2_d[rows, :], ytl[i2], reads=["ytl%d" % i2], writes=["dbg2"])
            for g in range(2):
                pt_ = psT[g]
                pnm = pn((pS0, pS1)[g])
                P.op("pe", (lambda pt_=pt_, g=g, i2=i2: lambda e: [e.transpose(out=pt_[:, j * 128:(j + 1) * 128], in_=otl[i2][:, (g * 8 + j) * 128:(g * 8 + j + 1) * 128], identity=identb[:]) for j in range(8)][-1])(),
                     reads=["otl%d" % i2, "identb"], writes=[pnm])
                P.op("act", lambda e, pt_=pt_, g=g, i2=i2: e.activation(out=oT[i2][:, g * 8:(g + 1) * 8, :], in_=pt_[:, 0:1024].rearrange("p (a b) -> p a b", a=8), func=AF.Copy),
                     writes=[pnm, "oT%d" % i2])
            for cc in range(4):
                pb = next_pb()
                mms = [(pb[:, :], oT[i2][:, kc, :], WO[:, kc, cc * 512:(cc + 1) * 512], kc == 0, kc == 15) for kc in range(16)]
                P.op("pe", pe_group(mms), reads=["oT%d" % i2, "WO"], writes=[pn(pb)])
                P.op("dve", lambda e, pb=pb, cc=cc, i2=i2: e.scalar_tensor_tensor(out=ytl[i2][:, cc * 512:(cc + 1) * 512], in0=xtl[i2][:, cc * 512:(cc + 1) * 512], scalar=ALPHA, in1=pb[:, :], op0=ALU.mult, op1=ALU.add),
                     reads=["xtl%d" % i2], writes=[pn(pb), "ytl%d" % i2])
            layer_norm(ytl[i2], "ytl%d" % i2, lng, "lng", lnb, "lnb", xtl[i2], "xtl%d" % i2)
            P.dma("sp", x1_scr[rows, :], xtl[i2], reads=["xtl%d" % i2], writes=["x1_scr"])
            if stage != "full" and l == 0:
                P.dma("sp", dbg_d[rows, :], xtl[i2], reads=["xtl%d" % i2], writes=["dbg"])
            P.op("pool", lambda e, i2=i2: e.tensor_copy(out=x1b[i2], in_=xtl[i2]), reads=["xtl%d" % i2], writes=["x1b%d" % i2])
            P.dma("sp", x1b_scr[rows, :], x1b[i2], reads=["x1b%d" % i2], writes=["x1b_scr"])
            for g in range(4):
                pm_ = (pM0, pM1)[g % 2]
                P.op("pe", (lambda pm_=pm_, g=g, i2=i2: lambda e: [e.transpose(out=pm_[:, j * 128:(j + 1) * 128], in_=xtl[i2][:, (g * 4 + j) * 128:(g * 4 + j + 1) * 128], identity=ident[:]) for j in range(4)][-1])(),
                     reads=["xtl%d" % i2, "ident"], writes=[pn(pm_)])
                P.op("act", lambda e, pm_=pm_, g=g: e.activation(out=x1T[:, g * 4:(g + 1) * 4, :], in_=pm_[:, :].rearrange("p (a b) -> p a b", a=4), func=AF.Copy),
                     writes=[pn(pm_), "x1T"])
            mms = [(pO0[:, 0:36], x1T[:, kc, :], wr[:, kc, :], kc == 0, kc == 15) for kc in range(16)]
            P.op("pe", pe_group(mms), reads=["x1T", "wr"], writes=[pn(pO0)])
            router(t, pO0, rl)

    def router(t, pl, rl):
        V = lambda f, reads=(), writes=("rl",): P.op("dve", f, reads=reads, writes=writes)
        lg = rl[:, 0:36]
        V(lambda e: e.tensor_copy(out=lg, in_=pl[:, 0:36]), writes=["rl", pn(pl)])
        V(lambda e: e.tensor_reduce(out=rl[:, 36:37], in_=rl[:, 0:4], axis=AX.X, op=ALU.max))
        V(lambda e: e.tensor_scalar(out=rl[:, 40:44], in0=rl[:, 0:4], scalar1=rl[:, 36:37], scalar2=None, op0=ALU.subtract))
        V(lambda e: e.memset(rl[:, 37:38], 0.0))
        P.op("act", lambda e: e.activation(out=rl[:, 44:48], in_=rl[:, 40:44], func=AF.Exp, accum_out=rl[:, 37:38]), writes=["rl"])
        V(lambda e: e.reciprocal(out=rl[:, 38:39], in_=rl[:, 37:38]))
        V(lambda e: e.tensor_scalar(out=rl[:, 40:44], in0=rl[:, 40:44], scalar1=0.0, scalar2=1e30, op0=ALU.is_lt, op1=ALU.mult))
        em = rl[:, 4:36].rearrange("p (g k) -> p g k", g=4)
        V(lambda e: e.tensor_tensor(out=em, in0=em, in1=rl[:, 40:44].unsqueeze(2).broadcast_to([128, 4, 8]), op=ALU.subtract))
        V(lambda e: e.tensor_reduce(out=rl[:, 48:49], in_=rl[:, 4:36], axis=AX.X, op=ALU.max))
        V(lambda e: e.tensor_scalar(out=oh[:, t, 0, :], in0=rl[:, 4:36], scalar1=rl[:, 48:49], scalar2=None, op0=ALU.is_equal), writes=["rl", "oh"])
        V(lambda e: e.scalar_tensor_tensor(out=rl[:, 4:36], in0=oh[:, t, 0, :], scalar=-1e30, in1=rl[:, 4:36], op0=ALU.mult, op1=ALU.add), reads=["oh"])
        V(lambda e: e.tensor_reduce(out=rl[:, 49:50], in_=rl[:, 4:36], axis=AX.X, op=ALU.max))
        V(lambda e: e.tensor_scalar(out=oh[:, t, 1, :], in0=rl[:, 4:36], scalar1=rl[:, 49:50], scalar2=None, op0=ALU.is_equal), writes=["rl", "oh"])
        V(lambda e: e.tensor_tensor(out=mk[:, t, :], in0=oh[:, t, 0, :], in1=oh[:, t, 1, :], op=ALU.add), reads=["oh"], writes=["mk"])
        V(lambda e: e.tensor_tensor(out=rl[:, 50:51], in0=rl[:, 49:50], in1=rl[:, 48:49], op=ALU.subtract))
        P.op("act", lambda e: e.activation(out=rl[:, 51:52], in_=rl[:, 50:51], func=AF.Exp), writes=["rl"])
        V(lambda e: e.tensor_scalar(out=rl[:, 51:52], in0=rl[:, 51:52], scalar1=1.0, scalar2=None, op0=ALU.add))
        V(lambda e: e.reciprocal(out=rl[:, 52:53], in_=rl[:, 51:52]))
        V(lambda e: e.tensor_tensor(out=gatew[:, t, 0:1], in0=rl[:, 52:53], in1=rl[:, 38:39], op=ALU.mult), writes=["rl", "gatew"])
        V(lambda e: e.tensor_tensor(out=gatew[:, t, 1:2], in0=rl[:, 38:39], in1=gatew[:, t, 0:1], op=ALU.subtract), writes=["rl", "gatew"])

    def moe(l, last):
        phase()
        pos = carve([128, 32], F32)
        tmp = carve([128, 32], F32)
        sf = carve([128, 4], F32)
        xbt = [carve([128, D], BF16) for _ in range(2)]
        for t in range(NT):
            mms = [(pM0[:, 0:32], onesb[:, :], mk[:, tp, :], tp == 0, False) for tp in range(t)]
            mms.append((pM0[:, 0:32], ltri[:, :], mk[:, t, :], t == 0, True))
            P.op("pe", pe_group(mms), reads=["mk", "onesb", "ltri"], writes=[pn(pM0)])
            P.op("dve", lambda e: e.scalar_tensor_tensor(out=pos, in0=pM0[:, 0:32], scalar=float(CAP - 1), in1=ecap[:], op0=ALU.min, op1=ALU.add), reads=["ecap"], writes=[pn(pM0), "pos"])
            for k in range(2):
                P.op("dve", lambda e, k=k, t=t: e.tensor_tensor(out=tmp, in0=pos, in1=oh[:, t, k, :], op=ALU.mult), reads=["pos", "oh"], writes=["tmp"])
                P.op("dve", lambda e, k=k: e.tensor_reduce(out=sf[:, k:k + 1], in_=tmp, axis=AX.X, op=ALU.add), reads=["tmp"], writes=["sf"])
            P.op("dve", lambda e, t=t: e.tensor_copy(out=slots[:, t, :], in_=sf[:, 0:2]), reads=["sf"], writes=["slots"])
            i2 = t % 2
            P.dma("sp", xbt[i2], x1b_scr[t * 128:(t + 1) * 128, :], reads=["x1b_scr"], writes=["xbt%d" % i2])
            for k in range(2):
                P.idma(lambda g, t=t, k=k, i2=i2: g.indirect_dma_start(out=xg_scr[:, :], out_offset=bass.IndirectOffsetOnAxis(ap=slots[:, t, k:k + 1], axis=0),
                                                                      in_=xbt[i2], in_offset=None),
                       reads=["xbt%d" % i2, "slots"], writes=["xg_scr"])
        phase()
        wbuf = []
        e0 = XT[:, :, :].rearrange("p a b -> p (a b)")
        wbuf.append((e0[:, 0:8192].rearrange("p (a b) -> p a b", a=16), e0[:, 8192:16384].rearrange("p (a b) -> p a b", a=16),
                     e0[:, 16384:24576].rearrange("p (a b) -> p a b", a=4)))
        wbuf.append((carve([128, 16, 512], BF16), carve([128, 16, 512], BF16), carve([128, 4, D], BF16)))
        ysb = [e0[:, 24576:28672].bitcast(F32), e0[:, 28672:32768].bitcast(F32)]
        xg = [carve([128, D], BF16) for _ in range(2)]
        xgT = [carve([128, 16, CAP], BF16) for _ in range(2)]
        hTm = [carve([128, 4, CAP], BF16) for _ in range(2)]
        sg = [carve([128, CAP], F32) for _ in range(2)]
        psT = [pS0[:, :].bitcast(BF16), pS1[:, :].bitcast(BF16)]

        wds = [carve([128, 2, D], F32) for _ in range(2)]

        def load_w_exp(ex):
            wg, wu, wd = wbuf[ex % 2]
            wn = "wexp%d" % (ex % 2)
            P.dma("pool", wg, W["w_gate"][l, ex].rearrange("(kc p) f -> p kc f", p=128), writes=[wn + "g"])
            P.dma("pool", wu, W["w_up"][l, ex].rearrange("(kc p) f -> p kc f", p=128), writes=[wn + "g"])
            wdv = W["w_down"][l, ex].rearrange("(kc p) f -> p kc f", p=128)
            for hf in range(2):
                P.dma("sp", wds[hf], wdv[:, 2 * hf:2 * hf + 2, :], writes=["wds%d" % hf])

        def cast_wd(ex):
            wg, wu, wd = wbuf[ex % 2]
            wn = "wexp%d" % (ex % 2)
            for hf in range(2):
                for j in range(2):
                    kc = 2 * hf + j
                    if j == 0:
                        P.op("act", lambda e, kc=kc, hf=hf, j=j: e.activation(out=wd[:, kc, :], in_=wds[hf][:, j, :], func=AF.Copy), reads=["wds%d" % hf], writes=[wn + "d"])
                    else:
                        P.op("dve", lambda e, kc=kc, hf=hf, j=j: e.tensor_copy(out=wd[:, kc, :], in_=wds[hf][:, j, :]), reads=["wds%d" % hf], writes=[wn + "d"])

        def prep(ex):
            b2 = ex % 2
            for s_ in range(CAP // 128):
                r0 = ex * CAP + s_ * 128
                P.dma("sp", xg[s_], xg_scr[r0:r0 + 128, :], reads=["xg_scr"], writes=["xg%d" % s_])
                for g in range(2):
                    pt_ = psT[g]
                    pnm = pn((pS0, pS1)[g])
                    P.op("pe", (lambda pt_=pt_, g=g, s_=s_: lambda e: [e.transpose(out=pt_[:, j * 128:(j + 1) * 128], in_=xg[s_][:, (g * 8 + j) * 128:(g * 8 + j + 1) * 128], identity=identb[:]) for j in range(8)][-1])(),
                         reads=["xg%d" % s_, "identb"], writes=[pnm])
                    if g == 0:
                        P.op("act", lambda e, pt_=pt_, g=g, s_=s_: e.activation(out=xgT[b2][:, g * 8:(g + 1) * 8, s_ * 128:(s_ + 1) * 128], in_=pt_[:, 0:1024].rearrange("p (a b) -> p a b", a=8), func=AF.Copy),
                             writes=[pnm, "xgT%d" % b2])
                    else:
                        P.op("dve", lambda e, pt_=pt_, g=g, s_=s_: e.tensor_copy(out=xgT[b2][:, g * 8:(g + 1) * 8, s_ * 128:(s_ + 1) * 128], in_=pt_[:, 0:1024].rearrange("p (a b) -> p a b", a=8)),
                             writes=[pnm, "xgT%d" % b2])

        def gateup(ex):
            b2 = ex % 2
            wg, wu, wd = wbuf[b2]
            wn = "wexp%d" % b2
            for fc in range(4):
                pg, pu = ((pA, pB), (pM0, pM1))[fc % 2]
                s2 = fc % 2
                mms = [(pg[:, 0:CAP], wg[:, kc, fc * 128:(fc + 1) * 128], xgT[b2][:, kc, :], kc == 0, kc == 15) for kc in range(16)]
                P.op("pe", pe_group(mms), reads=[wn + "g", "xgT%d" % b2], writes=[pn(pg)])
                mms = [(pu[:, 0:CAP], wu[:, kc, fc * 128:(fc + 1) * 128], xgT[b2][:, kc, :], kc == 0, kc == 15) for kc in range(16)]
                P.op("pe", pe_group(mms), reads=[wn + "g", "xgT%d" % b2], writes=[pn(pu)])
                P.op("act", lambda e, pg=pg, s2=s2: e.activation(out=sg[s2], in_=pg[:, 0:CAP], func=AF.Silu), writes=[pn(pg), "sg%d" % s2])
                P.op("dve", lambda e, fc=fc, pu=pu, s2=s2: e.tensor_tensor(out=hTm[b2][:, fc, :], in0=sg[s2], in1=pu[:, 0:CAP], op=ALU.mult), reads=["sg%d" % s2], writes=[pn(pu), "hTm%d" % b2])

        def down(ex):
            b2 = ex % 2
            wg, wu, wd = wbuf[b2]
            wn = "wexp%d" % b2
            for s_ in range(CAP // 128):
                for cc in range(4):
                    pb = (pO0, pO1)[cc % 2]
                    mms = [(pb[:, :], hTm[b2][:, kc, s_ * 128:(s_ + 1) * 128], wd[:, kc, cc * 512:(cc + 1) * 512], kc == 0, kc == 3) for kc in range(4)]
                    P.op("pe", pe_group(mms), reads=[wn + "d", "hTm%d" % b2], writes=[pn(pb)])
                    if cc % 2 == 0:
                        P.op("act", lambda e, pb=pb, cc=cc, s_=s_: e.activation(out=ysb[s_][:, cc * 512:(cc + 1) * 512], in_=pb[:, :], func=AF.Copy), writes=[pn(pb), "ysb%d" % s_])
                    else:
                        P.op("dve", lambda e, pb=pb, cc=cc, s_=s_: e.tensor_copy(out=ysb[s_][:, cc * 512:(cc + 1) * 512], in_=pb[:, :]), writes=[pn(pb), "ysb%d" % s_])
                r0 = ex * CAP + s_ * 128
                P.dma("sp", y_scr[r0:r0 + 128, :], ysb[s_], reads=["ysb%d" % s_], writes=["y_scr"])

        load_w_exp(0)
        cast_wd(0)
        load_w_exp(1)
        prep(0)
        for ex in range(NEXP):
            gateup(ex)
            if ex + 1 < NEXP:
                prep(ex + 1)
                cast_wd(ex + 1)
            down(ex)
            if ex + 2 < NEXP:
                load_w_exp(ex + 2)
        phase()
        lng = carve([128, D], F32)
        lnb = carve([128, D], F32)
        P.dma("sp", lng, W["ln2_g"][l:l + 1, :].broadcast_to([128, D]), writes=["lng"])
        P.dma("sp", lnb, W["ln2_b"][l:l + 1, :].broadcast_to([128, D]), writes=["lnb"])
        y0 = [carve([128, D], F32) for _ in range(2)]
        y1 = [carve([128, D], F32) for _ in range(2)]
        x1t = [carve([128, D], F32) for _ in range(2)]
        st = carve([128, 16], F32)
        dst = out_d if last else xres_scr
        for t in range(NT):
            i2 = t % 2
            rows = slice(t * 128, (t + 1) * 128)
            P.dma("sp", x1t[i2], x1_scr[rows, :], reads=["x1_scr"], writes=["x1t%d" % i2])
            P.idma(lambda g, t=t, i2=i2: g.indirect_dma_start(out=y0[i2], out_offset=None, in_=y_scr[:, :], in_offset=bass.IndirectOffsetOnAxis(ap=slots[:, t, 0:1], axis=0)), reads=["y_scr", "slots"], writes=["y0%d" % i2])
            P.idma(lambda g, t=t, i2=i2: g.indirect_dma_start(out=y1[i2], out_offset=None, in_=y_scr[:, :], in_offset=bass.IndirectOffsetOnAxis(ap=slots[:, t, 1:2], axis=0)), reads=["y_scr", "slots"], writes=["y1%d" % i2])
            P.op("dve", lambda e, t=t, i2=i2: e.tensor_scalar(out=y0[i2], in0=y0[i2], scalar1=gatew[:, t, 0:1], scalar2=None, op0=ALU.mult), reads=["gatew"], writes=["y0%d" % i2])
            P.op("dve", lambda e, t=t, i2=i2: e.scalar_tensor_tensor(out=y0[i2], in0=y1[i2], scalar=gatew[:, t, 1:2], in1=y0[i2], op0=ALU.mult, op1=ALU.add), reads=["gatew", "y1%d" % i2], writes=["y0%d" % i2])
            if stage != "full" and l == 0:
                P.dma("sp", dbg2_d[rows, :], y0[i2], reads=["y0%d" % i2], writes=["dbg2"])
            P.op("dve", lambda e, i2=i2: e.scalar_tensor_tensor(out=y0[i2], in0=x1t[i2], scalar=ALPHA, in1=y0[i2], op0=ALU.mult, op1=ALU.add), reads=["x1t%d" % i2], writes=["y0%d" % i2])
            y, yn, out, outn = y0[i2], "y0%d" % i2, x1t[i2], "x1t%d" % i2
            P.op("dve", lambda e, y=y: e.tensor_reduce(out=st[:, 0:1], in_=y, axis=AX.X, op=ALU.add), reads=[yn], writes=["st"])
            P.op("dve", lambda e: e.tensor_scalar(out=st[:, 1:2], in0=st[:, 0:1], scalar1=-1.0 / D, scalar2=None, op0=ALU.mult), writes=["st"])
            P.op("act", lambda e, y=y: e.activation(out=y, in_=y, func=AF.Identity, bias=st[:, 1:2], scale=1.0), reads=["st"], writes=[yn])
            P.op("dve", lambda e: e.memset(st[:, 2:3], 0.0), writes=["st"])
            P.op("act", lambda e, y=y, out=out: e.activation(out=out, in_=y, func=AF.Square, accum_out=st[:, 2:3]), reads=[yn], writes=[outn, "st"])
            P.op("act", lambda e: e.activation(out=st[:, 3:4], in_=st[:, 2:3], func=AF.Ln, scale=1.0 / D, bias=epsr[:, 1:2]), reads=["epsr"], writes=["st"])
            P.op("act", lambda e: e.activation(out=st[:, 3:4], in_=st[:, 3:4], func=AF.Exp, scale=-0.5), writes=["st"])
            P.op("dve", lambda e, y=y, out=out: e.scalar_tensor_tensor(out=out, in0=y, scalar=st[:, 3:4], in1=lng, op0=ALU.mult, op1=ALU.mult), reads=[yn, "st", "lng"], writes=[outn])
            P.op("pool", lambda e, out=out: e.tensor_tensor(out=out, in0=out, in1=lnb, op=ALU.add), reads=["lnb"], writes=[outn])
            P.dma("sp", dst[rows, :], out, reads=[outn], writes=["dst"])

    x_src = x_d
    for l in range(n_layers):
        last = (l == n_layers - 1)
        if layer(l, x_src, last) == "stop":
            break
        if stage in ("ln1", "odbg"):
            break
        moe(l, last)
        x_src = xres_scr
    P.barrier()
    P.emit()
    return nc


_CONSTS = None


def kernel(**inputs):
    global _CONSTS
    if _CONSTS is None:
        _CONSTS = make_consts()
    nc = build()
    x = np.ascontiguousarray(inputs["x"], dtype=np.float32)
    shared = {k: np.ascontiguousarray(inputs[k], dtype=np.float32) for k in W_SHAPES if k != "w_in"}
    shared["w_in"] = relayout_w_in(np.asarray(inputs["w_in"], dtype=np.float32))
    for k, v in _CONSTS.items():
        shared["c_" + k] = np.ascontiguousarray(v.reshape(CONST_SHAPES[k]), dtype=np.float32)
    in_maps = []
    for b in range(4):
        m = dict(shared)
        m["x"] = x[b]
        in_maps.append(m)
    res = run_bass_kernel_spmd(nc, in_maps, core_ids=list(range(4)))
    return np.stack([np.asarray(r["out"], dtype=np.float32) for r in res.results], axis=0)
```

```python
import contextlib
import numpy as np
import concourse.bass as bass
import concourse.mybir as mybir
from concourse.bass_utils import run_bass_kernel_spmd

F32 = mybir.dt.float32
BF16 = mybir.dt.bfloat16
I32 = mybir.dt.int32
ALU = mybir.AluOpType
AF = mybir.ActivationFunctionType
AX = mybir.AxisListType

S = 2048
D = 2048
NT = 16
DEPTH = 2
IN_COLS = 4434
CAP = 256
NEXP = 32
THETA = 500000.0
ALPHA = (2 * DEPTH) ** 0.25
LN_EPS = 1e-5
RMS_EPS = 1e-6
C_QLAT, C_KVLAT, C_KROPE = 0, 384, 512
C_DQ, C_DK, C_DV = 576, 1344, 2112
C_NQ = 2880
C_NKC, C_NVC, C_NKS, C_NVS, C_NKW, C_NVW = 3648, 3776, 3904, 4032, 4160, 4288
C_GATE = 4416


class Prog:
    CENG = ("pe", "act", "dve", "pool")
    DMAQ = ("sp", "act", "pool")
    RING = 8

    def __init__(self, nc):
        self.nc = nc
        self.es = contextlib.ExitStack()
        self.streams = {e: [] for e in ("pe", "act", "dve", "pool", "sp")}
        self.cnt = {e: 0 for e in self.CENG}
        self.sems = {}
        for e in self.CENG:
            self.sems[e] = self.es.enter_context(nc.semaphore("s_" + e))
        self.dsems = {}
        self.dcnt = {}
        for q in self.DMAQ:
            self.dcnt[q] = 0
            for i in range(self.RING):
                self.dsems[(q, i)] = self.es.enter_context(nc.semaphore("d_%s%d" % (q, i)))
        self.seen = {s: {} for s in self.streams}
        self.tiles = {}

    def sb(self, name, shape, dt):
        return self.es.enter_context(self.nc.sbuf_tensor(name, list(shape), dt))

    def ps(self, name, shape, dt=F32):
        return self.es.enter_context(self.nc.psum_tensor(name, list(shape), dt))

    def _need(self, stream, ev, waits, is_dma=False):
        if ev is None:
            return
        key, val = ev
        if key == stream and key == "pe" and not is_dma:
            return
        if self.seen[stream].get(key, 0) >= val:
            return
        if waits.get(key, 0) < val:
            waits[key] = val

    def _deps(self, stream, reads, writes, is_dma=False):
        waits = {}
        for t in reads:
            st = self.tiles.setdefault(t, {"w": None, "r": []})
            self._need(stream, st["w"], waits, is_dma)
        for t in writes:
            st = self.tiles.setdefault(t, {"w": None, "r": []})
            self._need(stream, st["w"], waits, is_dma)
            for ev in st["r"]:
                self._need(stream, ev, waits, is_dma)
        return waits

    def _commit(self, ev, reads, writes):
        for t in reads:
            if t in writes:
                continue
            r = self.tiles[t]["r"]
            r.append(ev)
            if len(r) > 48:
                best = {}
                for k, v in r:
                    if best.get(k, 0) < v:
                        best[k] = v
                self.tiles[t]["r"] = list(best.items())
        for t in writes:
            self.tiles[t]["w"] = ev
            self.tiles[t]["r"] = []

    def _sem(self, key):
        return self.sems[key] if key in self.sems else self.dsems[key]

    def _emit_waits(self, stream, waits):
        for key, val in waits.items():
            self.streams[stream].append(("w", self._sem(key), val))
            self.seen[stream][key] = val

    def op(self, eng, fn, reads=(), writes=()):
        reads, writes = tuple(reads), tuple(writes)
        self._emit_waits(eng, self._deps(eng, reads, writes))
        self.cnt[eng] += 1
        ev = (eng, self.cnt[eng])
        self.streams[eng].append(("c", fn, self.sems[eng]))
        self._commit(ev, reads, writes)
        return ev

    def _dma_common(self, q, reads, writes):
        waits = self._deps(q, reads, writes, True)
        k = self.dcnt[q]
        self.dcnt[q] += 1
        key = (q, k % self.RING)
        tgt = 16 * (k // self.RING + 1)
        if tgt > 16:
            prev = tgt - 16
            if self.seen[q].get(key, 0) < prev and waits.get(key, 0) < prev:
                waits[key] = prev
        self._emit_waits(q, waits)
        return key, tgt

    def dma(self, q, out, in_, reads=(), writes=(), **kw):
        reads, writes = tuple(reads), tuple(writes)
        key, tgt = self._dma_common(q, reads, writes)
        self.streams[q].append(("d", out, in_, kw, self.dsems[key]))
        self._commit((key, tgt), reads, writes)

    def idma(self, fn, reads=(), writes=()):
        reads, writes = tuple(reads), tuple(writes)
        key, tgt = self._dma_common("pool", reads, writes)
        self.streams["pool"].append(("i", fn, self.dsems[key]))
        self._commit((key, tgt), reads, writes)

    def barrier(self):
        cur = {}
        for e in self.CENG:
            if self.cnt[e] > 0:
                cur[e] = self.cnt[e]
        for q in self.DMAQ:
            k = self.dcnt[q]
            for i in range(self.RING):
                n = (k - i + self.RING - 1) // self.RING if k > i else 0
                if n > 0:
                    cur[(q, i)] = 16 * n
        for s in self.streams:
            waits = {}
            for key, val in cur.items():
                if self.seen[s].get(key, 0) < val:
                    waits[key] = val
            self._emit_waits(s, waits)
        self.tiles = {}

    def emit(self):
        nc = self.nc
        streams = self.streams

        def run(e, lst):
            for it in lst:
                k = it[0]
                if k == "w":
                    e.wait_ge(it[1], it[2])
                elif k == "c":
                    it[1](e).then_inc(it[2], 1)
                elif k == "d":
                    e.dma_start(out=it[1], in_=it[2], **it[3]).then_inc(it[4], 16)
                elif k == "i":
                    it[1](e).then_inc(it[2], 16)

        with nc.Block() as block:
            @block.sync
            def _(e):
                run(e, streams["sp"])

            @block.tensor
            def _(e):
                run(e, streams["pe"])

            @block.scalar
            def _(e):
                run(e, streams["act"])

            @block.vector
            def _(e):
                run(e, streams["dve"])

            @block.gpsimd
            def _(e):
                run(e, streams["pool"])
        self.es.close()


def make_consts():
    c = {}
    c["ident"] = np.eye(128, dtype=np.float32)
    pos = np.arange(S, dtype=np.float32)

    def rope_tab(rot, p):
        half = rot // 2
        inv = (np.float32(THETA) ** (-np.arange(half, dtype=np.float32) / np.float32(half))).astype(np.float32)
        ang = (p.astype(np.float32)[None, :] * inv[:, None]).astype(np.float32)
        cos = np.cos(ang.astype(np.float64)).astype(np.float32)
        sin = np.sin(ang.astype(np.float64)).astype(np.float32)
        t = np.zeros((rot, 2, p.shape[0]), np.float32)
        t[:half, 0], t[half:, 0] = cos, cos
        t[:half, 1], t[half:, 1] = -sin, sin
        return t

    c["cs32"] = rope_tab(32, pos)
    c["cs64"] = rope_tab(64, pos)
    kc = np.zeros((32, 2, 128), np.float32)
    kc[:, :, :127] = rope_tab(32, (np.arange(127) * 16 + 31).astype(np.float32))
    c["cskc"] = kc

    def perm(n):
        m = np.zeros((n, n), np.float32)
        h = n // 2
        for j in range(n):
            m[(j + h) % n, j] = 1.0
        return m

    c["pm32"] = perm(32)
    c["pm64"] = perm(64)
    kk = np.arange(128)[:, None]
    qq = np.arange(128)[None, :]
    md = np.zeros((128, 16, 128), np.float32)
    for delta in range(16):
        d = 128 * delta + qq - kk
        m = ((d >= 0) & (d <= 128)).astype(np.float32) + ((d >= 0) & (d % 4 == 0) & (d <= 512)).astype(np.float32) \
            + ((d >= 0) & (d % 16 == 0)).astype(np.float32)
        md[:, 15 - delta, :] = m
    c["mdil"] = md.reshape(128, 2048)
    c["tri"] = (kk <= qq).astype(np.float32)
    c["upp"] = (kk > qq).astype(np.float32)
    cc = np.arange(128)[:, None]
    vcm = ((16 * cc + 31) <= np.arange(S)[None, :]).astype(np.float32)
    vcm[127] = 0
    c["vcm"] = vcm
    cs = np.arange(128) * 16
    ss = np.arange(32) * 64
    cover = ((cs[:, None] < ss[None, :] + 64) & (cs[:, None] + 32 > ss[None, :])).astype(np.float32)
    cover[127] = 0
    c["cover"] = cover
    p = np.arange(S)
    jj = np.arange(32)[None, :]
    qblk = (p // 64)[:, None]
    valid = (ss[None, :] <= p[:, None])
    forced = (jj == 0) | (jj == qblk) | (jj == qblk - 1)
    selA = (valid & ~forced).astype(np.float32)
    selB = np.where(valid, np.where(forced, 1e4, 0.0), -1.0).astype(np.float32)
    c["selA"] = selA.reshape(16, 128, 32).transpose(1, 0, 2).copy()
    c["selB"] = selB.reshape(16, 128, 32).transpose(1, 0, 2).copy()
    E = np.zeros((32, 16, 128), np.float32)
    for kt in range(16):
        E[2 * kt, kt, :64] = 1
        E[2 * kt + 1, kt, 64:] = 1
    c["emat"] = E.reshape(32, 2048)
    c["ltri"] = (kk < qq).astype(np.float32)
    c["ecap"] = np.tile((np.arange(32) * CAP).astype(np.float32)[None, :], (128, 1))
    return c


CONST_SHAPES = {"ident": (128, 128), "cs32": (32, 2, 2048), "cs64": (64, 2, 2048), "cskc": (32, 2, 128),
                "pm32": (32, 32), "pm64": (64, 64), "mdil": (128, 2048), "tri": (128, 128), "upp": (128, 128),
                "vcm": (128, 2048), "cover": (128, 32), "selA": (128, 16, 32), "selB": (128, 16, 32),
                "emat": (32, 2048), "ltri": (128, 128), "ecap": (128, 32)}

W_SHAPES = {"w_in": (2, 36, 128, 2048), "q_lat_norm": (2, 384), "w_q_up": (2, 384, 768), "kv_lat_norm": (2, 128),
            "w_kv_up": (2, 128, 1024), "cmp_pos_k": (2, 32, 128), "cmp_w1_k": (2, 4096, 256),
            "cmp_w2_k": (2, 256, 128), "cmp_pos_v": (2, 32, 128), "cmp_w1_v": (2, 4096, 256),
            "cmp_w2_v": (2, 256, 128), "w_out": (2, 2048, 2048), "ln1_g": (2, 2048), "ln1_b": (2, 2048),
            "w_grp": (2, 2048, 4), "w_exp": (2, 2048, 32), "w_gate": (2, 32, 2048, 512),
            "w_up": (2, 32, 2048, 512), "w_down": (2, 32, 512, 2048), "ln2_g": (2, 2048), "ln2_b": (2, 2048)}


W_CHUNKS = ([(C_QLAT + 128 * c, 128) for c in range(3)] + [(C_KVLAT, 128), (C_KROPE, 64)]
            + [(C_DQ + 128 * h, 128) for h in range(6)] + [(C_DK + 128 * h, 128) for h in range(6)]
            + [(C_DV + 128 * h, 128) for h in range(6)] + [(C_GATE, 18), (C_NKC, 128), (C_NVC, 128)]
            + [(C_NQ + 128 * h, 128) for h in range(6)] + [(C_NKS, 128), (C_NKW, 128), (C_NVS, 128), (C_NVW, 128)])
W_CHUNK_IDX = {c: i for i, c in enumerate(W_CHUNKS)}


def relayout_w_in(w_in):
    L = w_in.shape[0]
    out = np.zeros((L, len(W_CHUNKS), 128, 16, 128), np.float32)
    for i, (c0, n) in enumerate(W_CHUNKS):
        out[:, i, :, :, 0:n] = w_in[:, :, c0:c0 + n].reshape(L, 16, 128, n).transpose(0, 2, 1, 3)
    return out.reshape(L, len(W_CHUNKS), 128, 2048)


def pe_group(mms):
    def fn(e):
        ins = None
        for (o, l, r, st, sp) in mms:
            ins = e.matmul(o, lhsT=l, rhs=r, start=st, stop=sp)
        return ins
    return fn


def build(n_layers=DEPTH, stage="full"):
    nc = bass.Bass("TRN2", target_bir_lowering=False)

    def din(name, shape, dt=F32):
        return nc.dram_tensor(name, list(shape), dt, kind="ExternalInput").ap()

    def dscr(name, shape, dt):
        return nc.dram_tensor(name, list(shape), dt, kind="Internal").ap()

    x_d = din("x", [S, D])
    W = {k: din(k, v) for k, v in W_SHAPES.items()}
    CD = {k: din("c_" + k, v) for k, v in CONST_SHAPES.items()}
    out_d = nc.dram_tensor("out", [S, D], F32, kind="ExternalOutput").ap()
    dbg_d = dbg2_d = None
    if stage != "full":
        dbg_d = nc.dram_tensor("dbg", [S, D], F32, kind="ExternalOutput").ap()
        dbg2_d = nc.dram_tensor("dbg2", [S, D], F32, kind="ExternalOutput").ap()
        dbg3_d = nc.dram_tensor("dbg3", [8, 128, S], BF16, kind="ExternalOutput").ap()

    o_scr = dscr("o_scr", [S, D], BF16)
    nq_scr = dscr("nq_scr", [6, 128, S], BF16)
    ocmp_scr = dscr("ocmp_scr", [6, S, 128], F32)
    x1_scr = dscr("x1_scr", [S, D], F32)
    x1b_scr = dscr("x1b_scr", [S, D], BF16)
    xres_scr = dscr("xres_scr", [S, D], F32)
    xg_scr = dscr("xg_scr", [NEXP * CAP, D], BF16)
    y_scr = dscr("y_scr", [NEXP * CAP, D], F32)

    P = Prog(nc)
    XT = P.sb("XT", [128, 16, S], BF16)
    ARENA = P.sb("ARENA", [128, 30 * 1024], F32)
    ident = P.sb("ident", [128, 128], F32)
    identb = P.sb("identb", [128, 128], BF16)
    cs32 = P.sb("cs32", [32, 2, S], F32)
    pm32 = P.sb("pm32", [32, 32], BF16)
    pm64 = P.sb("pm64", [64, 64], BF16)
    onesb = P.sb("onesb", [128, 128], BF16)
    tri = P.sb("tri", [128, 128], BF16)
    upp = P.sb("upp", [128, 128], BF16)
    ltri = P.sb("ltri", [128, 128], BF16)
    ecap = P.sb("ecap", [128, 32], F32)
    gates = P.sb("gates", [128, 16, 18], F32)
    slots = P.sb("slots", [128, 16, 2], I32)
    gatew = P.sb("gatew", [128, 16, 2], F32)
    mk = P.sb("mk", [128, 16, 32], BF16)
    oh = P.sb("oh", [128, 16, 2, 32], BF16)
    PS = [P.ps("ps%d" % i, [128, 512], F32) for i in range(8)]
    pA, pB, pS0, pS1, pO0, pO1, pM0, pM1 = PS
    PN = {id(t): "ps%d" % i for i, t in enumerate(PS)}

    def pn(t):
        return PN[id(t)]

    arena_off = [0]

    def carve(shape, dt):
        n = int(np.prod(shape[1:]))
        words = n if dt in (F32, I32) else (n + 1) // 2
        words = (words + 15) // 16 * 16
        o = arena_off[0]
        arena_off[0] += words
        assert arena_off[0] <= 30 * 1024, ("arena overflow", arena_off[0])
        v = ARENA[0:shape[0], o:o + words]
        if dt != F32:
            v = v.bitcast(dt)
        v = v[:, 0:n]
        if len(shape) == 3:
            v = v.rearrange("p (a b) -> p a b", a=shape[1])
        elif len(shape) == 4:
            v = v.rearrange("p (a b c) -> p a b c", a=shape[1], b=shape[2])
        return v

    def phase():
        P.barrier()
        arena_off[0] = 0

    P.dma("sp", ident[:], CD["ident"], writes=["ident"])
    P.dma("pool", identb[:], CD["ident"], writes=["identb"])
    P.dma("sp", cs32[:], CD["cs32"], writes=["cs32"])
    P.dma("pool", pm32[:], CD["pm32"], writes=["pm32"])
    P.dma("pool", pm64[:], CD["pm64"], writes=["pm64"])
    P.dma("pool", tri[:], CD["tri"], writes=["tri"])
    P.dma("pool", upp[:], CD["upp"], writes=["upp"])
    P.dma("pool", ltri[:], CD["ltri"], writes=["ltri"])
    P.dma("sp", ecap[:], CD["ecap"], writes=["ecap"])
    P.op("dve", lambda e: e.memset(onesb[:], 1.0), writes=["onesb"])
    epsr = P.sb("epsr", [128, 2], F32)
    P.op("dve", lambda e: e.memset(epsr[:, 0:1], RMS_EPS), writes=["epsr"])
    P.op("dve", lambda e: e.memset(epsr[:, 1:2], LN_EPS), writes=["epsr"])

    P.op("dve", lambda e: e.memset(oh[:, :, :, :], 0.0), writes=["oh"])
    xg_half = xg_scr.rearrange("(a p) (h n) -> a h p n", p=128, n=1024)
    oh_flat = oh[:, :, :, :].rearrange("p a b c -> p (a b c)")
    zf_next = [0]

    def zero_fill_some(n):
        for _ in range(n):
            i = zf_next[0]
            if i >= 2 * (NEXP * CAP // 128):
                return
            zf_next[0] += 1
            P.dma("sp", xg_half[i // 2, i % 2], oh_flat, reads=["oh"], writes=["xg_scr"])

    def layer(l, x_src, last):
        phase()
        QTS = [carve([128, S], BF16) for _ in range(2)]
        QR = carve([64, S], BF16)
        KT = [carve([128, S], BF16) for _ in range(2)]
        KR = carve([64, S], BF16)
        VA = carve([128, 16, 2, 129], BF16)
        QLT = carve([128, 3, S], BF16)
        KVLT = carve([128, S], BF16)
        SCR16 = carve([128, 4096], F32)
        WST = [carve([128, 16, 128], BF16) for _ in range(3)]
        PT = [carve([128, 512], BF16) for _ in range(5)]
        mdil = carve([128, S], BF16)
        vcm = mdil
        emat = mdil[0:32, :]
        nbT = carve([32, S], BF16)
        selA = carve([128, 16, 32], F32)
        selB = carve([128, 16, 32], F32)
        imp = carve([128, 16, 32], F32)
        cover = carve([128, 32], F32)
        rt12 = carve([128, 1024], F32)
        rt1 = rt12[0:64, 0:512]
        rt2 = rt12[0:64, 512:1024]
        sqb = carve([128, 3, 512], BF16)
        rstd = carve([128, 512], F32)
        wqu = carve([128, 3, 768], BF16)
        wkvu = carve([128, 1024], BF16)
        qg = carve([128, 4], F32)
        xin = [SCR16[:, 0:2048], SCR16[:, 2048:4096]]
        fin = carve([128, 8], F32)
        ofin = [carve([128, 128], BF16) for _ in range(2)]
        onsa = QLT[:, :, :].rearrange("p a b -> p (a b)")[:, 0:4096].bitcast(F32).rearrange("p (a b) -> p a b", a=16)
        ocl = [carve([128, 128], F32) for _ in range(2)]
        KCT = carve([128, 128], BF16)
        VCA = carve([128, 161], BF16)
        peT = carve([128, 32], F32)
        XPE = QLT[:, :, :].rearrange("p a b -> p (a b)")[:, 0:32 * 127].rearrange("p (a b) -> p a b", a=32)
        hT = carve([128, 2, 127], BF16)
        w2 = carve([128, 2, 128], BF16)
        cskc = carve([32, 2, 128], F32)
        hsc = [carve([128, 127], F32) for _ in range(3)]
        selw = rt12[:, :].rearrange("p (a b) -> p a b", a=32)
        selr = carve([128, 32], F32)

        P.dma("pool", mdil, CD["mdil"], writes=["msk"])
        P.dma("sp", selA, CD["selA"], writes=["selA"])
        P.dma("sp", selB, CD["selB"], writes=["selB"])
        P.dma("sp", cover, CD["cover"], writes=["cover"])
        P.dma("sp", cskc, CD["cskc"], writes=["cskc"])
        cs64 = SCR16[0:64, :].rearrange("p (a b) -> p a b", a=2)
        P.dma("pool", wqu, W["w_q_up"][l].rearrange("(kc p) n -> p kc n", p=128), writes=["wqu"])
        P.dma("pool", wkvu, W["w_kv_up"][l], writes=["wkvu"])
        P.dma("sp", qg[:, 0:3], W["q_lat_norm"][l].rearrange("(kc p) -> p kc", p=128), writes=["qg"], allow_slow_non_contiguous=True)
        P.dma("sp", qg[:, 3:4], W["kv_lat_norm"][l].rearrange("(kc p) -> p kc", p=128), writes=["qg"], allow_slow_non_contiguous=True)
        P.op("dve", lambda e: e.memset(VA[:, :, :, 128:129], 1.0), writes=["va0", "va1"])
        P.op("dve", lambda e: e.memset(imp, 0.0), writes=["imp"])

        for t in range(NT):
            xt_ = xin[t % 2]
            P.dma("sp", xt_, x_src[t * 128:(t + 1) * 128, :], writes=["xin%d" % (t % 2)])
            for g in range(4):
                pb = (pA, pB)[(t * 4 + g) % 2]
                P.op("pe", (lambda pb=pb, xt_=xt_, g=g: lambda e: [e.transpose(out=pb[:, j * 128:(j + 1) * 128], in_=xt_[:, (g * 4 + j) * 128:(g * 4 + j + 1) * 128], identity=ident[:]) for j in range(4)][-1])(),
                     reads=["xin%d" % (t % 2), "ident"], writes=[pn(pb)])
                eng = "act" if g % 2 == 0 else "dve"
                dst = XT[:, g * 4:(g + 1) * 4, t * 128:(t + 1) * 128]
                src = pb[:, :].rearrange("p (a b) -> p a b", a=4)
                if eng == "act":
                    P.op("act", lambda e, dst=dst, src=src: e.activation(out=dst, in_=src, func=AF.Copy), writes=[pn(pb), "XT"])
                else:
                    P.op("dve", lambda e, dst=dst, src=src: e.tensor_copy(out=dst, in_=src), writes=[pn(pb), "XT"])

        P.dma("sp", cs64, CD["cs64"], writes=["scr16", "xin0", "xin1"])
        wst_i = [0]
        pb_i = [0]

        def next_pb():
            pb_i[0] += 1
            return (pA, pB)[pb_i[0] % 2]

        def load_w(col0, ncols):
            s = wst_i[0] % 3
            wst_i[0] += 1
            P.dma("pool", WST[s], W["w_in"][l, W_CHUNK_IDX[(col0, ncols)]].rearrange("p (kc n) -> p kc n", kc=16),
                  writes=["wst%d" % s])
            return s

        def proj_fm(col0, ncols, evac):
            s = load_w(col0, ncols)
            for tc in range(4):
                pb = next_pb()
                mms = [(pb[0:ncols, :], WST[s][:, kc, 0:ncols], XT[:, kc, tc * 512:(tc + 1) * 512], kc == 0, kc == 15) for kc in range(16)]
                P.op("pe", pe_group(mms), reads=["wst%d" % s, "XT"], writes=[pn(pb)])
                evac(pb, tc)

        def proj_tm(col0, ncols, evac):
            s = load_w(col0, ncols)
            for kt in range(NT):
                pb = next_pb()
                mms = [(pb[:, 0:ncols], XT[:, kc, kt * 128:(kt + 1) * 128], WST[s][:, kc, 0:ncols], kc == 0, kc == 15) for kc in range(16)]
                P.op("pe", pe_group(mms), reads=["wst%d" % s, "XT"], writes=[pn(pb)])
                evac(pb, kt)

        def rope_evac(dst, dname, nrows, R, cs, csname, pm, pmname):
            def ev(pb, tc, ncol=512, c0=None):
                c0 = tc * 512 if c0 is None else c0
                sl = slice(c0, c0 + ncol)
                tn = "%s.%d" % (dname, tc)
                P.op("act", lambda e: e.activation(out=dst[0:nrows, sl], in_=pb[0:nrows, 0:ncol], func=AF.Copy), writes=[pn(pb), tn])
                pm_ = pM0
                P.op("pe", lambda e: e.matmul(pm_[0:R, 0:ncol], lhsT=pm[0:R, 0:R], rhs=dst[0:R, sl], start=True, stop=True),
                     reads=[tn, pmname], writes=[pn(pm_)])
                P.op("dve", lambda e: e.tensor_tensor(out=rt1[0:R, 0:ncol], in0=dst[0:R, sl], in1=cs[0:R, 0, sl], op=ALU.mult),
                     reads=[tn, csname], writes=["rt1"])
                P.op("dve", lambda e: e.tensor_tensor(out=rt2[0:R, 0:ncol], in0=pm_[0:R, 0:ncol], in1=cs[0:R, 1, sl], op=ALU.mult),
                     reads=[csname], writes=["rt2", pn(pm_)])
                P.op("pool", lambda e: e.tensor_tensor(out=dst[0:R, sl], in0=rt1[0:R, 0:ncol], in1=rt2[0:R, 0:ncol], op=ALU.add),
                     reads=["rt1", "rt2"], writes=[tn])
            return ev

        def copy_evac(dst, dname, nrows):
            def ev(pb, tc):
                sl = slice(tc * 512, (tc + 1) * 512)
                P.op("act", lambda e: e.activation(out=dst[0:nrows, sl], in_=pb[0:nrows, :], func=AF.Copy),
                     writes=[pn(pb), "%s.%d" % (dname, tc)])
            return ev

        def v_evac(slot, ncols=128):
            def ev(pb, kt):
                P.op("dve", lambda e: e.tensor_copy(out=VA[:, kt, slot, 0:ncols], in_=pb[:, 0:ncols]), writes=[pn(pb), "va%d" % slot])
            return ev

        def names4(n):
            return ["%s.%d" % (n, i) for i in range(4)]

        pt_i = [0]
        ps_i = [0]
        po_i = [0]

        def attn(qparts, kparts, vslot, scale, kts_fn, mask_fn, W_out, finalize, nk=128, bias=False, vsrc=None, vname=None):
            qreads = [n for (_, _, nm) in qparts for n in nm]
            kreads = [n for (_, _, nm) in kparts for n in nm]
            vname_ = vname or ("va%d" % vslot)
            items = []
            for qt in range(NT):
                kts = kts_fn(qt)
                po = (pO0, pO1)[po_i[0] % 2]
                po_i[0] += 1
                groups = [kts[i:i + 4] for i in range(0, len(kts), 4)]
                for gi, grp in enumerate(groups):
                    psb = (pS0, pS1, pM1, pM0)[ps_i[0] % 4]
                    ps_i[0] += 1
                    pts = pt_i[0] % 5
                    pt_i[0] += 1
                    items.append((qt, gi, grp, len(groups), po, psb, pts))

            def stage1(it):
                qt, gi, grp, ng, po, psb, pts = it
                qs = slice(qt * 128, (qt + 1) * 128)
                mms = []
                for j, kt in enumerate(grp):
                    o = psb[0:nk, j * 128:(j + 1) * 128]
                    np_ = len(qparts)
                    for pi in range(np_):
                        qa, K, _ = qparts[pi]
                        ka, _, _ = kparts[pi]
                        mms.append((o, ka[0:K, kt * 128:kt * 128 + nk], qa[0:K, qs], pi == 0, (pi == np_ - 1) and not bias))
                    if bias:
                        mms.append((o, emat[0:32, kt * 128:(kt + 1) * 128], nbT[0:32, qs], False, True))
                P.op("pe", pe_group(mms), reads=qreads + kreads + (["msk", "nbT"] if bias else []), writes=[pn(psb)])
                n = len(grp) * 128
                ptn = "pt%d" % pts
                P.op("act", lambda e: e.activation(out=PT[pts][0:nk, 0:n], in_=psb[0:nk, 0:n], func=AF.Exp, scale=scale),
                     writes=[pn(psb), ptn])
                for (eng, fn, rd) in mask_fn(qt, grp, PT[pts]):
                    P.op(eng, fn, reads=rd, writes=[ptn])

            def stage2(it):
                qt, gi, grp, ng, po, psb, pts = it
                ptn = "pt%d" % pts
                mms = []
                for j, kt in enumerate(grp):
                    vv = VA[0:nk, kt, vslot, 0:W_out] if vsrc is None else vsrc
                    mms.append((po[:, 0:W_out], PT[pts][0:nk, j * 128:(j + 1) * 128], vv,
                                gi == 0 and j == 0, gi == ng - 1 and j == len(grp) - 1))
                P.op("pe", pe_group(mms), reads=[ptn, vname_], writes=[pn(po)])
                if gi == ng - 1:
                    finalize(qt, po)

            SK = 3
            for i in range(len(items) + SK):
                if i < len(items):
                    stage1(items[i])
                if i >= SK:
                    stage2(items[i - SK])

        def causal_mask(qt, grp, pt):
            ops = []
            if grp[-1] == qt:
                j = len(grp) - 1
                ops.append(("dve", lambda e, j=j, pt=pt: e.tensor_tensor(out=pt[:, j * 128:(j + 1) * 128], in0=pt[:, j * 128:(j + 1) * 128], in1=tri[:], op=ALU.mult), ["tri"]))
            return ops

        def fin_simple(colbase):
            def f(qt, po):
                of = ofin[qt % 2]
                ofn = "ofin%d" % (qt % 2)
                fc_ = fin[:, 4 + (qt % 2):5 + (qt % 2)]
                fcn = "fin%d" % (qt % 2)
                P.op("dve", lambda e: e.reciprocal(out=fc_, in_=po[:, 128:129]), writes=[pn(po), fcn])
                P.op("dve", lambda e: e.tensor_scalar(out=of[:], in0=po[:, 0:128], scalar1=fc_, scalar2=None, op0=ALU.mult),
                     reads=[fcn], writes=[pn(po), ofn])
                P.dma("sp", o_scr[qt * 128:(qt + 1) * 128, colbase:colbase + 128], of[:], reads=[ofn], writes=["o_scr"])
            return f

        def qlat_evac(c):
            def ev(pb, tc):
                sl = slice(tc * 512, (tc + 1) * 512)
                P.op("act", lambda e: e.activation(out=QLT[:, c, sl], in_=pb[:, :], func=AF.Copy), writes=[pn(pb), "qlt.%d" % tc])
            return ev
        for c in range(3):
            proj_fm(C_QLAT + 128 * c, 128, qlat_evac(c))
        proj_fm(C_KVLAT, 128, copy_evac(KVLT, "kvlt", 128))
        proj_fm(C_KROPE, 64, rope_evac(KR, "kr", 64, 64, cs64, "scr16", pm64, "pm64"))

        def rms_apply(views, nfeat, gcols, tnames_fn):
            for tc in range(4):
                sl = slice(tc * 512, (tc + 1) * 512)
                n = len(views)
                for c in range(n):
                    P.op("dve", lambda e, c=c, sl=sl: e.tensor_tensor(out=sqb[:, c, :], in0=views[c][:, sl], in1=views[c][:, sl], op=ALU.mult),
                         reads=[tnames_fn(tc)], writes=["sqb"])
                mms = [(pM1[:, :], onesb[:, :], sqb[:, c, :], c == 0, c == n - 1) for c in range(n)]
                P.op("pe", pe_group(mms), reads=["sqb", "onesb"], writes=[pn(pM1)])
                P.op("act", lambda e: e.activation(out=rstd[:], in_=pM1[:, :], func=AF.Ln, scale=1.0 / nfeat, bias=epsr[:, 0:1]),
                     reads=["epsr"], writes=[pn(pM1), "rstd"])
                P.op("act", lambda e: e.activation(out=rstd[:], in_=rstd[:], func=AF.Exp, scale=-0.5), writes=["rstd"])
                for c in range(n):
                    P.op("dve", lambda e, c=c, sl=sl: e.scalar_tensor_tensor(out=views[c][:, sl], in0=views[c][:, sl], scalar=qg[:, gcols[c]:gcols[c] + 1], in1=rstd[:], op0=ALU.mult, op1=ALU.mult),
                         reads=["rstd", "qg"], writes=[tnames_fn(tc)])
        rms_apply([QLT[:, 0, :], QLT[:, 1, :], QLT[:, 2, :]], 384.0, [0, 1, 2], lambda tc: "qlt.%d" % tc)
        rms_apply([KVLT], 128.0, [3], lambda tc: "kvlt.%d" % tc)

        sc_mla = 192.0 ** -0.5
        for h in range(4):
            qs_, ks_ = h % 2, h % 2
            for tc in range(4):
                sl = slice(tc * 512, (tc + 1) * 512)
                pb = next_pb()
                mms = [(pb[:, :], wqu[:, c, h * 192:h * 192 + 128], QLT[:, c, sl], c == 0, c == 2) for c in range(3)]
                P.op("pe", pe_group(mms), reads=["wqu", "qlt.%d" % tc], writes=[pn(pb)])
                copy_evac(QTS[qs_], "qts%d" % qs_, 128)(pb, tc)
                pb = next_pb()
                mms = [(pb[0:64, :], wqu[:, c, h * 192 + 128:h * 192 + 192], QLT[:, c, sl], c == 0, c == 2) for c in range(3)]
                P.op("pe", pe_group(mms), reads=["wqu", "qlt.%d" % tc], writes=[pn(pb)])
                rope_evac(QR, "qr", 64, 64, cs64, "scr16", pm64, "pm64")(pb, tc)
                pb = next_pb()
                P.op("pe", pe_group([(pb[:, :], wkvu[:, h * 256:h * 256 + 128], KVLT[:, sl], True, True)]), reads=["wkvu", "kvlt.%d" % tc], writes=[pn(pb)])
                copy_evac(KT[ks_], "kt%d" % ks_, 128)(pb, tc)
            for kt in range(NT):
                pb = next_pb()
                P.op("pe", pe_group([(pb[:, 0:128], KVLT[:, kt * 128:(kt + 1) * 128], wkvu[:, h * 256 + 128:h * 256 + 256], True, True)]),
                     reads=["wkvu"] + names4("kvlt"), writes=[pn(pb)])
                v_evac(h % 2)(pb, kt)
            attn([(QTS[qs_], 128, names4("qts%d" % qs_)), (QR, 64, names4("qr"))],
                 [(KT[ks_], 128, names4("kt%d" % ks_)), (KR, 64, names4("kr"))],
                 h % 2, sc_mla, lambda qt: list(range(qt + 1)), causal_mask, 129, fin_simple(h * 128))
            if l == 0:
                zero_fill_some(8)

        sc = 128.0 ** -0.5

        def dil_mask(qt, grp, pt):
            n = len(grp) * 128
            i0 = (15 - qt + grp[0]) * 128
            return [("dve", lambda e: e.tensor_tensor(out=pt[:, 0:n], in0=pt[:, 0:n], in1=mdil[:, i0:i0 + n], op=ALU.mult), ["msk"])]
        for h in range(6):
            s_ = h % 2
            proj_fm(C_DQ + 128 * h, 128, rope_evac(QTS[s_], "qts%d" % s_, 128, 32, cs32, "cs32", pm32, "pm32"))
            proj_fm(C_DK + 128 * h, 128, rope_evac(KT[s_], "kt%d" % s_, 128, 32, cs32, "cs32", pm32, "pm32"))
            proj_tm(C_DV + 128 * h, 128, v_evac(s_))
            if stage == "pdbg":
                P.barrier()
                P.dma("sp", dbg3_d[0], XT[:, 0, :], writes=["dbg3"])
                P.dma("sp", dbg3_d[1], QTS[0], writes=["dbg3"])
                P.dma("sp", dbg3_d[2], KT[0], writes=["dbg3"])
                P.dma("sp", dbg3_d[3].rearrange("p (a b) -> p a b", a=16), VA[:, :, 0, 0:128], writes=["dbg3"])
                P.dma("sp", dbg3_d[4], KVLT, writes=["dbg3"])
                P.dma("sp", dbg3_d[5], QLT[:, 0, :], writes=["dbg3"])
                P.dma("sp", dbg3_d[6, 0:64], KR, writes=["dbg3"])
                P.dma("sp", dbg3_d[7].rearrange("p (a b) -> p a b", a=16), VA[:, :, 0, 1:129], writes=["dbg3"])
                return "stop"
            attn([(QTS[s_], 128, names4("qts%d" % s_))], [(KT[s_], 128, names4("kt%d" % s_))], s_, sc,
                 lambda qt: list(range(qt + 1)), dil_mask, 129, fin_simple(512 + h * 128))
            if l == 0:
                zero_fill_some(8)

        def gate_evac(pb, kt):
            P.op("act", lambda e: e.activation(out=gates[:, kt, :], in_=pb[:, 0:18], func=AF.Sigmoid), writes=[pn(pb), "gates"])
        proj_tm(C_GATE, 18, gate_evac)

        w1 = SCR16[:, :].bitcast(BF16).rearrange("p (a b) -> p a b", a=32)
        for which in range(2):
            col = (C_NKC, C_NVC)[which]
            src = KT[which]
            proj_fm(col, 128, copy_evac(src, "kt%d" % which, 128))
            P.dma("sp", peT, W[("cmp_pos_k", "cmp_pos_v")[which]][l].rearrange("i d -> d i"), writes=["peT"], allow_slow_non_contiguous=True)
            P.dma("pool", w1, W[("cmp_w1_k", "cmp_w1_v")[which]][l].rearrange("(i d) j -> d i j", d=128), writes=["scr16"])
            P.dma("pool", w2, W[("cmp_w2_k", "cmp_w2_v")[which]][l].rearrange("(jc p) d -> p jc d", p=128), writes=["w2"])
            ovl = bass.AP(src.tensor, src.offset, [list(src.ap[0]), [1, 32], [16, 127]])
            P.op("dve", lambda e, ovl=ovl: e.tensor_tensor(out=XPE, in0=ovl, in1=peT.unsqueeze(2).broadcast_to([128, 32, 127]), op=ALU.add),
                 reads=names4("kt%d" % which) + ["peT"], writes=["xpe"])
            for jc in range(2):
                pb = next_pb()
                mms = [(pb[:, 0:127], w1[:, i, jc * 128:(jc + 1) * 128], XPE[:, i, :], i == 0, i == 31) for i in range(32)]
                P.op("pe", pe_group(mms), reads=["scr16", "xpe"], writes=[pn(pb)])
                P.op("act", lambda e, pb=pb: e.activation(out=hsc[0], in_=pb[:, 0:127], func=AF.Square), writes=[pn(pb), "hsc0"])
                P.op("dve", lambda e: e.tensor_scalar(out=hsc[0], in0=hsc[0], scalar1=0.044715, scalar2=1.0, op0=ALU.mult, op1=ALU.add), writes=["hsc0"])
                P.op("dve", lambda e, pb=pb: e.tensor_tensor(out=hsc[0], in0=hsc[0], in1=pb[:, 0:127], op=ALU.mult), writes=["hsc0", pn(pb)])
                P.op("act", lambda e: e.activation(out=hsc[1], in_=hsc[0], func=AF.Sigmoid, scale=1.5957691216057308), reads=["hsc0"], writes=["hsc1"])
                P.op("dve", lambda e, pb=pb, jc=jc: e.tensor_tensor(out=hT[:, jc, :], in0=hsc[1], in1=pb[:, 0:127], op=ALU.mult), reads=["hsc1"], writes=["hT", pn(pb)])
            if which == 0:
                pb = next_pb()
                mms = [(pb[:, 0:127], w2[:, jc, :], hT[:, jc, :], jc == 0, jc == 1) for jc in range(2)]
                P.op("pe", pe_group(mms), reads=["w2", "hT"], writes=[pn(pb)])
                rope_evac(KCT, "kct", 128, 32, cskc, "cskc", pm32, "pm32")(pb, 0, ncol=127, c0=0)
            else:
                pb = next_pb()
                mms = [(pb[0:127, 0:128], hT[:, jc, :], w2[:, jc, :], jc == 0, jc == 1) for jc in range(2)]
                P.op("pe", pe_group(mms), reads=["w2", "hT"], writes=[pn(pb)])
                P.op("dve", lambda e, pb=pb: e.tensor_copy(out=VCA[0:127, 0:128], in_=pb[0:127, 0:128]), writes=[pn(pb), "vca"])
                P.op("dve", lambda e: e.memset(VCA[0:127, 128:129], 1.0), writes=["vca"])
                P.op("dve", lambda e: e.tensor_copy(out=VCA[0:127, 129:161], in_=cover[0:127, :]), reads=["cover"], writes=["vca"])

        P.dma("pool", vcm, CD["vcm"], writes=["msk"])
        def cmp_mask(qt, grp, pt):
            return [("dve", lambda e: e.tensor_tensor(out=pt[0:127, 0:128], in0=pt[0:127, 0:128], in1=vcm[0:127, qt * 128:(qt + 1) * 128], op=ALU.mult), ["msk"])]
        for h in range(6):
            s_ = h % 2
            proj_fm(C_NQ + 128 * h, 128, rope_evac(QTS[s_], "qts%d" % s_, 128, 32, cs32, "cs32", pm32, "pm32"))
            P.dma("sp", nq_scr[h], QTS[s_], reads=names4("qts%d" % s_), writes=["nq_scr%d" % h])

            def fin_cmp(qt, po, h=h):
                oc = ocl[qt % 2]
                ocn = "ocl%d" % (qt % 2)
                P.op("dve", lambda e: e.tensor_scalar(out=fin[:, 0:1], in0=po[:, 128:129], scalar1=1e-30, scalar2=None, op0=ALU.max), writes=[pn(po), "fin"])
                P.op("dve", lambda e: e.reciprocal(out=fin[:, 1:2], in_=fin[:, 0:1]), writes=["fin"])
                P.op("dve", lambda e: e.scalar_tensor_tensor(out=imp[:, qt, :], in0=po[:, 129:161], scalar=fin[:, 1:2], in1=imp[:, qt, :], op0=ALU.mult, op1=ALU.add),
                     reads=["fin"], writes=[pn(po), "imp"])
                P.op("dve", lambda e: e.tensor_tensor(out=fin[:, 2:3], in0=fin[:, 1:2], in1=gates[:, qt, 3 * h:3 * h + 1], op=ALU.mult), reads=["gates"], writes=["fin"])
                P.op("dve", lambda e: e.tensor_scalar(out=oc[:], in0=po[:, 0:128], scalar1=fin[:, 2:3], scalar2=None, op0=ALU.mult), reads=["fin"], writes=[pn(po), ocn])
                P.dma("sp", ocmp_scr[h, qt * 128:(qt + 1) * 128, :], oc[:], reads=[ocn], writes=["ocmp_scr%d" % h])
            attn([(QTS[s_], 128, names4("qts%d" % s_))], [(KCT, 128, ["kct.0"])], 0, sc, lambda qt: [0], cmp_mask, 161, fin_cmp,
                 nk=127, vsrc=VCA[0:127, 0:161], vname="vca")
            if l == 0:
                zero_fill_some(8)

        for qt in range(NT):
            P.op("dve", lambda e, qt=qt: e.tensor_tensor(out=selr[:], in0=imp[:, qt, :], in1=selA[:, qt, :], op=ALU.mult), reads=["imp", "selA"], writes=["selr"])
            P.op("dve", lambda e, qt=qt: e.tensor_tensor(out=selr[:], in0=selr[:], in1=selB[:, qt, :], op=ALU.add), reads=["selB"], writes=["selr"])
            P.op("dve", lambda e: e.tensor_tensor(out=selw, in0=selr[:].unsqueeze(1).broadcast_to([128, 32, 32]), in1=selr[:].unsqueeze(2).broadcast_to([128, 32, 32]), op=ALU.is_gt),
                 reads=["selr"], writes=["selw", "rt1", "rt2"])
            P.op("dve", lambda e: e.tensor_reduce(out=selr[:], in_=selw, axis=AX.X, op=ALU.add), reads=["selw"], writes=["selr"])
            P.op("dve", lambda e: e.tensor_scalar(out=selr[:], in0=selr[:], scalar1=15.5, scalar2=-30000.0, op0=ALU.is_gt, op1=ALU.mult), writes=["selr"])
            P.op("pe", lambda e: e.transpose(out=pM1[0:32, 0:128], in_=selr[:], identity=ident[:]), reads=["selr", "ident"], writes=[pn(pM1)])
            P.op("act", lambda e, qt=qt: e.activation(out=nbT[0:32, qt * 128:(qt + 1) * 128], in_=pM1[0:32, 0:128], func=AF.Copy), writes=[pn(pM1), "nbT"])

        P.dma("pool", emat, CD["emat"], writes=["msk"])
        proj_fm(C_NKS, 128, rope_evac(KT[0], "kt0", 128, 32, cs32, "cs32", pm32, "pm32"))
        proj_fm(C_NKW, 128, rope_evac(KT[1], "kt1", 128, 32, cs32, "cs32", pm32, "pm32"))
        proj_tm(C_NVS, 128, v_evac(0))
        proj_tm(C_NVW, 128, v_evac(1))

        def win_mask(qt, grp, pt):
            ops = []
            for j, kt in enumerate(grp):
                if kt == qt:
                    ops.append(("dve", lambda e, j=j: e.tensor_tensor(out=pt[:, j * 128:(j + 1) * 128], in0=pt[:, j * 128:(j + 1) * 128], in1=tri[:], op=ALU.mult), ["tri"]))
                elif kt == qt - 4:
                    ops.append(("dve", lambda e, j=j: e.tensor_tensor(out=pt[:, j * 128:(j + 1) * 128], in0=pt[:, j * 128:(j + 1) * 128], in1=upp[:], op=ALU.mult), ["upp"]))
            return ops
        for h in range(6):
            s_ = h % 2
            P.dma("sp", QTS[s_], nq_scr[h], reads=["nq_scr%d" % h], writes=names4("qts%d" % s_))

            def fin_slc(qt, po, h=h):
                P.op("dve", lambda e: e.tensor_scalar(out=fin[:, 0:1], in0=po[:, 128:129], scalar1=1e-30, scalar2=None, op0=ALU.max), writes=[pn(po), "fin"])
                P.op("dve", lambda e: e.reciprocal(out=fin[:, 1:2], in_=fin[:, 0:1]), writes=["fin"])
                P.op("dve", lambda e: e.tensor_tensor(out=fin[:, 2:3], in0=fin[:, 1:2], in1=gates[:, qt, 3 * h + 1:3 * h + 2], op=ALU.mult), reads=["gates"], writes=["fin"])
                P.op("dve", lambda e: e.tensor_scalar(out=onsa[:, qt, :], in0=po[:, 0:128], scalar1=fin[:, 2:3], scalar2=None, op0=ALU.mult), reads=["fin"], writes=[pn(po), "onsa"])
            attn([(QTS[s_], 128, names4("qts%d" % s_))], [(KT[0], 128, names4("kt0"))], 0, sc, lambda qt: list(range(qt + 1)), causal_mask, 129, fin_slc, bias=True)

            def fin_win(qt, po, h=h):
                oc = ocl[qt % 2]
                ocn = "ocl%d" % (qt % 2)
                of = ofin[qt % 2]
                ofn = "ofin%d" % (qt % 2)
                P.dma("sp", oc[:], ocmp_scr[h, qt * 128:(qt + 1) * 128, :], reads=["ocmp_scr%d" % h], writes=[ocn])
                P.op("dve", lambda e: e.tensor_scalar(out=fin[:, 0:1], in0=po[:, 128:129], scalar1=1e-30, scalar2=None, op0=ALU.max), writes=[pn(po), "fin"])
                P.op("dve", lambda e: e.reciprocal(out=fin[:, 1:2], in_=fin[:, 0:1]), writes=["fin"])
                P.op("dve", lambda e: e.tensor_tensor(out=fin[:, 2:3], in0=fin[:, 1:2], in1=gates[:, qt, 3 * h + 2:3 * h + 3], op=ALU.mult), reads=["gates"], writes=["fin"])
                P.op("dve", lambda e: e.scalar_tensor_tensor(out=onsa[:, qt, :], in0=po[:, 0:128], scalar=fin[:, 2:3], in1=onsa[:, qt, :], op0=ALU.mult, op1=ALU.add),
                     reads=["fin"], writes=[pn(po), "onsa"])
                P.op("dve", lambda e: e.tensor_tensor(out=of[:], in0=onsa[:, qt, :], in1=oc[:], op=ALU.add), reads=["onsa", ocn], writes=[ofn])
                P.dma("sp", o_scr[qt * 128:(qt + 1) * 128, 1280 + h * 128:1280 + (h + 1) * 128], of[:], reads=[ofn], writes=["o_scr"])
            attn([(QTS[s_], 128, names4("qts%d" % s_))], [(KT[1], 128, names4("kt1"))], 1, sc, lambda qt: list(range(max(0, qt - 4), qt + 1)), win_mask, 129, fin_win)

        if l == 0:
            zero_fill_some(1000)
        phase()
        WO = XT
        P.dma("pool", WO[:, :, :], W["w_out"][l].rearrange("(kc p) n -> p kc n", p=128), writes=["WO"])
        lng = carve([128, D], F32)
        lnb = carve([128, D], F32)
        P.dma("sp", lng, W["ln1_g"][l:l + 1, :].broadcast_to([128, D]), writes=["lng"])
        P.dma("sp", lnb, W["ln1_b"][l:l + 1, :].broadcast_to([128, D]), writes=["lnb"])
        otl = [carve([128, D], BF16) for _ in range(2)]
        oT = [carve([128, 16, 128], BF16) for _ in range(2)]
        xtl = [carve([128, D], F32) for _ in range(2)]
        ytl = [carve([128, D], F32) for _ in range(2)]
        x1b = [carve([128, D], BF16) for _ in range(2)]
        x1T = carve([128, 16, 128], F32)
        wr = carve([128, 16, 36], F32)
        st = carve([128, 16], F32)
        rl = carve([128, 64], F32)
        P.dma("sp", wr[:, :, 0:4], W["w_grp"][l].rearrange("(kc p) n -> p kc n", p=128), writes=["wr"])
        P.dma("sp", wr[:, :, 4:36], W["w_exp"][l].rearrange("(kc p) n -> p kc n", p=128), writes=["wr"])
        psT = [pS0[:, :].bitcast(BF16), pS1[:, :].bitcast(BF16)]

        def layer_norm(y, yn, g, gname, b, bname, out, outn):
            P.op("dve", lambda e: e.tensor_reduce(out=st[:, 0:1], in_=y, axis=AX.X, op=ALU.add), reads=[yn], writes=["st"])
            P.op("dve", lambda e: e.tensor_scalar(out=st[:, 1:2], in0=st[:, 0:1], scalar1=-1.0 / D, scalar2=None, op0=ALU.mult), writes=["st"])
            P.op("act", lambda e: e.activation(out=y, in_=y, func=AF.Identity, bias=st[:, 1:2], scale=1.0), reads=["st"], writes=[yn])
            P.op("dve", lambda e: e.memset(st[:, 2:3], 0.0), writes=["st"])
            P.op("act", lambda e: e.activation(out=out, in_=y, func=AF.Square, accum_out=st[:, 2:3]), reads=[yn], writes=[outn, "st"])
            P.op("act", lambda e: e.activation(out=st[:, 3:4], in_=st[:, 2:3], func=AF.Ln, scale=1.0 / D, bias=epsr[:, 1:2]), reads=["epsr"], writes=["st"])
            P.op("act", lambda e: e.activation(out=st[:, 3:4], in_=st[:, 3:4], func=AF.Exp, scale=-0.5), writes=["st"])
            P.op("dve", lambda e: e.scalar_tensor_tensor(out=out, in0=y, scalar=st[:, 3:4], in1=g, op0=ALU.mult, op1=ALU.mult), reads=[yn, "st", gname], writes=[outn])
            P.op("pool", lambda e: e.tensor_tensor(out=out, in0=out, in1=b, op=ALU.add), reads=[bname], writes=[outn])

        for t in range(NT):
            i2 = t % 2
            rows = slice(t * 128, (t + 1) * 128)
            P.dma("sp", otl[i2], o_scr[rows, :], reads=["o_scr"], writes=["otl%d" % i2])
            P.dma("sp", xtl[i2], x_src[rows, :], writes=["xtl%d" % i2])
            if stage == "odbg":
                P.op("pool", lambda e, i2=i2: e.tensor_copy(out=ytl[i2], in_=otl[i2]), reads=["otl%d" % i2], writes=["ytl%d" % i2])
                P.dma("sp", dbg2_d[rows, :], ytl[i2], reads=["ytl%d" % i2], writes=["dbg2"])
            for g in range(2):
                pt_ = psT[g]
                pnm = pn((pS0, pS1)[g])
                P.op("pe", (lambda pt_=pt_, g=g, i2=i2: lambda e: [e.transpose(out=pt_[:, j * 128:(j + 1) * 128], in_=otl[i2][:, (g * 8 + j) * 128:(g * 8 + j + 1) * 128], identity=identb[:]) for j in range(8)][-1])(),
                     reads=["otl%d" % i2, "identb"], writes=[pnm])
                P.op("act", lambda e, pt_=pt_, g=g, i2=i2: e.activation(out=oT[i2][:, g * 8:(g + 1) * 8, :], in_=pt_[:, 0:1024].rearrange("p (a b) -> p a b", a=8), func=AF.Copy),
                     writes=[pnm, "oT%d" % i2])
            for cc in range(4):
                pb = next_pb()
                mms = [(pb[:, :], oT[i2][:, kc, :], WO[:, kc, cc * 512:(cc + 1) * 512], kc == 0, kc == 15) for kc in range(16)]
                P.op("pe", pe_group(mms), reads=["oT%d" % i2, "WO"], writes=[pn(pb)])
                P.op("dve", lambda e, pb=pb, cc=cc, i2=i2: e.scalar_tensor_tensor(out=ytl[i2][:, cc * 512:(cc + 1) * 512], in0=xtl[i2][:, cc * 512:(cc + 1) * 512], scalar=ALPHA, in1=pb[:, :], op0=ALU.mult, op1=ALU.add),
                     reads=["xtl%d" % i2], writes=[pn(pb), "ytl%d" % i2])
            layer_norm(ytl[i2], "ytl%d" % i2, lng, "lng", lnb, "lnb", xtl[i2], "xtl%d" % i2)
            P.dma("sp", x1_scr[rows, :], xtl[i2], reads=["xtl%d" % i2], writes=["x1_scr"])
            if stage != "full" and l == 0:
                P.dma("sp", dbg_d[rows, :], xtl[i2], reads=["xtl%d" % i2], writes=["dbg"])
            P.op("pool", lambda e, i2=i2: e.tensor_copy(out=x1b[i2], in_=xtl[i2]), reads=["xtl%d" % i2], writes=["x1b%d" % i2])
            P.dma("sp", x1b_scr[rows, :], x1b[i2], reads=["x1b%d" % i2], writes=["x1b_scr"])
            for g in range(4):
                pm_ = (pM0, pM1)[g % 2]
                P.op("pe", (lambda pm_=pm_, g=g, i2=i2: lambda e: [e.transpose(out=pm_[:, j * 128:(j + 1) * 128], in_=xtl[i2][:, (g * 4 + j) * 128:(g * 4 + j + 1) * 128], identity=ident[:]) for j in range(4)][-1])(),
                     reads=["xtl%d" % i2, "ident"], writes=[pn(pm_)])
                P.op("act", lambda e, pm_=pm_, g=g: e.activation(out=x1T[:, g * 4:(g + 1) * 4, :], in_=pm_[:, :].rearrange("p (a b) -> p a b", a=4), func=AF.Copy),
                     writes=[pn(pm_), "x1T"])
            mms = [(pO0[:, 0:36], x1T[:, kc, :], wr[:, kc, :], kc == 0, kc == 15) for kc in range(16)]
            P.op("pe", pe_group(mms), reads=["x1T", "wr"], writes=[pn(pO0)])
            router(t, pO0, rl)

    def router(t, pl, rl):
        V = lambda f, reads=(), writes=("rl",): P.op("dve", f, reads=reads, writes=writes)
        lg = rl[:, 0:36]
        V(lambda e: e.tensor_copy(out=lg, in_=pl[:, 0:36]), writes=["rl", pn(pl)])
        V(lambda e: e.tensor_reduce(out=rl[:, 36:37], in_=rl[:, 0:4], axis=AX.X, op=ALU.max))
        V(lambda e: e.tensor_scalar(out=rl[:, 40:44], in0=rl[:, 0:4], scalar1=rl[:, 36:37], scalar2=None, op0=ALU.subtract))
        V(lambda e: e.memset(rl[:, 37:38], 0.0))
        P.op("act", lambda e: e.activation(out=rl[:, 44:48], in_=rl[:, 40:44], func=AF.Exp, accum_out=rl[:, 37:38]), writes=["rl"])
        V(lambda e: e.reciprocal(out=rl[:, 38:39], in_=rl[:, 37:38]))
        V(lambda e: e.tensor_scalar(out=rl[:, 40:44], in0=rl[:, 40:44], scalar1=0.0, scalar2=1e30, op0=ALU.is_lt, op1=ALU.mult))
        em = rl[:, 4:36].rearrange("p (g k) -> p g k", g=4)
        V(lambda e: e.tensor_tensor(out=em, in0=em, in1=rl[:, 40:44].unsqueeze(2).broadcast_to([128, 4, 8]), op=ALU.subtract))
        V(lambda e: e.tensor_reduce(out=rl[:, 48:49], in_=rl[:, 4:36], axis=AX.X, op=ALU.max))
        V(lambda e: e.tensor_scalar(out=oh[:, t, 0, :], in0=rl[:, 4:36], scalar1=rl[:, 48:49], scalar2=None, op0=ALU.is_equal), writes=["rl", "oh"])
        V(lambda e: e.scalar_tensor_tensor(out=rl[:, 4:36], in0=oh[:, t, 0, :], scalar=-1e30, in1=rl[:, 4:36], op0=ALU.mult, op1=ALU.add), reads=["oh"])
        V(lambda e: e.tensor_reduce(out=rl[:, 49:50], in_=rl[:, 4:36], axis=AX.X, op=ALU.max))
        V(lambda e: e.tensor_scalar(out=oh[:, t, 1, :], in0=rl[:, 4:36], scalar1=rl[:, 49:50], scalar2=None, op0=ALU.is_equal), writes=["rl", "oh"])
        V(lambda e: e.tensor_tensor(out=mk[:, t, :], in0=oh[:, t, 0, :], in1=oh[:, t, 1, :], op=ALU.add), reads=["oh"], writes=["mk"])
        V(lambda e: e.tensor_tensor(out=rl[:, 50:51], in0=rl[:, 49:50], in1=rl[:, 48:49], op=ALU.subtract))
        P.op("act", lambda e: e.activation(out=rl[:, 51:52], in_=rl[:, 50:51], func=AF.Exp), writes=["rl"])
        V(lambda e: e.tensor_scalar(out=rl[:, 51:52], in0=rl[:, 51:52], scalar1=1.0, scalar2=None, op0=ALU.add))
        V(lambda e: e.reciprocal(out=rl[:, 52:53], in_=rl[:, 51:52]))
        V(lambda e: e.tensor_tensor(out=gatew[:, t, 0:1], in0=rl[:, 52:53], in1=rl[:, 38:39], op=ALU.mult), writes=["rl", "gatew"])
        V(lambda e: e.tensor_tensor(out=gatew[:, t, 1:2], in0=rl[:, 38:39], in1=gatew[:, t, 0:1], op=ALU.subtract), writes=["rl", "gatew"])

    def moe(l, last):
        phase()
        pos = carve([128, 32], F32)
        tmp = carve([128, 32], F32)
        sf = carve([128, 4], F32)
        xbt = [carve([128, D], BF16) for _ in range(2)]
        for t in range(NT):
            mms = [(pM0[:, 0:32], onesb[:, :], mk[:, tp, :], tp == 0, False) for tp in range(t)]
            mms.append((pM0[:, 0:32], ltri[:, :], mk[:, t, :], t == 0, True))
            P.op("pe", pe_group(mms), reads=["mk", "onesb", "ltri"], writes=[pn(pM0)])
            P.op("dve", lambda e: e.scalar_tensor_tensor(out=pos, in0=pM0[:, 0:32], scalar=float(CAP - 1), in1=ecap[:], op0=ALU.min, op1=ALU.add), reads=["ecap"], writes=[pn(pM0), "pos"])
            for k in range(2):
                P.op("dve", lambda e, k=k, t=t: e.tensor_tensor(out=tmp, in0=pos, in1=oh[:, t, k, :], op=ALU.mult), reads=["pos", "oh"], writes=["tmp"])
                P.op("dve", lambda e, k=k: e.tensor_reduce(out=sf[:, k:k + 1], in_=tmp, axis=AX.X, op=ALU.add), reads=["tmp"], writes=["sf"])
            P.op("dve", lambda e, t=t: e.tensor_copy(out=slots[:, t, :], in_=sf[:, 0:2]), reads=["sf"], writes=["slots"])
            i2 = t % 2
            P.dma("sp", xbt[i2], x1b_scr[t * 128:(t + 1) * 128, :], reads=["x1b_scr"], writes=["xbt%d" % i2])
            for k in range(2):
                P.idma(lambda g, t=t, k=k, i2=i2: g.indirect_dma_start(out=xg_scr[:, :], out_offset=bass.IndirectOffsetOnAxis(ap=slots[:, t, k:k + 1], axis=0),
                                                                      in_=xbt[i2], in_offset=None),
                       reads=["xbt%d" % i2, "slots"], writes=["xg_scr"])
        phase()
        wbuf = []
        e0 = XT[:, :, :].rearrange("p a b -> p (a b)")
        wbuf.append((e0[:, 0:8192].rearrange("p (a b) -> p a b", a=16), e0[:, 8192:16384].rearrange("p (a b) -> p a b", a=16),
                     e0[:, 16384:24576].rearrange("p (a b) -> p a b", a=4)))
        wbuf.append((carve([128, 16, 512], BF16), carve([128, 16, 512], BF16), carve([128, 4, D], BF16)))
        ysb = [e0[:, 24576:28672].bitcast(F32), e0[:, 28672:32768].bitcast(F32)]
        xg = [carve([128, D], BF16) for _ in range(2)]
        xgT = [carve([128, 16, CAP], BF16) for _ in range(2)]
        hTm = [carve([128, 4, CAP], BF16) for _ in range(2)]
        sg = [carve([128, CAP], F32) for _ in range(2)]
        psT = [pS0[:, :].bitcast(BF16), pS1[:, :].bitcast(BF16)]

        wds = [carve([128, 2, D], F32) for _ in range(2)]

        def load_w_exp(ex):
            wg, wu, wd = wbuf[ex % 2]
            wn = "wexp%d" % (ex % 2)
            P.dma("pool", wg, W["w_gate"][l, ex].rearrange("(kc p) f -> p kc f", p=128), writes=[wn + "g"])
            P.dma("pool", wu, W["w_up"][l, ex].rearrange("(kc p) f -> p kc f", p=128), writes=[wn + "g"])
            wdv = W["w_down"][l, ex].rearrange("(kc p) f -> p kc f", p=128)
            for hf in range(2):
                P.dma("sp", wds[hf], wdv[:, 2 * hf:2 * hf + 2, :], writes=["wds%d" % hf])

        def cast_wd(ex):
            wg, wu, wd = wbuf[ex % 2]
            wn = "wexp%d" % (ex % 2)
            for hf in range(2):
                for j in range(2):
                    kc = 2 * hf + j
                    if j == 0:
                        P.op("act", lambda e, kc=kc, hf=hf, j=j: e.activation(out=wd[:, kc, :], in_=wds[hf][:, j, :], func=AF.Copy), reads=["wds%d" % hf], writes=[wn + "d"])
                    else:
                        P.op("dve", lambda e, kc=kc, hf=hf, j=j: e.tensor_copy(out=wd[:, kc, :], in_=wds[hf][:, j, :]), reads=["wds%d" % hf], writes=[wn + "d"])

        def prep(ex):
            b2 = ex % 2
            for s_ in range(CAP // 128):
                r0 = ex * CAP + s_ * 128
                P.dma("sp", xg[s_], xg_scr[r0:r0 + 128, :], reads=["xg_scr"], writes=["xg%d" % s_])
                for g in range(2):
                    pt_ = psT[g]
                    pnm = pn((pS0, pS1)[g])
                    P.op("pe", (lambda pt_=pt_, g=g, s_=s_: lambda e: [e.transpose(out=pt_[:, j * 128:(j + 1) * 128], in_=xg[s_][:, (g * 8 + j) * 128:(g * 8 + j + 1) * 128], identity=identb[:]) for j in range(8)][-1])(),
                         reads=["xg%d" % s_, "identb"], writes=[pnm])
                    if g == 0:
                        P.op("act", lambda e, pt_=pt_, g=g, s_=s_: e.activation(out=xgT[b2][:, g * 8:(g + 1) * 8, s_ * 128:(s_ + 1) * 128], in_=pt_[:, 0:1024].rearrange("p (a b) -> p a b", a=8), func=AF.Copy),
                             writes=[pnm, "xgT%d" % b2])
                    else:
                        P.op("dve", lambda e, pt_=pt_, g=g, s_=s_: e.tensor_copy(out=xgT[b2][:, g * 8:(g + 1) * 8, s_ * 128:(s_ + 1) * 128], in_=pt_[:, 0:1024].rearrange("p (a b) -> p a b", a=8)),
                             writes=[pnm, "xgT%d" % b2])

        def gateup(ex):
            b2 = ex % 2
            wg, wu, wd = wbuf[b2]
            wn = "wexp%d" % b2
            for fc in range(4):
                pg, pu = ((pA, pB), (pM0, pM1))[fc % 2]
                s2 = fc % 2
                mms = [(pg[:, 0:CAP], wg[:, kc, fc * 128:(fc + 1) * 128], xgT[b2][:, kc, :], kc == 0, kc == 15) for kc in range(16)]
                P.op("pe", pe_group(mms), reads=[wn + "g", "xgT%d" % b2], writes=[pn(pg)])
                mms = [(pu[:, 0:CAP], wu[:, kc, fc * 128:(fc + 1) * 128], xgT[b2][:, kc, :], kc == 0, kc == 15) for kc in range(16)]
                P.op("pe", pe_group(mms), reads=[wn + "g", "xgT%d" % b2], writes=[pn(pu)])
                P.op("act", lambda e, pg=pg, s2=s2: e.activation(out=sg[s2], in_=pg[:, 0:CAP], func=AF.Silu), writes=[pn(pg), "sg%d" % s2])
                P.op("dve", lambda e, fc=fc, pu=pu, s2=s2: e.tensor_tensor(out=hTm[b2][:, fc, :], in0=sg[s2], in1=pu[:, 0:CAP], op=ALU.mult), reads=["sg%d" % s2], writes=[pn(pu), "hTm%d" % b2])

        def down(ex):
            b2 = ex % 2
            wg, wu, wd = wbuf[b2]
            wn = "wexp%d" % b2
            for s_ in range(CAP // 128):
                for cc in range(4):
                    pb = (pO0, pO1)[cc % 2]
                    mms = [(pb[:, :], hTm[b2][:, kc, s_ * 128:(s_ + 1) * 128], wd[:, kc, cc * 512:(cc + 1) * 512], kc == 0, kc == 3) for kc in range(4)]
                    P.op("pe", pe_group(mms), reads=[wn + "d", "hTm%d" % b2], writes=[pn(pb)])
                    if cc % 2 == 0:
                        P.op("act", lambda e, pb=pb, cc=cc, s_=s_: e.activation(out=ysb[s_][:, cc * 512:(cc + 1) * 512], in_=pb[:, :], func=AF.Copy), writes=[pn(pb), "ysb%d" % s_])
                    else:
                        P.op("dve", lambda e, pb=pb, cc=cc, s_=s_: e.tensor_copy(out=ysb[s_][:, cc * 512:(cc + 1) * 512], in_=pb[:, :]), writes=[pn(pb), "ysb%d" % s_])
                r0 = ex * CAP + s_ * 128
                P.dma("sp", y_scr[r0:r0 + 128, :], ysb[s_], reads=["ysb%d" % s_], writes=["y_scr"])

        load_w_exp(0)
        cast_wd(0)
        load_w_exp(1)
        prep(0)
        for ex in range(NEXP):
            gateup(ex)
            if ex + 1 < NEXP:
                prep(ex + 1)
                cast_wd(ex + 1)
            down(ex)
            if ex + 2 < NEXP:
                load_w_exp(ex + 2)
        phase()
        lng = carve([128, D], F32)
        lnb = carve([128, D], F32)
        P.dma("sp", lng, W["ln2_g"][l:l + 1, :].broadcast_to([128, D]), writes=["lng"])
        P.dma("sp", lnb, W["ln2_b"][l:l + 1, :].broadcast_to([128, D]), writes=["lnb"])
        y0 = [carve([128, D], F32) for _ in range(2)]
        y1 = [carve([128, D], F32) for _ in range(2)]
        x1t = [carve([128, D], F32) for _ in range(2)]
        st = carve([128, 16], F32)
        dst = out_d if last else xres_scr
        for t in range(NT):
            i2 = t % 2
            rows = slice(t * 128, (t + 1) * 128)
            P.dma("sp", x1t[i2], x1_scr[rows, :], reads=["x1_scr"], writes=["x1t%d" % i2])
            P.idma(lambda g, t=t, i2=i2: g.indirect_dma_start(out=y0[i2], out_offset=None, in_=y_scr[:, :], in_offset=bass.IndirectOffsetOnAxis(ap=slots[:, t, 0:1], axis=0)), reads=["y_scr", "slots"], writes=["y0%d" % i2])
            P.idma(lambda g, t=t, i2=i2: g.indirect_dma_start(out=y1[i2], out_offset=None, in_=y_scr[:, :], in_offset=bass.IndirectOffsetOnAxis(ap=slots[:, t, 1:2], axis=0)), reads=["y_scr", "slots"], writes=["y1%d" % i2])
            P.op("dve", lambda e, t=t, i2=i2: e.tensor_scalar(out=y0[i2], in0=y0[i2], scalar1=gatew[:, t, 0:1], scalar2=None, op0=ALU.mult), reads=["gatew"], writes=["y0%d" % i2])
            P.op("dve", lambda e, t=t, i2=i2: e.scalar_tensor_tensor(out=y0[i2], in0=y1[i2], scalar=gatew[:, t, 1:2], in1=y0[i2], op0=ALU.mult, op1=ALU.add), reads=["gatew", "y1%d" % i2], writes=["y0%d" % i2])
            if stage != "full" and l == 0:
                P.dma("sp", dbg2_d[rows, :], y0[i2], reads=["y0%d" % i2], writes=["dbg2"])
            P.op("dve", lambda e, i2=i2: e.scalar_tensor_tensor(out=y0[i2], in0=x1t[i2], scalar=ALPHA, in1=y0[i2], op0=ALU.mult, op1=ALU.add), reads=["x1t%d" % i2], writes=["y0%d" % i2])
            y, yn, out, outn = y0[i2], "y0%d" % i2, x1t[i2], "x1t%d" % i2
            P.op("dve", lambda e, y=y: e.tensor_reduce(out=st[:, 0:1], in_=y, axis=AX.X, op=ALU.add), reads=[yn], writes=["st"])
            P.op("dve", lambda e: e.tensor_scalar(out=st[:, 1:2], in0=st[:, 0:1], scalar1=-1.0 / D, scalar2=None, op0=ALU.mult), writes=["st"])
            P.op("act", lambda e, y=y: e.activation(out=y, in_=y, func=AF.Identity, bias=st[:, 1:2], scale=1.0), reads=["st"], writes=[yn])
            P.op("dve", lambda e: e.memset(st[:, 2:3], 0.0), writes=["st"])
            P.op("act", lambda e, y=y, out=out: e.activation(out=out, in_=y, func=AF.Square, accum_out=st[:, 2:3]), reads=[yn], writes=[outn, "st"])
            P.op("act", lambda e: e.activation(out=st[:, 3:4], in_=st[:, 2:3], func=AF.Ln, scale=1.0 / D, bias=epsr[:, 1:2]), reads=["epsr"], writes=["st"])
            P.op("act", lambda e: e.activation(out=st[:, 3:4], in_=st[:, 3:4], func=AF.Exp, scale=-0.5), writes=["st"])
            P.op("dve", lambda e, y=y, out=out: e.scalar_tensor_tensor(out=out, in0=y, scalar=st[:, 3:4], in1=lng, op0=ALU.mult, op1=ALU.mult), reads=[yn, "st", "lng"], writes=[outn])
            P.op("pool", lambda e, out=out: e.tensor_tensor(out=out, in0=out, in1=lnb, op=ALU.add), reads=["lnb"], writes=[outn])
            P.dma("sp", dst[rows, :], out, reads=[outn], writes=["dst"])

    x_src = x_d
    for l in range(n_layers):
        last = (l == n_layers - 1)
        if layer(l, x_src, last) == "stop":
            break
        if stage in ("ln1", "odbg"):
            break
        moe(l, last)
        x_src = xres_scr
    P.barrier()
    P.emit()
    return nc


_CONSTS = None


def kernel(**inputs):
    global _CONSTS
    if _CONSTS is None:
        _CONSTS = make_consts()
    nc = build()
    x = np.ascontiguousarray(inputs["x"], dtype=np.float32)
    shared = {k: np.ascontiguousarray(inputs[k], dtype=np.float32) for k in W_SHAPES if k != "w_in"}
    shared["w_in"] = relayout_w_in(np.asarray(inputs["w_in"], dtype=np.float32))
    for k, v in _CONSTS.items():
        shared["c_" + k] = np.ascontiguousarray(v.reshape(CONST_SHAPES[k]), dtype=np.float32)
    in_maps = []
    for b in range(4):
        m = dict(shared)
        m["x"] = x[b]
        in_maps.append(m)
    res = run_bass_kernel_spmd(nc, in_maps, core_ids=list(range(4)))
    return np.stack([np.asarray(r["out"], dtype=np.float32) for r in res.results], axis=0)
```
